# Optimizing a Trainium2 kernel written in Bass

```python
import math
import jax, jax.numpy as jnp
from jax import lax
import numpy as np


D_MODEL = 2048
BATCH = 16
SEQ = 2048
DEPTH = 1
DEC_BATCH = 16
DEC_SEQ = 64
PAST_LEN = 2048

CHUNK = 64

FOX_HEADS = 8
FOX_HD = 128
FOX_W = FOX_HEADS * FOX_HD
Q_BLOCK = 128
FORGET_BIAS_MEAN = 2.0
NEG_INF = -1e30

RWKV_HEADS = 16
RWKV_HD = 64
RWKV_W = RWKV_HEADS * RWKV_HD
DECAY_RANK = 64
ICLR_RANK = 64
GATE_RANK = 160
RWKV_SIZES = (RWKV_W, RWKV_W, RWKV_W, DECAY_RANK, ICLR_RANK, GATE_RANK)
RWKV_SHIFT_W = sum(RWKV_SIZES)
RWKV_OFFSETS = tuple(int(o) for o in np.cumsum(RWKV_SIZES)[:-1])
DECAY_SCALE = math.exp(-0.5)
GN_EPS = 64e-5

N_BRANCH = 2
IN_SIZES = (FOX_W, FOX_W, FOX_W, FOX_HEADS, RWKV_SHIFT_W, N_BRANCH * D_MODEL)
IN_WIDTH = sum(IN_SIZES)
IN_OFFSETS = tuple(int(o) for o in np.cumsum(IN_SIZES)[:-1])

N_GROUPS = 4
EXPERTS_PER_GROUP = 8
N_EXPERTS = N_GROUPS * EXPERTS_PER_GROUP
TOP_K = 2
D_EXPERT = 512
MOE_BLOCK = 128

PLE_DIM = 256

NORM_EPS = 1e-6

kernel_name = 'streaming_fox_rwkv7_hmoe_step'


def rmsnorm(x, g):
    xf = x.astype(jnp.float32)
    y = xf * lax.rsqrt(jnp.mean(xf * xf, axis=-1, keepdims=True) + NORM_EPS)
    return (y * g.astype(jnp.float32)).astype(x.dtype)


def forgetting_attention(q, k, v, logf, q_offset):
    Lq = q.shape[1]
    F = jnp.transpose(jnp.cumsum(logf, axis=1), (0, 2, 1))
    scale = FOX_HD ** -0.5
    outs = []
    for start in range(0, Lq, Q_BLOCK):
        stop = min(start + Q_BLOCK, Lq)
        key_end = q_offset + stop
        qb = q[:, start:stop]
        kb = k[:, :key_end]
        vb = v[:, :key_end]
        tq = q_offset + jnp.arange(start, stop)
        ts = jnp.arange(key_end)
        logits = jnp.einsum('bqhd,bkhd->bhqk', qb, kb).astype(jnp.float32) * scale
        logits = logits + F[:, :, q_offset + start:q_offset + stop, None] - F[:, :, None, :key_end]
        logits = jnp.where(ts[None, :] <= tq[:, None], logits, NEG_INF)
        probs = jax.nn.softmax(logits, axis=-1).astype(v.dtype)
        outs.append(jnp.einsum('bhqk,bkhd->bqhd', probs, vb))
    return jnp.concatenate(outs, axis=1)


def rwkv7_time_mix(cols, shift0, wkv0, mu_shift, w0, w_w2, a0, w_a2, w_g2, k_k, k_a, r_k, lnx_g, lnx_b):
    B, L, _ = cols.shape
    f32 = jnp.float32
    prev = jnp.concatenate([shift0[:, None].astype(cols.dtype), cols[:, :-1]], axis=1)
    xm = cols + (prev - cols) * mu_shift
    r, k, v, xw, xa, xg = jnp.split(xm, RWKV_OFFSETS, axis=-1)
    log_w = -DECAY_SCALE * jax.nn.sigmoid((w0 + jnp.tanh(xw) @ w_w2).astype(f32))
    a = jax.nn.sigmoid((a0 + xa @ w_a2).astype(f32))
    g = (jax.nn.sigmoid(xg) @ w_g2).astype(f32)
    heads = lambda t: t.astype(f32).reshape(B, L, RWKV_HEADS, RWKV_HD)
    r, k, v, a, log_w = heads(r), heads(k), heads(v), heads(a), heads(log_w)
    kk = k * k_k.astype(f32).reshape(RWKV_HEADS, RWKV_HD)
    kk = kk / jnp.maximum(jnp.sqrt(jnp.sum(kk * kk, axis=-1, keepdims=True)), 1e-12)
    k_rep = k * (1.0 + (a - 1.0) * k_a.astype(f32).reshape(RWKV_HEADS, RWKV_HD))
    decay = jnp.exp(log_w)

    def step(S, inp):
        r_t, w_t, k_t, kk_t, a_t, v_t = inp
        Sk = jnp.einsum('bhvk,bhk->bhv', S, kk_t)
        S = (S * w_t[:, :, None, :] - Sk[..., None] * (a_t * kk_t)[:, :, None, :]
             + v_t[..., None] * k_t[:, :, None, :])
        return S, jnp.einsum('bhvk,bhk->bhv', S, r_t)

    xs = tuple(jnp.moveaxis(t, 1, 0) for t in (r, decay, k_rep, kk, a, v))
    S_final, y = lax.scan(step, wkv0.astype(f32), xs)
    y = jnp.moveaxis(y, 0, 1)
    mu = jnp.mean(y, axis=-1, keepdims=True)
    var = jnp.mean(jnp.square(y - mu), axis=-1, keepdims=True)
    y = ((y - mu) * lax.rsqrt(var + GN_EPS)).reshape(B, L, RWKV_W) * lnx_g + lnx_b
    bonus = jnp.sum(r * k_rep * r_k.astype(f32), axis=-1, keepdims=True) * v
    out = ((y + bonus.reshape(B, L, RWKV_W)) * g).astype(cols.dtype)
    return out, cols[:, -1], S_final.astype(cols.dtype)


def routed_ffn(h, w_rg, b_rg, w_re, b_re, w1, w3, w2):
    T, D = h.shape
    f32 = jnp.float32
    tok = jnp.arange(T)
    group_logits = (h @ w_rg + b_rg).astype(f32)
    g_idx = jnp.argmax(group_logits, axis=-1)
    p_group = jax.nn.softmax(group_logits, axis=-1)[tok, g_idx]
    exp_logits = (h @ w_re + b_re).astype(f32).reshape(T, N_GROUPS, EXPERTS_PER_GROUP)
    in_group = exp_logits[tok, g_idx]
    top_v, top_i = lax.top_k(in_group, TOP_K)
    gate = p_group[:, None] * jax.nn.softmax(top_v, axis=-1)
    expert = g_idx[:, None] * EXPERTS_PER_GROUP + top_i
    A = T * TOP_K
    e_flat = expert.reshape(A)
    order = jnp.argsort(e_flat)
    e_sorted = e_flat[order]
    counts = jnp.bincount(e_flat, length=N_EXPERTS)
    padded = (counts + MOE_BLOCK - 1) // MOE_BLOCK * MOE_BLOCK
    starts = jnp.cumsum(counts) - counts
    padded_ends = jnp.cumsum(padded)
    dest = padded_ends[e_sorted] - padded[e_sorted] + jnp.arange(A) - starts[e_sorted]
    n_blocks = -(-A // MOE_BLOCK) + N_EXPERTS
    slots = n_blocks * MOE_BLOCK
    slot_tok = jnp.full((slots,), T, jnp.int32).at[dest].set(jnp.repeat(tok, TOP_K)[order].astype(jnp.int32))
    slot_gate = jnp.zeros((slots,), f32).at[dest].set(gate.reshape(A)[order])
    block_expert = jnp.minimum(jnp.searchsorted(padded_ends, jnp.arange(n_blocks) * MOE_BLOCK, side='right'), N_EXPERTS - 1)
    h_pad = jnp.concatenate([h, jnp.zeros((1, D), h.dtype)], axis=0)
    xb = h_pad[slot_tok].reshape(n_blocks, MOE_BLOCK, D)

    def expert_block(args):
        xblk, e = args
        return (jax.nn.silu(xblk @ w1[e]) * (xblk @ w3[e])) @ w2[e]

    yb = lax.map(expert_block, (xb, block_expert)).reshape(slots, D)
    y = jnp.zeros((T + 1, D), h.dtype).at[slot_tok].add(yb * slot_gate[:, None].astype(h.dtype))
    return y[:T]


def setup_inputs(seed: int = 0) -> dict:
    key = jax.random.key(seed)
    ks = iter(jax.random.split(key, 48))
    f32 = jnp.float32
    L = DEPTH
    D = D_MODEL

    def normal(shape, scale=1.0):
        return jax.random.normal(next(ks), shape, f32) * scale

    def uniform(shape):
        return jax.random.uniform(next(ks), shape, f32)

    return {
        'x_prompt': normal((BATCH, SEQ, D)),
        'x_sample': normal((DEC_BATCH, DEC_SEQ, D)),
        'cache_k': normal((L, DEC_BATCH, PAST_LEN, FOX_HEADS, FOX_HD)),
        'cache_v': normal((L, DEC_BATCH, PAST_LEN, FOX_HEADS, FOX_HD)),
        'cache_logf': jax.nn.log_sigmoid(FORGET_BIAS_MEAN + normal((L, DEC_BATCH, PAST_LEN, FOX_HEADS))),
        'state_wkv': normal((L, DEC_BATCH, RWKV_HEADS, RWKV_HD, RWKV_HD), 0.3),
        'state_shift': normal((L, DEC_BATCH, RWKV_SHIFT_W)),
        'p_prompt': normal((L, BATCH, SEQ, PLE_DIM)),
        'p_sample': normal((L, DEC_BATCH, DEC_SEQ, PLE_DIM)),
        'g_mix': 1.0 + normal((L, D), 0.1),
        'w_in': normal((L, D, IN_WIDTH), D ** -0.5),
        'b_f': FORGET_BIAS_MEAN + normal((L, FOX_HEADS), 0.5),
        'mu_shift': uniform((L, RWKV_SHIFT_W)),
        'w0': normal((L, RWKV_W), 0.5),
        'w_w2': normal((L, DECAY_RANK, RWKV_W), DECAY_RANK ** -0.5),
        'a0': normal((L, RWKV_W), 0.1),
        'w_a2': normal((L, ICLR_RANK, RWKV_W), ICLR_RANK ** -0.5),
        'w_g2': normal((L, GATE_RANK, RWKV_W), GATE_RANK ** -0.5),
        'k_k': 1.0 + normal((L, RWKV_W), 0.1),
        'k_a': 1.0 + normal((L, RWKV_W), 0.1),
        'r_k': normal((L, RWKV_HEADS, RWKV_HD), 0.1),
        'lnx_g': 1.0 + normal((L, RWKV_W), 0.1),
        'lnx_b': normal((L, RWKV_W), 0.01),
        'w_fox_up': normal((L, FOX_W, D), FOX_W ** -0.5),
        'w_rwkv_up': normal((L, RWKV_W, D), RWKV_W ** -0.5),
        'w_o': normal((L, D, D), D ** -0.5),
        'g_ffn': 1.0 + normal((L, D), 0.1),
        'w_rg': normal((L, D, N_GROUPS), D ** -0.5),
        'b_rg': normal((L, N_GROUPS), 0.01),
        'w_re': normal((L, D, N_EXPERTS), D ** -0.5),
        'b_re': normal((L, N_EXPERTS), 0.01),
        'w1': normal((L, N_EXPERTS, D, D_EXPERT), D ** -0.5),
        'w3': normal((L, N_EXPERTS, D, D_EXPERT), D ** -0.5),
        'w2': normal((L, N_EXPERTS, D_EXPERT, D), D_EXPERT ** -0.5),
        'w_ple': normal((L, PLE_DIM, D), PLE_DIM ** -0.5),
        'w_pg': normal((L, D, D), D ** -0.5),
        'b_pg': normal((L, D), 0.01),
        'g_final': 1.0 + normal((D,), 0.1),
    }


def reference(x_prompt, x_sample, cache_k, cache_v, cache_logf, state_wkv, state_shift, p_prompt, p_sample,
              g_mix, w_in, b_f, mu_shift, w0, w_w2, a0, w_a2, w_g2, k_k, k_a, r_k, lnx_g, lnx_b,
              w_fox_up, w_rwkv_up, w_o, g_ffn, w_rg, b_rg, w_re, b_re, w1, w3, w2, w_ple, w_pg, b_pg, g_final):

    def layer(i, x, p, past_k, past_v, past_logf, wkv0, shift0):
        B, L, D = x.shape
        h = rmsnorm(x, g_mix[i])
        z = h @ w_in[i]
        q, k, v, f_logit, rw_cols, gate_logits = jnp.split(z, IN_OFFSETS, axis=-1)
        q = q.reshape(B, L, FOX_HEADS, FOX_HD)
        k = k.reshape(B, L, FOX_HEADS, FOX_HD)
        v = v.reshape(B, L, FOX_HEADS, FOX_HD)
        logf = jax.nn.log_sigmoid((f_logit + b_f[i]).astype(jnp.float32))
        k_all = jnp.concatenate([past_k.astype(k.dtype), k], axis=1)
        v_all = jnp.concatenate([past_v.astype(v.dtype), v], axis=1)
        logf_all = jnp.concatenate([past_logf.astype(jnp.float32), logf], axis=1)
        fox = forgetting_attention(q, k_all, v_all, logf_all, past_k.shape[1]).reshape(B, L, FOX_W)
        rw, shift_last, wkv = rwkv7_time_mix(rw_cols, shift0, wkv0, mu_shift[i], w0[i], w_w2[i], a0[i], w_a2[i],
                                             w_g2[i], k_k[i], k_a[i], r_k[i], lnx_g[i], lnx_b[i])
        g_fox, g_rw = jnp.split(jax.nn.sigmoid(gate_logits), N_BRANCH, axis=-1)
        merged = g_fox * (fox @ w_fox_up[i]) + g_rw * (rw @ w_rwkv_up[i])
        x = x + merged @ w_o[i]
        ffn = routed_ffn(rmsnorm(x, g_ffn[i]).reshape(B * L, D), w_rg[i], b_rg[i], w_re[i], b_re[i],
                         w1[i], w3[i], w2[i]).reshape(B, L, D)
        x = x + ffn
        x = x + jax.nn.sigmoid(x @ w_pg[i] + b_pg[i]) * (p @ w_ple[i])
        return x, (k, v, logf.astype(x.dtype), wkv, shift_last)

    dt = x_prompt.dtype
    bp = x_prompt.shape[0]
    empty_kv = jnp.zeros((bp, 0, FOX_HEADS, FOX_HD), dt)
    empty_logf = jnp.zeros((bp, 0, FOX_HEADS), dt)
    wkv_zero = jnp.zeros((bp, RWKV_HEADS, RWKV_HD, RWKV_HD), dt)
    shift_zero = jnp.zeros((bp, RWKV_SHIFT_W), dt)

    xp, xs = x_prompt, x_sample
    prompt_states, sample_states = [], []
    for i in range(DEPTH):
        xp, st = layer(i, xp, p_prompt[i], empty_kv, empty_kv, empty_logf, wkv_zero, shift_zero)
        prompt_states.append(st)
        xs, st = layer(i, xs, p_sample[i], cache_k[i], cache_v[i], cache_logf[i], state_wkv[i], state_shift[i])
        sample_states.append(st)

    def stacked(states, j):
        return jnp.stack([s[j] for s in states], axis=0)

    return (rmsnorm(xp, g_final), rmsnorm(xs, g_final),
            stacked(prompt_states, 0), stacked(prompt_states, 1), stacked(prompt_states, 2),
            stacked(prompt_states, 3), stacked(prompt_states, 4),
            stacked(sample_states, 0), stacked(sample_states, 1), stacked(sample_states, 2),
            stacked(sample_states, 3), stacked(sample_states, 4))
```

```python
import numpy as np
from contextlib import ExitStack
import concourse.bass as bass
import concourse.mybir as mybir
from concourse.bass_utils import run_bass_kernel_spmd

F32 = mybir.dt.float32
F32R = mybir.dt.float32r
I32 = mybir.dt.int32
U32 = mybir.dt.uint32
AF = mybir.ActivationFunctionType
ALU = mybir.AluOpType
AX = mybir.AxisListType

D = 2048
NH = 8
HD = 128
FW = 1024
RH = 16
RD = 64
RW = 1024
RSW = 3360
INW = 10536
OFF_Q, OFF_K, OFF_V, OFF_F, OFF_RW, OFF_G = 0, 1024, 2048, 3072, 3080, 6440
NEXP = 32
DE = 512
PLE = 256
NORM_EPS = 1e-6
STQ = "act"
GN_EPS = 64e-5
DECAY_SCALE = float(np.exp(-0.5))


class T:
    __slots__ = ("w", "r", "name")

    def __init__(self, name=""):
        self.w = None
        self.r = {}
        self.name = name


class Sched:
    NDS = 16

    def __init__(self, nc):
        self.nc = nc
        self.E = dict(pe=nc.tensor, act=nc.scalar, dve=nc.vector, pool=nc.gpsimd, sp=nc.sync)
        self.csem = {e: nc.alloc_semaphore(name=f"c_{e}") for e in ("pe", "act", "dve", "pool")}
        self.ccnt = {e: 0 for e in self.csem}
        self.dsem = {q: [nc.alloc_semaphore(name=f"d_{q}{i}") for i in range(self.NDS)]
                     for q in ("sp", "pool", "act")}
        self.dcnt = {q: [0] * self.NDS for q in self.dsem}
        self.dnext = {q: 0 for q in self.dsem}
        self.waited = {}
        self.nwaits = 0
        self.ninst = 0

    def _wait(self, eng, tok):
        sem, val, src = tok
        if src == eng and eng == "pe":
            return
        key = (eng, id(sem))
        if self.waited.get(key, 0) >= val:
            return
        self.E[eng].wait_ge(sem, val)
        self.nwaits += 1
        self.waited[key] = val

    def _deps(self, eng, reads, writes):
        for t in reads:
            if t.w is not None:
                self._wait(eng, t.w)
        for t in writes:
            if t.w is not None:
                self._wait(eng, t.w)
            for tok in t.r.values():
                self._wait(eng, tok)

    def _mark(self, tok, reads, writes):
        k = id(tok[0])
        for t in reads:
            t.r[k] = tok
        for t in writes:
            t.w = tok
            t.r = {}

    def op(self, eng, fn, reads=(), writes=()):
        self._deps(eng, reads, writes)
        inst = fn(self.E[eng])
        self.ccnt[eng] += 1
        inst.then_inc(self.csem[eng], 1)
        self.ninst += 1
        self._mark((self.csem[eng], self.ccnt[eng], eng), reads, writes)

    def dma(self, q, out, in_, reads=(), writes=(), fn=None, **kw):
        self._deps(q, reads, writes)
        i = self.dnext[q]
        self.dnext[q] = (i + 1) % self.NDS
        sem = self.dsem[q][i]
        if self.dcnt[q][i] > 0:
            self._wait(q, (sem, self.dcnt[q][i], "dma"))
        if fn is not None:
            inst = fn(self.E[q])
        else:
            inst = self.E[q].dma_start(out=out, in_=in_, **kw)
        self.dcnt[q][i] += 16
        inst.then_inc(sem, 16)
        self.ninst += 1
        self._mark((sem, self.dcnt[q][i], "dma"), reads, writes)

    def barrier(self, engines=("pe", "act", "dve", "pool", "sp")):
        for e in engines:
            for s, c in self.ccnt.items():
                if c > 0:
                    self._wait(e, (self.csem[s], c, "x"))
            for q in self.dsem:
                for i in range(self.NDS):
                    if self.dcnt[q][i] > 0:
                        self._wait(e, (self.dsem[q][i], self.dcnt[q][i], "dma"))


def r32(ap):
    return ap.bitcast(F32R)


class Prog:
    def __init__(self, L, LS, PAST, phases=None):
        self.L, self.LS, self.PAST = L, LS, PAST
        self.TT = 2 * L + 2 * LS
        self.phases = phases
        nc = bass.Bass("TRN2", target_bir_lowering=False)
        nc.dge_precook = False
        self.nc = nc
        self.s = Sched(nc)
        self.ins = {}
        self.outs = {}
        self.build()

    def din(self, name, shape, dt=F32):
        t = self.nc.dram_tensor(name, list(shape), dt, kind="ExternalInput").ap()
        self.ins[name] = t
        return t

    def cin(self, name, shape, dt=F32):
        if name in self.ins:
            return self.ins[name]
        return self.din(name, shape, dt)

    def dout(self, name, shape, dt=F32):
        t = self.nc.dram_tensor(name, list(shape), dt, kind="ExternalOutput").ap()
        self.outs[name] = t
        return t

    def dscr(self, name, shape, dt=F32):
        return self.nc.dram_tensor(name, list(shape), dt, kind="Internal").ap()

    def build(self):
        nc, s = self.nc, self.s
        L, LS, PAST, TT = self.L, self.LS, self.PAST, self.TT
        x_all = self.din("x_all", [TT, D])
        w_in = self.din("w_in", [D, INW])
        g_mix = self.din("g_mix", [1, D])
        b_f = self.din("b_f", [1, NH])
        ident_d = self.din("ident", [128, 128])
        k_all = self.dout("k_all", [TT, FW])
        v_all = self.dout("v_all", [TT, FW])
        logf_all = self.dout("logf_all", [TT, NH])
        shift_out = self.dout("shift_out", [4, RSW])
        ends = {L - 1: 0, 2 * L - 1: 1, 2 * L + LS - 1: 2, 2 * L + 2 * LS - 1: 3}
        qT_d = self.dscr("qT_d", [NH, 128, TT])
        kT_d = self.dscr("kT_d", [NH, 128, TT])
        rwT_d = self.dscr("rwT_d", [53, 64, TT])
        sgT_d = self.dscr("sgT_d", [32, 128, TT])
        self.dbg = {}
        if self.phases is not None and "dbg12" in self.phases:
            self.dbg["qT"] = self.dout("dbg_qT", [NH, 128, TT])
            self.dbg["rwT"] = self.dout("dbg_rwT", [53, 64, TT])
            self.dbg["sgT"] = self.dout("dbg_sgT", [32, 128, TT])
            qT_d, rwT_d, sgT_d = self.dbg["qT"], self.dbg["rwT"], self.dbg["sgT"]

        groups = []
        G = 512
        for sq in range(2):
            for g0 in range(0, L, G):
                groups.append((sq * L + g0, min(G, L - g0)))
        groups.append((2 * L, 2 * LS))

        with ExitStack() as es:
            ident = es.enter_context(nc.sbuf_tensor("sb_ident", [128, 128], F32))
            gbc = es.enter_context(nc.sbuf_tensor("sb_gbc", [128, D], F32))
            bfbc = es.enter_context(nc.sbuf_tensor("sb_bfbc", [128, NH], F32))
            xt0 = es.enter_context(nc.sbuf_tensor("sb_xt0", [128, D], F32))
            xt1 = es.enter_context(nc.sbuf_tensor("sb_xt1", [128, D], F32))
            xn = es.enter_context(nc.sbuf_tensor("sb_xn", [128, D], F32))
            junk = es.enter_context(nc.sbuf_tensor("sb_junk", [128, D], F32))
            stat = es.enter_context(nc.sbuf_tensor("sb_stat", [128, 8], F32))
            hT = es.enter_context(nc.sbuf_tensor("sb_hT", [128, 16, G], F32R))
            ws0 = es.enter_context(nc.sbuf_tensor("sb_ws0", [128, 16, 512], F32R))
            ws1 = es.enter_context(nc.sbuf_tensor("sb_ws1", [128, 16, 512], F32R))
            stg0 = es.enter_context(nc.sbuf_tensor("sb_stg0", [128, 512], F32))
            stg1 = es.enter_context(nc.sbuf_tensor("sb_stg1", [128, 512], F32))
            stg2 = es.enter_context(nc.sbuf_tensor("sb_stg2", [128, 512], F32))
            stg3 = es.enter_context(nc.sbuf_tensor("sb_stg3", [128, 512], F32))
            lf0 = es.enter_context(nc.sbuf_tensor("sb_lf0", [128, 16], F32))
            t_ident, t_gbc, t_bfbc = T("ident"), T("gbc"), T("bfbc")
            s.dma("sp", ident[:], ident_d[:, :], writes=[t_ident])
            s.dma("sp", gbc[:], g_mix[0:1, :].partition_broadcast(128), writes=[t_gbc])
            s.dma("sp", bfbc[:], b_f[0:1, :].partition_broadcast(128), writes=[t_bfbc])
            ps = self.ps = [nc.alloc_psum_tensor(f"ps{i}", [128, 512], F32) for i in range(8)]
            t_ps = self.t_ps = [T(f"ps{i}") for i in range(8)]
            self.psn = 0

            def next_ps():
                i = self.psn
                self.psn = (i + 1) % 8
                return ps[i], t_ps[i]

            xts = [(xt0, T("xt0")), (xt1, T("xt1"))]
            wss = [(ws0, T("ws0")), (ws1, T("ws1"))]
            stgs = [(stg0, T("stg0")), (stg1, T("stg1")), (stg2, T("stg2")), (stg3, T("stg3"))]
            t_xn, t_junk, t_stat, t_hT, t_lf = T("xn"), T("junk"), T("stat"), T("hT"), T("lf")
            self.stn = 0
            self.evn = 0

            def next_stg():
                i = self.stn
                self.stn = (i + 1) % 4
                return stgs[i]

            def evac(out_ap, in_ap, reads, writes, func=None):
                self.evn += 1
                if func is not None:
                    s.op("act", lambda e: e.activation(out=out_ap, in_=in_ap, func=func), reads, writes)
                elif self.evn % 2 == 0:
                    s.op("act", lambda e: e.activation(out=out_ap, in_=in_ap, func=AF.Copy), reads, writes)
                else:
                    s.op("dve", lambda e: e.tensor_copy(out=out_ap, in_=in_ap), reads, writes)

            slabs = []
            for c0 in range(0, 1024, 512):
                slabs.append((OFF_Q + c0, 512, "q"))
            for c0 in range(0, 1024, 512):
                slabs.append((OFF_K + c0, 512, "k"))
            for c0 in range(0, 1024, 512):
                slabs.append((OFF_V + c0, 512, "v"))
            slabs.append((OFF_F, 8, "f"))
            for c0 in range(0, RSW, 512):
                slabs.append((OFF_RW + c0, min(512, RSW - c0), "rw"))
            for c0 in range(0, 4096, 512):
                slabs.append((OFF_G + c0, 512, "g"))

            xi = 0
            wi = 0
            for (tok0, ntok) in groups:
                ntile = (ntok + 127) // 128
                for ti in range(ntile):
                    n = min(128, ntok - ti * 128)
                    xt, t_xt = xts[xi % 2]
                    xi += 1
                    r0 = tok0 + ti * 128
                    s.dma("sp", xt[0:n, :], x_all[r0:r0 + n, :], writes=[t_xt])
                    s.op("act", lambda e: e.activation(out=junk[0:n, :], in_=xt[0:n, :], func=AF.Square,
                                                       accum_out=stat[0:n, 0:1]),
                         reads=[t_xt], writes=[t_junk, t_stat])
                    s.op("act", lambda e: e.activation(out=stat[0:n, 1:2], in_=stat[0:n, 0:1], func=AF.Sqrt,
                                                       scale=1.0 / D, bias=NORM_EPS),
                         reads=[t_stat], writes=[t_stat])
                    s.op("dve", lambda e: e.reciprocal(out=stat[0:n, 2:3], in_=stat[0:n, 1:2]),
                         reads=[t_stat], writes=[t_stat])
                    s.op("dve", lambda e: e.scalar_tensor_tensor(out=xn[0:n, :], in0=xt[0:n, :],
                                                                 scalar=stat[0:n, 2:3], in1=gbc[0:n, :],
                                                                 op0=ALU.mult, op1=ALU.mult),
                         reads=[t_xt, t_stat, t_gbc], writes=[t_xn])
                    for k4 in range(4):
                        pt, t_pt = next_ps()
                        for j in range(4):
                            kc = k4 * 4 + j
                            s.op("pe", lambda e: e.transpose(out=pt[:, j * 128:j * 128 + n],
                                                             in_=xn[0:n, kc * 128:(kc + 1) * 128],
                                                             identity=ident[0:n, 0:n]),
                                 reads=[t_xn, t_ident], writes=[t_pt])
                        evac(hT[:, k4 * 4:k4 * 4 + 4, ti * 128:ti * 128 + n],
                             pt[:].rearrange("p (j t) -> p j t", j=4)[:, :, 0:n],
                             reads=[t_pt], writes=[t_hT])
                dbgf = self.phases or ()
                if "p1only" in dbgf:
                    s.dma("sp", self.dbg["rwT"][0:16, :, tok0:tok0 + ntok].rearrange("k p t -> p k t"),
                          hT[:, :, 0:ntok].bitcast(F32), reads=[t_hT])
                    continue
                for (c0, ncol, kind) in slabs:
                    if any(("only_" + kk) in dbgf for kk in "qkvfg") and ("only_" + kind[0]) not in dbgf:
                        continue
                    ws, t_ws = wss[wi % 2]
                    wi += 1
                    s.dma("sp", ws[:, :, 0:ncol],
                          r32(w_in[:, c0:c0 + ncol]).rearrange("(kc p) c -> p kc c", p=128),
                          writes=[t_ws])
                    if kind in ("q", "k", "rw", "g"):
                        for cb in range(0, ncol, 128):
                            m = min(128, ncol - cb)
                            pt, t_pt = next_ps()
                            for kc in range(16):
                                s.op("pe", lambda e: e.matmul(pt[:, 0:ntok], lhsT=ws[:, kc, cb:cb + 128],
                                                              rhs=hT[:, kc, 0:ntok],
                                                              start=(kc == 0), stop=(kc == 15)),
                                     reads=[t_ws, t_hT], writes=[t_pt])
                            stg, t_stg = next_stg()
                            evac(stg[0:m, 0:ntok], pt[0:m, 0:ntok], [t_pt], [t_stg],
                                 func=(AF.Sigmoid if kind == "g" else None))
                            if kind == "q":
                                dst = qT_d[(c0 - OFF_Q + cb) // 128, :, tok0:tok0 + ntok]
                            elif kind == "k":
                                dst = kT_d[(c0 - OFF_K + cb) // 128, :, tok0:tok0 + ntok]
                            elif kind == "rw":
                                hc = (c0 - OFF_RW + cb) // 64
                                dst = rwT_d[hc, 0:min(m, 64), tok0:tok0 + ntok]
                                if m > 64:
                                    s.dma(STQ, rwT_d[hc + 1, :, tok0:tok0 + ntok], stg[64:128, 0:ntok], reads=[t_stg])
                            else:
                                dst = sgT_d[(c0 - OFF_G + cb) // 128, :, tok0:tok0 + ntok]
                            s.dma(STQ, dst, stg[0:(min(m, 64) if kind == "rw" else m), 0:ntok], reads=[t_stg])
                            if kind == "rw":
                                for te, sidx in ends.items():
                                    if tok0 <= te < tok0 + ntok:
                                        cc = c0 - OFF_RW + cb
                                        s.dma(STQ, shift_out[sidx, cc:cc + m].rearrange("(p o) -> p o", o=1),
                                              stg[0:m, te - tok0:te - tok0 + 1], reads=[t_stg])
                    if kind in ("k", "v", "f"):
                        for ti in range(ntile):
                            n = min(128, ntok - ti * 128)
                            pt, t_pt = next_ps()
                            for kc in range(16):
                                s.op("pe", lambda e: e.matmul(pt[0:n, 0:ncol],
                                                              lhsT=hT[:, kc, ti * 128:ti * 128 + n],
                                                              rhs=ws[:, kc, 0:ncol],
                                                              start=(kc == 0), stop=(kc == 15)),
                                     reads=[t_ws, t_hT], writes=[t_pt])
                            r0 = tok0 + ti * 128
                            if kind == "f":
                                s.op("dve", lambda e: e.tensor_tensor(out=lf0[0:n, 0:8], in0=pt[0:n, 0:8],
                                                                      in1=bfbc[0:n, :], op=ALU.add),
                                     reads=[t_pt, t_bfbc], writes=[t_lf])
                                s.op("act", lambda e: e.activation(out=lf0[0:n, 0:8], in_=lf0[0:n, 0:8],
                                                                   func=AF.Exp, scale=-1.0),
                                     reads=[t_lf], writes=[t_lf])
                                s.op("act", lambda e: e.activation(out=lf0[0:n, 0:8], in_=lf0[0:n, 0:8],
                                                                   func=AF.Ln, bias=1.0),
                                     reads=[t_lf], writes=[t_lf])
                                s.op("dve", lambda e: e.tensor_scalar(out=lf0[0:n, 8:16], in0=lf0[0:n, 0:8],
                                                                      scalar1=-1.0, scalar2=None, op0=ALU.mult),
                                     reads=[t_lf], writes=[t_lf])
                                s.dma(STQ, logf_all[r0:r0 + n, :], lf0[0:n, 8:16], reads=[t_lf])
                            else:
                                stg, t_stg = next_stg()
                                evac(stg[0:n, 0:ncol], pt[0:n, 0:ncol], [t_pt], [t_stg])
                                dst = (k_all if kind == "k" else v_all)
                                cc = c0 - (OFF_K if kind == "k" else OFF_V)
                                s.dma(STQ, dst[r0:r0 + n, cc:cc + ncol], stg[0:n, 0:ncol], reads=[t_stg])
            s.barrier()
        self.qT_d, self.kT_d, self.rwT_d, self.sgT_d = qT_d, kT_d, rwT_d, sgT_d
        self.k_all, self.v_all, self.logf_all, self.ident_d = k_all, v_all, logf_all, ident_d
        self.x_all = x_all
        ph = self.phases or ()
        if "stop12" not in ph:
            if "noattn" not in ph:
                self.phase_attn()
            if "norwkv" not in ph:
                self.phase_rwkv()
            if "stoprw" not in ph:
                self.phase_merge()
                if "stopmerge" not in ph:
                    self.phase_experts()
                    if "stopexp" not in ph:
                        self.phase_final()
        s.barrier(engines=("sp",))

    def phase_attn(self):
        nc, s = self.nc, self.s
        L, LS, PAST, TT = self.L, self.LS, self.PAST, self.TT
        scale = float(HD) ** -0.5
        cache_k = self.din("cache_k", [2, PAST, NH, HD])
        cache_v = self.din("cache_v", [2, PAST, NH, HD])
        cache_logf = self.din("cache_logf", [2, PAST, NH])
        triu_d = self.cin("triu", [128, 128])
        ones_d = self.cin("ones", [128, 128])
        Lkmax = max(L, PAST + LS)
        nkbmax = (Lkmax + 127) // 128
        nkbp = ((nkbmax + 15) // 16) * 16
        Lqmax = max(L, LS)
        FThi_d = self.dscr("FThi_d", [4, NH, nkbp * 128], F32R)
        FTlo_d = self.dscr("FTlo_d", [4, NH, nkbp * 128], F32R)
        foxT_d = self.dscr("foxT_d", [NH, 128, TT])
        if "dbgattn" in (self.phases or ()):
            foxT_d = self.dout("dbg_foxT", [NH, 128, TT])
        self.foxT_d = foxT_d
        seqs = [(0, L, 0, None), (L, L, 0, None), (2 * L, LS, PAST, 0), (2 * L + LS, LS, PAST, 1)]
        with ExitStack() as es:
            def sb(name, shape, dt=F32):
                return es.enter_context(nc.sbuf_tensor("a_" + name, shape, dt))
            ident = sb("ident", [128, 128])
            triu = sb("triu", [128, 128])
            ones = sb("ones", [128, 128], F32R)
            onesf = sb("onesf", [128, 128])
            LF = sb("LF", [128, nkbp, NH])
            tot = sb("tot", [128, nkbp, NH])
            inc = sb("inc", [128, nkbp, NH])
            Fall = sb("Fall", [128, nkbp, NH])
            negF = sb("negF", [128, nkbp, NH])
            FThi = sb("FThi", [128, (nkbp // 16) * 128], F32R)
            FTlo = sb("FTlo", [128, (nkbp // 16) * 128], F32R)
            qT = sb("qT", [128, Lqmax], F32R)
            kT = sb("kT", [128, nkbmax * 128], F32R)
            V = sb("V", [128, nkbmax, 128], F32R)
            kc = sb("kc", [128, max(PAST // 128, 1), 128])
            Fq = sb("Fq", [128, Lqmax], F32R)
            PT0 = sb("PT0", [128, 512], F32R)
            PT1 = sb("PT1", [128, 512], F32R)
            PT2 = sb("PT2", [128, 512], F32R)
            rinv = sb("rinv", [128, 512])
            ost0 = sb("ost0", [128, 512])
            ost1 = sb("ost1", [128, 512])
            ps, t_ps = self.ps, self.t_ps
            t_c = T("consts")
            s.dma("sp", ident[:], self.ident_d[:, :], writes=[t_c])
            s.dma("sp", triu[:], triu_d[:, :], writes=[t_c])
            s.dma("sp", ones[:], r32(ones_d[:, :]), writes=[t_c])
            s.dma("sp", onesf[:], ones_d[:, :], writes=[t_c])
            t_LF, t_tot, t_inc, t_Fall, t_negF, t_FThi, t_FTlo, t_FTd = (T() for _ in range(8))
            t_qT, t_kT, t_V, t_kc, t_Fq = (T() for _ in range(5))
            s.op("dve", lambda e: e.memset(Fq[:, :].bitcast(F32), 0.0), writes=[t_Fq])
            s.op("dve", lambda e: e.memset(kT[:, :].bitcast(F32), 0.0), writes=[t_kT])
            s.op("dve", lambda e: e.memset(V[:, :, :].bitcast(F32), 0.0), writes=[t_V])
            s.op("dve", lambda e: e.memset(Fall[:, :, :], 0.0), writes=[t_Fall])
            PTs = [(PT0, T()), (PT1, T()), (PT2, T())]
            osts = [(ost0, T()), (ost1, T())]
            t_rinv = T()
            pti = 0
            psi = 0
            qbi = 0
            for si, (tok0, Lq, off, ci) in enumerate(seqs):
                Lk = off + Lq
                nkb = (Lk + 127) // 128
                s.op("dve", lambda e: e.memset(LF[:, :, :], 0.0), writes=[t_LF])
                if off:
                    s.dma("sp", LF[:, 0:off // 128, :], cache_logf[ci].rearrange("(kb p) h -> p kb h", p=128),
                          writes=[t_LF])
                for j in range(0, Lq, 128):
                    n = min(128, Lq - j)
                    s.dma("sp", LF[0:n, (off + j) // 128, :], self.logf_all[tok0 + j:tok0 + j + n, :], writes=[t_LF])
                N8 = nkb * NH
                psA, t_psA = ps[7], t_ps[7]
                psB, t_psB = ps[6], t_ps[6]
                s.op("pe", lambda e: e.matmul(psA[:, 0:N8], lhsT=triu[:, :],
                                              rhs=LF[:, 0:nkb, :].rearrange("p k h -> p (k h)"),
                                              start=True, stop=True),
                     reads=[t_c, t_LF], writes=[t_psA])
                s.op("pe", lambda e: e.matmul(psB[:, 0:N8], lhsT=onesf[:, :],
                                              rhs=LF[:, 0:nkb, :].rearrange("p k h -> p (k h)"),
                                              start=True, stop=True),
                     reads=[t_c, t_LF], writes=[t_psB])
                s.op("act", lambda e: e.activation(out=tot[:, 0:nkb, :].rearrange("p k h -> p (k h)"),
                                                   in_=psB[:, 0:N8], func=AF.Copy),
                     reads=[t_psB], writes=[t_tot])
                for h in range(NH):
                    s.op("dve", lambda e: e.tensor_tensor_scan(out=inc[:, 0:nkb, h], data0=onesf[:, 0:nkb],
                                                               data1=tot[:, 0:nkb, h], initial=0.0,
                                                               op0=ALU.mult, op1=ALU.add),
                         reads=[t_tot, t_c], writes=[t_inc])
                s.op("dve", lambda e: e.tensor_tensor(out=inc[:, 0:nkb, :], in0=inc[:, 0:nkb, :],
                                                      in1=tot[:, 0:nkb, :], op=ALU.subtract),
                     reads=[t_inc, t_tot], writes=[t_inc])
                s.op("dve", lambda e: e.tensor_tensor(out=Fall[:, 0:nkb, :].rearrange("p k h -> p (k h)"),
                                                      in0=psA[:, 0:N8],
                                                      in1=inc[:, 0:nkb, :].rearrange("p k h -> p (k h)"),
                                                      op=ALU.add),
                     reads=[t_psA, t_inc], writes=[t_Fall])
                s.op("dve", lambda e: e.tensor_scalar(out=negF[:, 0:nkb, :], in0=Fall[:, 0:nkb, :], scalar1=-1.0,
                                                      scalar2=None, op0=ALU.mult),
                     reads=[t_Fall], writes=[t_negF])
                ng = (nkb + 15) // 16
                for g in range(ng):
                    s.op("pe", lambda e: e.transpose(out=psA[:, g * 128:(g + 1) * 128],
                                                     in_=Fall[:, g * 16:(g + 1) * 16, :].rearrange("p k h -> p (k h)"),
                                                     identity=ident[:, :]),
                         reads=[t_Fall, t_c], writes=[t_psA])
                s.op("dve", lambda e: e.tensor_scalar(out=FThi[:, 0:ng * 128], in0=psA[:, 0:ng * 128],
                                                      scalar1=1.0 / scale, scalar2=None, op0=ALU.mult),
                     reads=[t_psA], writes=[t_FThi])
                s.op("dve", lambda e: e.scalar_tensor_tensor(out=FTlo[:, 0:ng * 128], in0=psA[:, 0:ng * 128],
                                                             scalar=1.0 / scale,
                                                             in1=FThi[:, 0:ng * 128].bitcast(F32),
                                                             op0=ALU.mult, op1=ALU.subtract),
                     reads=[t_psA, t_FThi], writes=[t_FTlo])
                for kb in range(nkb):
                    g, kl = kb // 16, kb % 16
                    s.dma("sp", FThi_d[si, :, kb * 128:(kb + 1) * 128],
                          FThi[kl * 8:(kl + 1) * 8, g * 128:(g + 1) * 128], reads=[t_FThi], writes=[t_FTd])
                    s.dma("sp", FTlo_d[si, :, kb * 128:(kb + 1) * 128],
                          FTlo[kl * 8:(kl + 1) * 8, g * 128:(g + 1) * 128], reads=[t_FTlo], writes=[t_FTd])
                if "attn_stopF" in (self.phases or ()):
                    continue
                for h in range(NH):
                    s.dma("sp", qT[:, 0:Lq], r32(self.qT_d[h, :, tok0:tok0 + Lq]), writes=[t_qT])
                    s.dma("sp", kT[:, off:Lk], r32(self.kT_d[h, :, tok0:tok0 + Lq]), writes=[t_kT])
                    if off:
                        s.dma("sp", kc[:, 0:off // 128, :],
                              cache_k[ci, :, h, :].rearrange("(kb p) d -> p kb d", p=128), writes=[t_kc])
                        s.dma("sp", V[:, 0:off // 128, :],
                              r32(cache_v[ci, :, h, :]).rearrange("(kb p) d -> p kb d", p=128), writes=[t_V])
                    if Lq % 128 == 0:
                        s.dma("sp", V[:, off // 128:off // 128 + Lq // 128, :],
                              r32(self.v_all[tok0:tok0 + Lq, h * 128:(h + 1) * 128]).rearrange(
                                  "(kb p) d -> p kb d", p=128), writes=[t_V])
                    else:
                        for j in range(0, Lq, 128):
                            n = min(128, Lq - j)
                            s.dma("sp", V[0:n, (off + j) // 128, :],
                                  r32(self.v_all[tok0 + j:tok0 + j + n, h * 128:(h + 1) * 128]), writes=[t_V])
                    s.dma("sp", Fq[0:1, 0:Lq], FThi_d[si, h:h + 1, off:Lk], reads=[t_FTd], writes=[t_Fq])
                    s.dma("sp", Fq[1:2, 0:Lq], FTlo_d[si, h:h + 1, off:Lk], reads=[t_FTd], writes=[t_Fq])
                    if off:
                        for k4 in range(0, off // 128, 4):
                            pt, t_pt = ps[7], t_ps[7]
                            nb = min(4, off // 128 - k4)
                            for j in range(nb):
                                s.op("pe", lambda e: e.transpose(out=pt[:, j * 128:(j + 1) * 128],
                                                                 in_=kc[:, k4 + j, :], identity=ident[:, :]),
                                     reads=[t_kc, t_c], writes=[t_pt])
                            s.op("dve", lambda e: e.tensor_copy(out=kT[:, k4 * 128:(k4 + nb) * 128],
                                                                in_=pt[:, 0:nb * 128]),
                                 reads=[t_pt], writes=[t_kT])
                    if "attn_noblocks" in (self.phases or ()):
                        continue
                    if "attn_only_prompt" in (self.phases or ()) and off:
                        continue
                    if "attn_only_sample" in (self.phases or ()) and not off:
                        continue
                    for t0 in range(0, Lq, 512):
                        nq = min(512, Lq - t0)
                        psO, t_psO = ps[3 + qbi % 2], t_ps[3 + qbi % 2]
                        psR, t_psR = ps[5], t_ps[5]
                        ost, t_ost = osts[qbi % 2]
                        qbi += 1
                        blocks = []
                        for kb in range(nkb):
                            s0 = kb * 128
                            ns = min(128, Lk - s0)
                            dlt = off + t0 - s0
                            if dlt >= ns - 1:
                                blocks.append((kb, s0, 0, False))
                            else:
                                cs = -dlt
                                assert cs >= 0
                                if cs < nq:
                                    blocks.append((kb, s0, cs, True))
                        for bi, (kb, s0, cs, diag) in enumerate(blocks):
                            first, last = bi == 0, bi == len(blocks) - 1
                            psS, t_psS = ps[psi % 3], t_ps[psi % 3]
                            psi += 1
                            PT, t_PT = PTs[pti % 3]
                            pti += 1
                            s.op("pe", lambda e: e.matmul(psS[:, cs:nq], lhsT=kT[:, s0:s0 + 128],
                                                          rhs=qT[:, t0 + cs:t0 + nq], start=True, stop=False),
                                 reads=[t_kT, t_qT], writes=[t_psS])
                            s.op("pe", lambda e: e.matmul(psS[:, cs:nq], lhsT=ones[:, :],
                                                          rhs=Fq[:, t0 + cs:t0 + nq], start=False, stop=True),
                                 reads=[t_c, t_Fq], writes=[t_psS])
                            for hh in range(1):
                                s.op("act", lambda e: e.activation(out=PT[:, cs:nq], in_=psS[:, cs:nq],
                                                                   func=AF.Exp, scale=scale,
                                                                   bias=negF[:, kb, h:h + 1]),
                                     reads=[t_psS, t_negF], writes=[t_PT])
                            if diag:
                                w = min(128, nq - cs)
                                s.op("dve", lambda e: e.tensor_tensor(out=PT[:, cs:cs + w],
                                                                      in0=PT[:, cs:cs + w].bitcast(F32),
                                                                      in1=triu[:, 0:w], op=ALU.mult),
                                     reads=[t_PT, t_c], writes=[t_PT])
                            s.op("pe", lambda e: e.matmul(psO[:, cs:nq], lhsT=V[:, kb, :],
                                                          rhs=PT[:, cs:nq], start=first, stop=last),
                                 reads=[t_V, t_PT], writes=[t_psO])
                            s.op("pe", lambda e: e.matmul(psR[:, cs:nq], lhsT=ones[:, :],
                                                          rhs=PT[:, cs:nq], start=first, stop=last),
                                 reads=[t_c, t_PT], writes=[t_psR])
                        s.op("dve", lambda e: e.reciprocal(out=rinv[:, 0:nq], in_=psR[:, 0:nq]),
                             reads=[t_psR], writes=[t_rinv])
                        s.op("dve", lambda e: e.tensor_tensor(out=ost[:, 0:nq], in0=psO[:, 0:nq],
                                                              in1=rinv[:, 0:nq], op=ALU.mult),
                             reads=[t_psO, t_rinv], writes=[t_ost])
                        s.dma(STQ, foxT_d[h, :, tok0 + t0:tok0 + t0 + nq], ost[:, 0:nq], reads=[t_ost])
            s.barrier()


    def phase_rwkv(self):
        nc, s = self.nc, self.s
        L, LS, PAST, TT = self.L, self.LS, self.PAST, self.TT
        C = 64
        state_wkv = self.din("state_wkv", [2, RH, RD, RD])
        state_shift = self.din("state_shift", [2, RSW])
        mu_d = self.din("mu_shift", [1, RSW])
        w0_d = self.din("w0", [1, RW])
        a0_d = self.din("a0", [1, RW])
        kk_d = self.din("k_k", [1, RW])
        ka_d = self.din("k_a", [1, RW])
        rk_d = self.din("r_k", [1, RW])
        lg_d = self.din("lnx_g", [1, RW])
        lb_d = self.din("lnx_b", [1, RW])
        ww2_d = self.din("w_w2", [64, RW])
        wa2_d = self.din("w_a2", [64, RW])
        wg2_d = self.din("w_g2", [160, RW])
        msu_d = self.din("m_su", [64, 512])
        msl_d = self.din("m_sl", [64, 512])
        mu8_d = self.din("m_u", [64, 512])
        i8_d = self.din("i8", [64, 512])
        ones_d = self.cin("ones", [128, 128])
        wkv_out = self.dout("wkv_out", [4, RH, RD, RD])
        rwoT_d = self.dscr("rwoT_d", [8, 128, TT])
        if "dbgrw" in (self.phases or ()):
            rwoT_d = self.dout("dbg_rwoT", [8, 128, TT])
        self.rwoT_d = rwoT_d
        rwT_d = self.rwT_d
        seqs = [(0, L, None), (L, L, None), (2 * L, LS, 0), (2 * L + LS, LS, 1)]
        with ExitStack() as es:
            def sb(name, shape, dt=F32):
                return es.enter_context(nc.sbuf_tensor("r_" + name, shape, dt))
            ident = sb("ident", [128, 128])
            ones = sb("ones", [64, 64])
            msu = sb("msu", [64, 8, 64]); msl = sb("msl", [64, 8, 64]); mu8 = sb("mu8", [64, 8, 64]); i8 = sb("i8", [64, 8, 64])
            mu = sb("mu", [64, 53]); w0 = sb("w0", [64, 16]); a0 = sb("a0", [64, 16]); k_k = sb("k_k", [64, 16])
            k_a = sb("k_a", [64, 16]); r_k = sb("r_k", [64, 16])
            lgb = sb("lgb", [64, RW]); lbb = sb("lbb", [64, RW])
            ww2 = sb("ww2", [64, RW]); wa2 = sb("wa2", [64, RW]); wg2 = sb("wg2", [64, 3, RW])
            ST = sb("ST", [64, 16, 64]); S0 = sb("S0", [64, 16, 64])
            Xs = [sb(f"X{i}", [64, 53, C + 1]) for i in range(2)]; xm = sb("xm", [64, 53, C])
            txws = [sb(f"txw{i}", [64, C]) for i in range(2)]; sxgs = [sb(f"sxg{i}", [64, 3, C]) for i in range(2)]
            names = ["lw", "a", "kkn", "krep", "Lc", "Ep", "rt", "En", "bh", "bg", "kg", "rkp", "tmp",
                     "Vtok", "bgT", "kgT", "N", "NT", "AK", "QK", "QB", "P0", "P1", "PT0", "PT1",
                     "X0", "X1"]
            alias = {"yc": "P0", "sq": "P1", "rwtok": "NT", "W0": "PT1", "U": "N"}
            tls, tts, st8s, rwoTs, t_st8s, t_rwoTs = [], [], [], [], [], []
            HG = 8
            NG = 16 // HG
            for g_ in range(NG):
                tl_ = {n: sb(f"{n}_{g_}", [64, HG, 64]) for n in names}
                tt_ = {n: T(n) for n in names}
                for k_, v_ in alias.items():
                    tl_[k_] = tl_[v_]
                    tt_[k_] = tt_[v_]
                tls.append(tl_)
                tts.append(tt_)
                st8s.append(sb(f"st8_{g_}", [64, 32]))
                rwoTs.append(sb(f"rwoT_{g_}", [128, HG // 2, 64]))
                t_st8s.append(T())
                t_rwoTs.append(T())
            ps, t_ps = self.ps, self.t_ps
            t_c = T("rconsts")
            s.dma("sp", ident[:], self.ident_d[:, :], writes=[t_c])
            s.dma("sp", ones[:], ones_d[0:64, 0:64], writes=[t_c])
            for tile_, d_ in ((msu, msu_d), (msl, msl_d), (mu8, mu8_d), (i8, i8_d)):
                s.dma("sp", tile_[:, :, :].rearrange("p a b -> p (a b)"), d_[:, :], writes=[t_c])
            s.dma("sp", mu[:, 0:52], mu_d[0, 0:52 * 64].rearrange("(j p) -> p j", p=64), writes=[t_c],
                  allow_slow_non_contiguous=True)
            s.op("dve", lambda e: e.memset(mu[:, 52:53], 0.0), writes=[t_c])
            s.dma("sp", mu[0:32, 52:53], mu_d[0, 52 * 64:RSW].rearrange("(p o) -> p o", o=1), writes=[t_c],
                  allow_slow_non_contiguous=True)
            for tile_, d_ in ((w0, w0_d), (a0, a0_d), (k_k, kk_d), (k_a, ka_d), (r_k, rk_d)):
                s.dma("sp", tile_[:, :], d_[0, :].rearrange("(j p) -> p j", p=64), writes=[t_c],
                      allow_slow_non_contiguous=True)
            s.dma("sp", lgb[:, :], lg_d[0:1, :].partition_broadcast(64), writes=[t_c])
            s.dma("sp", lbb[:, :], lb_d[0:1, :].partition_broadcast(64), writes=[t_c])
            s.dma("sp", ww2[:, :], ww2_d[:, :], writes=[t_c])
            s.dma("sp", wa2[:, :], wa2_d[:, :], writes=[t_c])
            s.dma("sp", wg2[:, 0, :], wg2_d[0:64, :], writes=[t_c])
            s.dma("sp", wg2[:, 1, :], wg2_d[64:128, :], writes=[t_c])
            s.dma("sp", wg2[0:32, 2, :], wg2_d[128:160, :], writes=[t_c])
            t_S0, t_xm = T(), T()
            t_Xs = [T(), T()]; t_Xps = [T(), T()]
            t_txws = [T(), T()]; t_sxgs = [T(), T()]
            t_STs = [T() for _ in range(NG)]
            self.rps = 0

            psv = []
            for i_ in range(8):
                psv.append((ps[i_], t_ps[i_]))
            psh = []
            for i_ in range(8):
                for j_ in range(2):
                    psh.append((ps[i_][:, j_ * 256:(j_ + 1) * 256], T()))
            self.rph = 0

            def nps():
                i = self.rps
                self.rps = (i + 1) % 8
                return psv[i]

            def nph():
                if "halfbank" not in (self.phases or ()):
                    return nps()
                i = self.rph
                self.rph = (i + 1) % 16
                return psh[i]

            def v3g(p_):
                return p_[0:64, 0:HG * 64].rearrange("p (a b) -> p a b", b=64)

            def v3(p_):
                return p_[0:64, :].rearrange("p (a b) -> p a b", b=64)

            def bc(ap2, n):
                return ap2.unsqueeze(2).to_broadcast([64, ap2.shape[1], n])

            def dve(fn, reads, writes):
                s.op("dve", fn, reads, writes)

            def act(fn, reads, writes):
                s.op("act", fn, reads, writes)

            def mm8(lhs, t_l, rhs, t_r, lslice=None):
                p_, t_p = nps()
                for hh in range(8):
                    s.op("pe", lambda e: e.matmul(p_[0:64, hh * 64:(hh + 1) * 64], lhsT=lhs(hh), rhs=rhs(hh),
                                                  start=True, stop=True),
                         reads=t_l + t_r, writes=[t_p])
                return p_, t_p

            def mm8h(lhs, t_l, rhs, t_r):
                p_, t_p = nph()
                for hh in range(HG):
                    s.op("pe", lambda e: e.matmul(p_[0:64, hh * 64:(hh + 1) * 64], lhsT=lhs(hh), rhs=rhs(hh),
                                                  start=True, stop=True),
                         reads=t_l + t_r, writes=[t_p])
                return p_, t_p

            for si, (tok0, Lq, ci) in enumerate(seqs):
                if ci is None:
                    dve(lambda e: e.memset(ST[:, :, :], 0.0), [], t_STs)
                else:
                    s.dma("sp", S0[:, :, :], state_wkv[ci].rearrange("h v k -> v h k"), writes=[t_S0])
                    for g in range(2):
                        p_, t_p = nps()
                        for hh in range(8):
                            s.op("pe", lambda e: e.matmul(p_[0:64, hh * 64:(hh + 1) * 64], lhsT=S0[:, g * 8 + hh, :],
                                                          rhs=ident[0:64, 0:64], start=True, stop=True),
                                 reads=[t_S0, t_c], writes=[t_p])
                        dve(lambda e: e.tensor_copy(out=ST[:, g * 8:g * 8 + 8, :], in_=v3(p_)), [t_p], t_STs)
                def load_x(c):
                    t0 = tok0 + c * C
                    X, t_X = Xs[c % 2], t_Xs[c % 2]
                    for j0 in range(0, 53, 14):
                        j1 = min(53, j0 + 14)
                        if j1 == 53:
                            s.dma("sp", X[:, j0:52, 1:C + 1],
                                  rwT_d[j0:52, :, t0:t0 + C].rearrange("j p t -> p j t"), writes=[t_X])
                            s.dma("sp", X[0:32, 52, 1:C + 1], rwT_d[52, 0:32, t0:t0 + C], writes=[t_X])
                        else:
                            s.dma("sp", X[:, j0:j1, 1:C + 1],
                                  rwT_d[j0:j1, :, t0:t0 + C].rearrange("j p t -> p j t"), writes=[t_X])

                def prologue(c):
                    t0 = tok0 + c * C
                    txw, sxg, t_txw, t_sxg = txws[c % 2], sxgs[c % 2], t_txws[c % 2], t_sxgs[c % 2]
                    X, t_X, t_Xp = Xs[c % 2], t_Xs[c % 2], t_Xps[c % 2]
                    Xo, t_Xo = Xs[(c + 1) % 2], t_Xs[(c + 1) % 2]
                    if c == 0:
                        load_x(0)
                    if c == 0:
                        if ci is None:
                            dve(lambda e: e.memset(X[:, :, 0:1], 0.0), [], [t_Xp])
                        else:
                            dve(lambda e: e.memset(X[:, :, 0:1], 0.0), [], [t_Xp])
                            for j0 in range(0, 52, 13):
                                s.dma("sp", X[:, j0:j0 + 13, 0],
                                      state_shift[ci, j0 * 64:(j0 + 13) * 64].rearrange("(j p) -> p j", p=64),
                                      writes=[t_Xp], allow_slow_non_contiguous=True)
                            s.dma("sp", X[0:32, 52, 0:1], state_shift[ci, 52 * 64:RSW].rearrange("(p o) -> p o", o=1),
                                  writes=[t_Xp], allow_slow_non_contiguous=True)
                    else:
                        dve(lambda e: e.tensor_copy(out=X[:, :, 0:1], in_=Xo[:, :, C:C + 1]), [t_Xo], [t_Xp])
                    if c + 1 < Lq // C:
                        load_x(c + 1)
                    if c == 0 and si == 0:
                        pass
                    dve(lambda e: e.tensor_tensor(out=xm[:, :, :], in0=X[:, :, 0:C], in1=X[:, :, 1:C + 1],
                                                  op=ALU.subtract), [t_X, t_Xp], [t_xm])
                    dve(lambda e: e.tensor_tensor(out=xm[:, :, :], in0=xm[:, :, :],
                                                  in1=mu[:, :].unsqueeze(2).to_broadcast([64, 53, C]), op=ALU.mult),
                        [t_xm, t_c], [t_xm])
                    dve(lambda e: e.tensor_tensor(out=xm[:, :, :], in0=xm[:, :, :], in1=X[:, :, 1:C + 1],
                                                  op=ALU.add), [t_xm, t_X], [t_xm])
                    act(lambda e: e.activation(out=txw[:, :], in_=xm[:, 48, :], func=AF.Tanh), [t_xm], [t_txw])
                    act(lambda e: e.activation(out=sxg[:, :, :], in_=xm[:, 50:53, :], func=AF.Sigmoid),
                        [t_xm], [t_sxg])

                def body(c, g):
                    t0 = tok0 + c * C
                    txw, sxg, t_txw, t_sxg = txws[c % 2], sxgs[c % 2], t_txws[c % 2], t_sxgs[c % 2]
                    h0 = g * HG
                    A, tt, st8, rwoT = tls[g], tts[g], st8s[g], rwoTs[g]
                    t_ST, t_st8, t_rwoT = t_STs[g], t_st8s[g], t_rwoTs[g]
                    r_ = xm[:, h0:h0 + HG, :]
                    k_ = xm[:, 16 + h0:16 + h0 + HG, :]
                    v_ = xm[:, 32 + h0:32 + h0 + HG, :]
                    yield
                    p_, t_p = mm8h(lambda hh: ww2[:, (h0 + hh) * 64:(h0 + hh + 1) * 64], [t_c],
                                  lambda hh: txw[:, :], [t_txw])
                    for hh in range(HG):
                        act(lambda e: e.activation(out=A["lw"][:, hh, :], in_=p_[0:64, hh * 64:(hh + 1) * 64],
                                                   func=AF.Sigmoid, bias=w0[:, h0 + hh:h0 + hh + 1]),
                            [t_p, t_c], [tt["lw"]])
                    yield
                    p_, t_p = mm8h(lambda hh: wa2[:, (h0 + hh) * 64:(h0 + hh + 1) * 64], [t_c],
                                  lambda hh: xm[:, 49, :], [t_xm])
                    for hh in range(HG):
                        act(lambda e: e.activation(out=A["a"][:, hh, :], in_=p_[0:64, hh * 64:(hh + 1) * 64],
                                                   func=AF.Sigmoid, bias=a0[:, h0 + hh:h0 + hh + 1]),
                            [t_p, t_c], [tt["a"]])
                    dve(lambda e: e.tensor_tensor(out=A["kkn"][:, :, :], in0=k_, in1=bc(k_k[:, h0:h0 + HG], C),
                                                  op=ALU.mult), [t_xm, t_c], [tt["kkn"]])
                    dve(lambda e: e.tensor_tensor(out=A["tmp"][:, :, :], in0=A["kkn"][:, :, :],
                                                  in1=A["kkn"][:, :, :], op=ALU.mult), [tt["kkn"]], [tt["tmp"]])
                    yield
                    p_, t_p = nph()
                    s.op("pe", lambda e: e.matmul(p_[0:64, 0:HG * 64], lhsT=ones[:, :],
                                                  rhs=A["tmp"][:, :, :].rearrange("p a b -> p (a b)"),
                                                  start=True, stop=True), [t_c, tt["tmp"]], [t_p])
                    act(lambda e: e.activation(out=A["tmp"][:, :, :], in_=v3g(p_), func=AF.Ln, bias=1e-24),
                        [t_p], [tt["tmp"]])
                    act(lambda e: e.activation(out=A["tmp"][:, :, :], in_=A["tmp"][:, :, :], func=AF.Exp, scale=-0.5),
                        [tt["tmp"]], [tt["tmp"]])
                    dve(lambda e: e.tensor_tensor(out=A["kkn"][:, :, :], in0=A["kkn"][:, :, :],
                                                  in1=A["tmp"][:, :, :], op=ALU.mult),
                        [tt["kkn"], tt["tmp"]], [tt["kkn"]])
                    dve(lambda e: e.scalar_tensor_tensor(out=A["tmp"][:, :, :], in0=A["a"][:, :, :], scalar=-1.0,
                                                         in1=bc(k_a[:, h0:h0 + HG], C), op0=ALU.add,
                                                         op1=ALU.mult), [tt["a"], t_c], [tt["tmp"]])
                    dve(lambda e: e.scalar_tensor_tensor(out=A["krep"][:, :, :], in0=A["tmp"][:, :, :], scalar=1.0,
                                                         in1=k_, op0=ALU.add, op1=ALU.mult),
                        [tt["tmp"], t_xm], [tt["krep"]])
                    for hh in range(HG):
                        dve(lambda e: e.tensor_tensor_scan(out=A["Lc"][:, hh, :], data0=ones[:, 0:C],
                                                           data1=A["lw"][:, hh, :], initial=0.0,
                                                           op0=ALU.mult, op1=ALU.add),
                            [tt["lw"], t_c], [tt["Lc"]])
                    act(lambda e: e.activation(out=A["Ep"][:, :, :], in_=A["Lc"][:, :, :], func=AF.Exp,
                                               scale=-DECAY_SCALE), [tt["Lc"]], [tt["Ep"]])
                    act(lambda e: e.activation(out=A["En"][:, :, :], in_=A["Lc"][:, :, :], func=AF.Exp,
                                               scale=DECAY_SCALE), [tt["Lc"]], [tt["En"]])
                    dve(lambda e: e.tensor_tensor(out=A["rt"][:, :, :], in0=r_, in1=A["Ep"][:, :, :], op=ALU.mult),
                        [t_xm, tt["Ep"]], [tt["rt"]])
                    dve(lambda e: e.tensor_tensor(out=A["Lc"][:, :, :], in0=A["Lc"][:, :, :], in1=A["lw"][:, :, :],
                                                  op=ALU.subtract), [tt["Lc"], tt["lw"]], [tt["Lc"]])
                    act(lambda e: e.activation(out=A["Lc"][:, :, :], in_=A["Lc"][:, :, :], func=AF.Exp,
                                               scale=-DECAY_SCALE), [tt["Lc"]], [tt["Lc"]])
                    dve(lambda e: e.scalar_tensor_tensor(out=A["Lc"][:, :, :], in0=A["kkn"][:, :, :], scalar=-1.0,
                                                         in1=A["Lc"][:, :, :], op0=ALU.mult, op1=ALU.mult),
                        [tt["kkn"], tt["Lc"]], [tt["Lc"]])
                    at, t_at = A["Lc"], tt["Lc"]
                    dve(lambda e: e.tensor_tensor(out=A["bh"][:, :, :], in0=A["a"][:, :, :], in1=A["kkn"][:, :, :],
                                                  op=ALU.mult), [tt["a"], tt["kkn"]], [tt["bh"]])
                    dve(lambda e: e.tensor_tensor(out=A["bh"][:, :, :], in0=A["bh"][:, :, :], in1=A["En"][:, :, :],
                                                  op=ALU.mult), [tt["bh"], tt["En"]], [tt["bh"]])
                    dve(lambda e: e.tensor_tensor(out=A["En"][:, :, :], in0=A["krep"][:, :, :],
                                                  in1=A["En"][:, :, :], op=ALU.mult),
                        [tt["krep"], tt["En"]], [tt["En"]])
                    kh, t_kh = A["En"], tt["En"]
                    gC = A["Ep"][:, :, C - 1]
                    dve(lambda e: e.tensor_tensor(out=A["bg"][:, :, :], in0=A["bh"][:, :, :], in1=bc(gC, C),
                                                  op=ALU.mult), [tt["bh"], tt["Ep"]], [tt["bg"]])
                    dve(lambda e: e.tensor_tensor(out=A["kg"][:, :, :], in0=kh[:, :, :], in1=bc(gC, C),
                                                  op=ALU.mult), [t_kh, tt["Ep"]], [tt["kg"]])
                    dve(lambda e: e.tensor_tensor(out=A["rkp"][:, :, :], in0=r_, in1=A["krep"][:, :, :],
                                                  op=ALU.mult), [t_xm, tt["krep"]], [tt["rkp"]])
                    dve(lambda e: e.tensor_tensor(out=A["rkp"][:, :, :], in0=A["rkp"][:, :, :],
                                                  in1=bc(r_k[:, h0:h0 + HG], C), op=ALU.mult),
                        [tt["rkp"], t_c], [tt["rkp"]])
                    for src, t_src, dst in ((lambda hh: v_[:, hh, :], t_xm, "Vtok"),
                                            (lambda hh: A["bg"][:, hh, :], tt["bg"], "bgT"),
                                            (lambda hh: A["kg"][:, hh, :], tt["kg"], "kgT")):
                        yield
                        p_, t_p = mm8h(src, [t_src], lambda hh: ident[0:64, 0:64], [t_c])
                        act(lambda e: e.activation(out=A[dst][:, :, :], in_=v3g(p_), func=AF.Copy),
                            [t_p], [tt[dst]])
                    yield "PREP_DONE"
                    for lh, t_lh, rh, t_rh, msk, dst in (
                            (A["bh"], tt["bh"], at, t_at, msu, "N"),
                            (at, t_at, A["bh"], tt["bh"], msl, "NT"),
                            (kh, t_kh, at, t_at, msu, "AK"),
                            (kh, t_kh, A["rt"], tt["rt"], mu8, "QK"),
                            (A["bh"], tt["bh"], A["rt"], tt["rt"], mu8, "QB")):
                        yield
                        p_, t_p = mm8h(lambda hh: lh[:, hh, :], [t_lh], lambda hh: rh[:, hh, :], [t_rh])
                        dve(lambda e: e.tensor_tensor(out=A[dst][:, :, :], in0=v3g(p_), in1=msk[:, 0:HG, :],
                                                      op=ALU.mult), [t_p, t_c], [tt[dst]])
                    dve(lambda e: e.tensor_tensor(out=A["X0"][:, :, :], in0=A["N"][:, :, :], in1=i8[:, 0:HG, :],
                                                  op=ALU.add), [tt["N"], t_c], [tt["X0"]])
                    Pn, PTn, Xn = "N", "NT", "X0"
                    for lvl in range(1, 6):
                        Pd, PTd, Xd = ("P0", "PT0", "X1") if lvl % 2 else ("P1", "PT1", "X0")
                        if lvl < 5:
                            yield
                            p_, t_p = mm8h(lambda hh: A[PTn][:, hh, :], [tt[PTn]],
                                          lambda hh: A[Pn][:, hh, :], [tt[Pn]])
                            act(lambda e: e.activation(out=A[Pd][:, :, :], in_=v3g(p_), func=AF.Copy),
                                [t_p], [tt[Pd]])
                        yield
                        p_, t_p = mm8h(lambda hh: A[Pn][:, hh, :], [tt[Pn]],
                                      lambda hh: A[PTn][:, hh, :], [tt[PTn]])
                        act(lambda e: e.activation(out=A[PTd][:, :, :], in_=v3g(p_), func=AF.Copy), [t_p], [tt[PTd]])
                        yield
                        p_, t_p = mm8h(lambda hh: A[PTd][:, hh, :], [tt[PTd]],
                                      lambda hh: A[Xn][:, hh, :], [tt[Xn]])
                        dve(lambda e: e.tensor_tensor(out=A[Xd][:, :, :], in0=v3g(p_), in1=A[Xn][:, :, :],
                                                      op=ALU.add), [t_p, tt[Xn]], [tt[Xd]])
                        Pn, PTn, Xn = Pd, PTd, Xd
                    yield
                    p_, t_p = nph()
                    for hh in range(HG):
                        o_ = p_[0:64, hh * 64:(hh + 1) * 64]
                        s.op("pe", lambda e: e.matmul(o_, lhsT=at[:, hh, :], rhs=ST[:, h0 + hh, :],
                                                      start=True, stop=False), [t_at, t_ST], [t_p])
                        s.op("pe", lambda e: e.matmul(o_, lhsT=A["AK"][:, hh, :], rhs=A["Vtok"][:, hh, :],
                                                      start=False, stop=True), [tt["AK"], tt["Vtok"]], [t_p])
                    act(lambda e: e.activation(out=A["W0"][:, :, :], in_=v3g(p_), func=AF.Copy), [t_p], [tt["W0"]])
                    yield
                    p_, t_p = mm8h(lambda hh: A[Xn][:, hh, :], [tt[Xn]], lambda hh: A["W0"][:, hh, :], [tt["W0"]])
                    act(lambda e: e.activation(out=A["U"][:, :, :], in_=v3g(p_), func=AF.Copy), [t_p], [tt["U"]])
                    yield
                    pY, t_pY = nph()
                    for hh in range(HG):
                        o_ = pY[0:64, hh * 64:(hh + 1) * 64]
                        s.op("pe", lambda e: e.matmul(o_, lhsT=A["rt"][:, hh, :], rhs=ST[:, h0 + hh, :],
                                                      start=True, stop=False), [tt["rt"], t_ST], [t_pY])
                        s.op("pe", lambda e: e.matmul(o_, lhsT=A["QK"][:, hh, :], rhs=A["Vtok"][:, hh, :],
                                                      start=False, stop=False), [tt["QK"], tt["Vtok"]], [t_pY])
                        s.op("pe", lambda e: e.matmul(o_, lhsT=A["QB"][:, hh, :], rhs=A["U"][:, hh, :],
                                                      start=False, stop=True), [tt["QB"], tt["U"]], [t_pY])
                    yield
                    pS, t_pS = nph()
                    for hh in range(HG):
                        o_ = pS[0:64, hh * 64:(hh + 1) * 64]
                        s.op("pe", lambda e: e.matmul(o_, lhsT=A["bgT"][:, hh, :], rhs=A["U"][:, hh, :],
                                                      start=True, stop=False), [tt["bgT"], tt["U"]], [t_pS])
                        s.op("pe", lambda e: e.matmul(o_, lhsT=A["kgT"][:, hh, :], rhs=A["Vtok"][:, hh, :],
                                                      start=False, stop=True), [tt["kgT"], tt["Vtok"]], [t_pS])
                    dve(lambda e: e.tensor_tensor(out=ST[:, h0:h0 + HG, :], in0=ST[:, h0:h0 + HG, :], in1=bc(gC, 64),
                                                  op=ALU.mult), [t_ST, tt["Ep"]], [t_ST])
                    dve(lambda e: e.tensor_tensor(out=ST[:, h0:h0 + HG, :], in0=ST[:, h0:h0 + HG, :], in1=v3g(pS),
                                                  op=ALU.add), [t_ST, t_pS], [t_ST])
                    dve(lambda e: e.tensor_reduce(out=st8[:, 0:HG], in_=v3g(pY), axis=AX.X, op=ALU.add),
                        [t_pY], [t_st8])
                    dve(lambda e: e.tensor_scalar(out=st8[:, 0:HG], in0=st8[:, 0:HG], scalar1=1.0 / 64,
                                                  scalar2=None, op0=ALU.mult), [t_st8], [t_st8])
                    dve(lambda e: e.tensor_tensor(out=A["yc"][:, :, :], in0=v3g(pY), in1=bc(st8[:, 0:HG], 64),
                                                  op=ALU.subtract), [t_pY, t_st8], [tt["yc"]])
                    dve(lambda e: e.tensor_tensor(out=A["sq"][:, :, :], in0=A["yc"][:, :, :], in1=A["yc"][:, :, :],
                                                  op=ALU.mult), [tt["yc"]], [tt["sq"]])
                    dve(lambda e: e.tensor_reduce(out=st8[:, 8:8 + HG], in_=A["sq"][:, :, :], axis=AX.X, op=ALU.add),
                        [tt["sq"]], [t_st8])
                    act(lambda e: e.activation(out=st8[:, 16:16 + HG], in_=st8[:, 8:8 + HG], func=AF.Sqrt, scale=1.0 / 64,
                                               bias=GN_EPS), [t_st8], [t_st8])
                    dve(lambda e: e.reciprocal(out=st8[:, 24:24 + HG], in_=st8[:, 16:16 + HG]), [t_st8], [t_st8])
                    dve(lambda e: e.tensor_tensor(out=A["yc"][:, :, :], in0=A["yc"][:, :, :],
                                                  in1=bc(st8[:, 24:24 + HG], 64), op=ALU.mult),
                        [tt["yc"], t_st8], [tt["yc"]])
                    lg3 = lgb[:, h0 * 64:(h0 + HG) * 64].rearrange("p (a b) -> p a b", b=64)
                    lb3 = lbb[:, h0 * 64:(h0 + HG) * 64].rearrange("p (a b) -> p a b", b=64)
                    dve(lambda e: e.tensor_tensor(out=A["yc"][:, :, :], in0=A["yc"][:, :, :], in1=lg3, op=ALU.mult),
                        [tt["yc"], t_c], [tt["yc"]])
                    dve(lambda e: e.tensor_tensor(out=A["yc"][:, :, :], in0=A["yc"][:, :, :], in1=lb3, op=ALU.add),
                        [tt["yc"], t_c], [tt["yc"]])
                    yield
                    pC, t_pC = nph()
                    for hh in range(HG):
                        s.op("pe", lambda e: e.matmul(pC[0:64, hh * 2:hh * 2 + 2], lhsT=A["rkp"][:, hh, :],
                                                      rhs=ones[:, 0:2], start=True, stop=True),
                             [tt["rkp"], t_c], [t_pC])
                    dve(lambda e: e.tensor_copy(out=st8[:, 0:HG], in_=pC[0:64, 0:2 * HG:2]), [t_pC], [t_st8])
                    dve(lambda e: e.tensor_tensor(out=A["sq"][:, :, :], in0=A["Vtok"][:, :, :],
                                                  in1=bc(st8[:, 0:HG], 64), op=ALU.mult),
                        [tt["Vtok"], t_st8], [tt["sq"]])
                    dve(lambda e: e.tensor_tensor(out=A["yc"][:, :, :], in0=A["yc"][:, :, :], in1=A["sq"][:, :, :],
                                                  op=ALU.add), [tt["yc"], tt["sq"]], [tt["yc"]])
                    yield
                    pG, t_pG = nph()
                    for part, kp in ((0, 64), (1, 64), (2, 32)):
                        s.op("pe", lambda e: e.matmul(pG[0:64, 0:HG * 64], lhsT=sxg[0:kp, part, :],
                                                      rhs=wg2[0:kp, part, h0 * 64:(h0 + HG) * 64],
                                                      start=(part == 0), stop=(part == 2)),
                             [t_sxg, t_c], [t_pG])
                    dve(lambda e: e.tensor_tensor(out=A["rwtok"][:, :, :], in0=v3g(pG), in1=A["yc"][:, :, :],
                                                  op=ALU.mult), [t_pG, tt["yc"]], [tt["rwtok"]])
                    yield
                    pT, t_pT = nph()
                    for jj in range(HG // 2):
                        s.op("pe", lambda e: e.matmul(pT[:, jj * 64:(jj + 1) * 64],
                                                      lhsT=A["rwtok"][:, 2 * jj:2 * jj + 2, :].rearrange(
                                                          "p a b -> p (a b)"),
                                                      rhs=ident[0:64, 0:64], start=True, stop=True),
                             [tt["rwtok"], t_c], [t_pT])
                    act(lambda e: e.activation(out=rwoT[:, :, :].rearrange("p a b -> p (a b)"), in_=pT[:, 0:(HG // 2) * 64],
                                               func=AF.Copy), [t_pT], [t_rwoT])
                    s.dma(STQ, rwoT_d[(HG // 2) * g:(HG // 2) * (g + 1), :, t0:t0 + C].rearrange("j p t -> p j t"),
                          rwoT[:, :, :], reads=[t_rwoT])
                order = [(c_, g_) for c_ in range(Lq // C) for g_ in range(NG)]
                oi = 0
                active = []
                running = set()

                def try_start():
                    nonlocal_oi = state["oi"]
                    if nonlocal_oi >= len(order):
                        return False
                    c_, g_ = order[nonlocal_oi]
                    if g_ in running:
                        return False
                    if active and not active[-1][2]:
                        return False
                    if len(active) >= 2:
                        return False
                    if g_ == 0:
                        prologue(c_)
                    active.append([body(c_, g_), g_, False])
                    running.add(g_)
                    state["oi"] = nonlocal_oi + 1
                    return True

                state = {"oi": 0}
                try_start()
                while active:
                    for ent in list(active):
                        try:
                            v_ = next(ent[0])
                            if v_ == "PREP_DONE":
                                ent[2] = True
                        except StopIteration:
                            active.remove(ent)
                            running.discard(ent[1])
                        try_start()
                    if not active:
                        try_start()
                for g in range(2):
                    p_, t_p = nps()
                    for hh in range(8):
                        s.op("pe", lambda e: e.matmul(p_[0:64, hh * 64:(hh + 1) * 64], lhsT=ST[:, g * 8 + hh, :],
                                                      rhs=ident[0:64, 0:64], start=True, stop=True),
                             t_STs + [t_c], [t_p])
                    dve(lambda e: e.tensor_copy(out=S0[:, g * 8:g * 8 + 8, :], in_=v3(p_)), [t_p], [t_S0])
                s.dma(STQ, wkv_out[si].rearrange("h v k -> v h k"), S0[:, :, :], reads=[t_S0])
            s.barrier()


    CAPB = 4
    CAPR = 512
    NSLOT = NEXP * 512
    BIG = 1.0e6

    def breg(self):
        if not hasattr(self, "_breg"):
            self._breg = self.nc.gpsimd.to_reg(self.NSLOT - 1)
        return self._breg

    def groups(self):
        L, LS = self.L, self.LS
        gs = []
        for sq in range(2):
            for g0 in range(0, L, 512):
                gs.append((sq * L + g0, min(512, L - g0)))
        gs.append((2 * L, 2 * LS))
        return gs

    def phase_merge(self):
        nc, s = self.nc, self.s
        L, LS, PAST, TT = self.L, self.LS, self.PAST, self.TT
        wfu_d = self.din("w_fox_up", [FW, D])
        wru_d = self.din("w_rwkv_up", [RW, D])
        wo_d = self.din("w_o", [D, D])
        gffn_d = self.din("g_ffn", [1, D])
        wrg_d = self.din("w_rg", [D, 4])
        brg_d = self.din("b_rg", [1, 4])
        wre_d = self.din("w_re", [D, NEXP])
        bre_d = self.din("b_re", [1, NEXP])
        slt_d = self.din("slt", [128, 128])
        ebase_d = self.din("ebase", [128, NEXP])
        ones_d = self.cin("ones", [128, 128])
        x2_d = self.x2_d = self.dscr("x2_d", [TT, D])
        xs_d = self.xs_d = self.dscr("xs_d", [self.NSLOT, D])
        if "dbgmerge" in (self.phases or ()):
            x2_d = self.x2_d = self.dout("dbg_x2", [TT, D])
            self.dbg_gates = self.dout("dbg_gates", [128, TT // 128, 2])
            self.dbg_dest = self.dout("dbg_dest", [128, TT // 128, 2], I32)
            xs_d = self.xs_d = self.dout("dbg_xs", [self.NSLOT, D])
        NTt = TT // 128
        self.gates_all = nc.alloc_sbuf_tensor("gates_all", [128, NTt, 2], F32)
        self.dest_all = nc.alloc_sbuf_tensor("dest_all", [128, NTt, 2], I32)
        self.t_route = T("route")
        gates_all, dest_all, t_route = self.gates_all, self.dest_all, self.t_route
        with ExitStack() as es:
            def sb(name, shape, dt=F32):
                return es.enter_context(nc.sbuf_tensor("m_" + name, shape, dt))
            ident = sb("ident", [128, 128]); slt = sb("slt", [128, 128]); onesf = sb("onesf", [128, 128])
            ebase = sb("ebase", [128, NEXP]); gfbc = sb("gfbc", [128, D]); brbc = sb("brbc", [128, 36])
            wr = sb("wr", [128, 16, 36]); cnt = sb("cnt", [128, NEXP])
            foxT = sb("foxT", [128, 8, 512], F32R); rwoT = sb("rwoT", [128, 8, 512], F32R)
            wfu = [sb(f"wfu{i}", [128, 8, 128], F32R) for i in range(2)]
            wru = [sb(f"wru{i}", [128, 8, 128], F32R) for i in range(2)]
            sgf = [sb(f"sgf{i}", [128, 512]) for i in range(2)]
            sgr = [sb(f"sgr{i}", [128, 512]) for i in range(2)]
            tmpA = sb("tmpA", [128, 512]); tmpB = sb("tmpB", [128, 512])
            mT = sb("mT", [128, 16, 512], F32R)
            X2 = sb("X2", [128, 4, D])
            wo = [sb(f"wo{i}", [128, 16, 256], F32R) for i in range(2)]
            H2s = [sb(f"H2{i}", [128, D]) for i in range(2)]; h2T = sb("h2T", [128, 16, 128])
            junk = h2T[:, :, :].rearrange("p a b -> p (a b)")
            lg = sb("lg", [128, 36]); G = sb("G", [128, 4]); pen = sb("pen", [128, 4]); ge = sb("ge", [128, 4])
            ml = sb("ml", [128, NEXP]); ml2 = sb("ml2", [128, NEXP]); oh0 = sb("oh0", [128, NEXP])
            oh1 = sb("oh1", [128, NEXP]); oh = sb("oh", [128, NEXP]); pos = sb("pos", [128, NEXP])
            t32 = sb("t32", [128, NEXP]); m = sb("m", [128, 32])
            ps, t_ps = self.ps, self.t_ps
            t_c = T("mconsts")
            s.dma("sp", ident[:], self.ident_d[:, :], writes=[t_c])
            s.dma("sp", slt[:], slt_d[:, :], writes=[t_c])
            s.dma("sp", onesf[:], ones_d[:, :], writes=[t_c])
            s.dma("sp", ebase[:], ebase_d[:, :], writes=[t_c])
            s.dma("sp", gfbc[:], gffn_d[0:1, :].partition_broadcast(128), writes=[t_c])
            s.dma("sp", brbc[:, 0:4], brg_d[0:1, :].partition_broadcast(128), writes=[t_c])
            s.dma("sp", brbc[:, 4:36], bre_d[0:1, :].partition_broadcast(128), writes=[t_c])
            s.dma("sp", wr[:, :, 0:4], wrg_d[:, :].rearrange("(kc p) g -> p kc g", p=128), writes=[t_c])
            s.dma("sp", wr[:, :, 4:36], wre_d[:, :].rearrange("(kc p) g -> p kc g", p=128), writes=[t_c])
            t_cnt = T()
            s.op("dve", lambda e: e.memset(cnt[:, :], 0.0), writes=[t_cnt])
            t_foxT, t_rwoT, t_tmpA, t_tmpB, t_mT, t_X2, t_h2T, t_r = (T() for _ in range(8))
            t_H2s = [T(), T()]
            t_junk = t_h2T
            t_wfu = [T(), T()]; t_wru = [T(), T()]; t_sgf = [T(), T()]; t_sgr = [T(), T()]; t_wo = [T(), T()]
            self.mps = 0

            def nps():
                i = self.mps
                self.mps = (i + 1) % 8
                return ps[i], t_ps[i]
            wi = 0
            woi = 0
            for (tok0, ntok) in self.groups():
                ntile = ntok // 128
                s.dma("sp", foxT[:, :, 0:ntok], r32(self.foxT_d[:, :, tok0:tok0 + ntok]).rearrange("h p t -> p h t"),
                      writes=[t_foxT])
                s.dma("sp", rwoT[:, :, 0:ntok], r32(self.rwoT_d[:, :, tok0:tok0 + ntok]).rearrange("h p t -> p h t"),
                      writes=[t_rwoT])
                s.dma("sp", X2[:, 0:ntile, :], self.x_all[tok0:tok0 + ntok, :].rearrange("(a p) c -> p a c", p=128),
                      writes=[t_X2])
                for cb in range(16):
                    b = wi % 2
                    wi += 1
                    s.dma("sp", wfu[b][:, :, :], r32(wfu_d[:, cb * 128:(cb + 1) * 128]).rearrange(
                        "(kc p) c -> p kc c", p=128), writes=[t_wfu[b]])
                    s.dma("sp", wru[b][:, :, :], r32(wru_d[:, cb * 128:(cb + 1) * 128]).rearrange(
                        "(kc p) c -> p kc c", p=128), writes=[t_wru[b]])
                    s.dma("sp", sgf[b][:, 0:ntok], self.sgT_d[cb, :, tok0:tok0 + ntok], writes=[t_sgf[b]])
                    s.dma("sp", sgr[b][:, 0:ntok], self.sgT_d[16 + cb, :, tok0:tok0 + ntok], writes=[t_sgr[b]])
                    pF, t_pF = nps()
                    for kc in range(8):
                        s.op("pe", lambda e: e.matmul(pF[:, 0:ntok], lhsT=wfu[b][:, kc, :], rhs=foxT[:, kc, 0:ntok],
                                                      start=(kc == 0), stop=(kc == 7)),
                             [t_wfu[b], t_foxT], [t_pF])
                    pR, t_pR = nps()
                    for kc in range(8):
                        s.op("pe", lambda e: e.matmul(pR[:, 0:ntok], lhsT=wru[b][:, kc, :], rhs=rwoT[:, kc, 0:ntok],
                                                      start=(kc == 0), stop=(kc == 7)),
                             [t_wru[b], t_rwoT], [t_pR])
                    s.op("dve", lambda e: e.tensor_tensor(out=tmpA[:, 0:ntok], in0=pF[:, 0:ntok], in1=sgf[b][:, 0:ntok],
                                                          op=ALU.mult), [t_pF, t_sgf[b]], [t_tmpA])
                    s.op("dve", lambda e: e.tensor_tensor(out=tmpB[:, 0:ntok], in0=pR[:, 0:ntok], in1=sgr[b][:, 0:ntok],
                                                          op=ALU.mult), [t_pR, t_sgr[b]], [t_tmpB])
                    s.op("dve", lambda e: e.tensor_tensor(out=mT[:, cb, 0:ntok], in0=tmpA[:, 0:ntok],
                                                          in1=tmpB[:, 0:ntok], op=ALU.add),
                         [t_tmpA, t_tmpB], [t_mT])
                for cs in range(8):
                    b = woi % 2
                    woi += 1
                    s.dma("sp", wo[b][:, :, :], r32(wo_d[:, cs * 256:(cs + 1) * 256]).rearrange(
                        "(kc p) c -> p kc c", p=128), writes=[t_wo[b]])
                    for ti in range(ntile):
                        p_, t_p = nps()
                        for kc in range(16):
                            s.op("pe", lambda e: e.matmul(p_[:, 0:256], lhsT=mT[:, kc, ti * 128:(ti + 1) * 128],
                                                          rhs=wo[b][:, kc, :], start=(kc == 0), stop=(kc == 15)),
                                 [t_mT, t_wo[b]], [t_p])
                        s.op("dve", lambda e: e.tensor_tensor(out=X2[:, ti, cs * 256:(cs + 1) * 256], in0=p_[:, 0:256],
                                                              in1=X2[:, ti, cs * 256:(cs + 1) * 256], op=ALU.add),
                             [t_p, t_X2], [t_X2])
                for ti in range(ntile):
                    tti = (tok0 + ti * 128) // 128
                    r0 = tok0 + ti * 128
                    H2, t_H2 = H2s[tti % 2], t_H2s[tti % 2]
                    s.dma(STQ, x2_d[r0:r0 + 128, :], X2[:, ti, :], reads=[t_X2])
                    s.op("act", lambda e: e.activation(out=junk, in_=X2[:, ti, :], func=AF.Square,
                                                       accum_out=m[:, 16:17]), [t_X2], [t_junk, t_r])
                    s.op("act", lambda e: e.activation(out=m[:, 17:18], in_=m[:, 16:17], func=AF.Sqrt,
                                                       scale=1.0 / D, bias=NORM_EPS), [t_r], [t_r])
                    s.op("dve", lambda e: e.reciprocal(out=m[:, 18:19], in_=m[:, 17:18]), [t_r], [t_r])
                    s.op("dve", lambda e: e.scalar_tensor_tensor(out=H2[:, :], in0=X2[:, ti, :], scalar=m[:, 18:19],
                                                                 in1=gfbc[:, :], op0=ALU.mult, op1=ALU.mult),
                         [t_X2, t_r, t_c], [t_H2])
                    for k4 in range(4):
                        p_, t_p = nps()
                        for j in range(4):
                            kc = k4 * 4 + j
                            s.op("pe", lambda e: e.transpose(out=p_[:, j * 128:(j + 1) * 128],
                                                             in_=H2[:, kc * 128:(kc + 1) * 128], identity=ident[:, :]),
                                 [t_H2, t_c], [t_p])
                        s.op("act", lambda e: e.activation(out=h2T[:, k4 * 4:k4 * 4 + 4, :].rearrange("p a b -> p (a b)"),
                                                           in_=p_[:, :], func=AF.Copy), [t_p], [t_h2T])
                    pL, t_pL = nps()
                    for kc in range(16):
                        s.op("pe", lambda e: e.matmul(pL[:, 0:36], lhsT=h2T[:, kc, :], rhs=wr[:, kc, :],
                                                      start=(kc == 0), stop=(kc == 15)), [t_h2T, t_c], [t_pL])
                    dv = lambda fn, rd, wrt: s.op("dve", fn, rd, wrt)
                    dv(lambda e: e.tensor_tensor(out=lg[:, :], in0=pL[:, 0:36], in1=brbc[:, :], op=ALU.add),
                       [t_pL, t_c], [t_r])
                    dv(lambda e: e.tensor_reduce(out=m[:, 0:1], in_=lg[:, 0:4], axis=AX.X, op=ALU.max), [t_r], [t_r])
                    dv(lambda e: e.tensor_scalar(out=G[:, :], in0=lg[:, 0:4], scalar1=m[:, 0:1], scalar2=None,
                                                 op0=ALU.is_equal), [t_r], [t_r])
                    dv(lambda e: e.tensor_scalar(out=m[:, 1:2], in0=m[:, 0:1], scalar1=-1.0, scalar2=None,
                                                 op0=ALU.mult), [t_r], [t_r])
                    s.op("act", lambda e: e.activation(out=ge[:, :], in_=lg[:, 0:4], func=AF.Exp, bias=m[:, 1:2],
                                                       accum_out=m[:, 2:3]), [t_r], [t_r])
                    dv(lambda e: e.reciprocal(out=m[:, 3:4], in_=m[:, 2:3]), [t_r], [t_r])
                    dv(lambda e: e.tensor_scalar(out=pen[:, :], in0=G[:, :], scalar1=-1.0, scalar2=1.0e30,
                                                 op0=ALU.add, op1=ALU.mult), [t_r], [t_r])
                    dv(lambda e: e.tensor_tensor(out=ml[:, :].rearrange("p (a b) -> p a b", b=8),
                                                 in0=lg[:, 4:36].rearrange("p (a b) -> p a b", b=8),
                                                 in1=pen[:, :].unsqueeze(2).to_broadcast([128, 4, 8]), op=ALU.add),
                       [t_r], [t_r])
                    dv(lambda e: e.tensor_reduce(out=m[:, 4:5], in_=ml[:, :], axis=AX.X, op=ALU.max), [t_r], [t_r])
                    dv(lambda e: e.tensor_scalar(out=oh0[:, :], in0=ml[:, :], scalar1=m[:, 4:5], scalar2=None,
                                                 op0=ALU.is_equal), [t_r], [t_r])
                    dv(lambda e: e.scalar_tensor_tensor(out=ml2[:, :], in0=oh0[:, :], scalar=-1.0e30, in1=ml[:, :],
                                                        op0=ALU.mult, op1=ALU.add), [t_r], [t_r])
                    dv(lambda e: e.tensor_reduce(out=m[:, 5:6], in_=ml2[:, :], axis=AX.X, op=ALU.max), [t_r], [t_r])
                    dv(lambda e: e.tensor_scalar(out=oh1[:, :], in0=ml2[:, :], scalar1=m[:, 5:6], scalar2=None,
                                                 op0=ALU.is_equal), [t_r], [t_r])
                    dv(lambda e: e.tensor_tensor(out=m[:, 6:7], in0=m[:, 5:6], in1=m[:, 4:5], op=ALU.subtract),
                       [t_r], [t_r])
                    s.op("act", lambda e: e.activation(out=m[:, 7:8], in_=m[:, 6:7], func=AF.Exp), [t_r], [t_r])
                    dv(lambda e: e.tensor_scalar(out=m[:, 8:9], in0=m[:, 7:8], scalar1=1.0, scalar2=None,
                                                 op0=ALU.add), [t_r], [t_r])
                    dv(lambda e: e.reciprocal(out=m[:, 9:10], in_=m[:, 8:9]), [t_r], [t_r])
                    dv(lambda e: e.tensor_tensor(out=gates_all[:, tti, 0:1], in0=m[:, 3:4], in1=m[:, 9:10],
                                                 op=ALU.mult), [t_r], [t_route])
                    dv(lambda e: e.tensor_tensor(out=gates_all[:, tti, 1:2], in0=gates_all[:, tti, 0:1],
                                                 in1=m[:, 7:8], op=ALU.mult), [t_r, t_route], [t_route])
                    dv(lambda e: e.tensor_tensor(out=oh[:, :], in0=oh0[:, :], in1=oh1[:, :], op=ALU.add), [t_r], [t_r])
                    pP, t_pP = nps()
                    s.op("pe", lambda e: e.matmul(pP[:, 0:NEXP], lhsT=slt[:, :], rhs=oh[:, :], start=True, stop=True),
                         [t_c, t_r], [t_pP])
                    s.op("pe", lambda e: e.matmul(pP[:, 64:64 + NEXP], lhsT=onesf[:, :], rhs=oh[:, :],
                                                  start=True, stop=True), [t_c, t_r], [t_pP])
                    dv(lambda e: e.tensor_tensor(out=pos[:, :], in0=pP[:, 0:NEXP], in1=cnt[:, :], op=ALU.add),
                       [t_pP, t_cnt], [t_r])
                    dv(lambda e: e.tensor_tensor(out=cnt[:, :], in0=pP[:, 64:64 + NEXP], in1=cnt[:, :], op=ALU.add),
                       [t_pP, t_cnt, t_r], [t_cnt])
                    for k, ohk in ((0, oh0), (1, oh1)):
                        dv(lambda e: e.tensor_tensor(out=t32[:, :], in0=ohk[:, :], in1=pos[:, :], op=ALU.mult),
                           [t_r], [t_r])
                        dv(lambda e: e.tensor_reduce(out=m[:, 10 + k:11 + k], in_=t32[:, :], axis=AX.X, op=ALU.add),
                           [t_r], [t_r])
                        dv(lambda e: e.tensor_tensor(out=t32[:, :], in0=ohk[:, :], in1=ebase[:, :], op=ALU.mult),
                           [t_r, t_c], [t_r])
                        dv(lambda e: e.tensor_reduce(out=m[:, 12 + k:13 + k], in_=t32[:, :], axis=AX.X, op=ALU.add),
                           [t_r], [t_r])
                    dv(lambda e: e.tensor_scalar(out=m[:, 14:16], in0=m[:, 10:12], scalar1=float(self.CAPR),
                                                 scalar2=None, op0=ALU.is_lt), [t_r], [t_r])
                    dv(lambda e: e.tensor_tensor(out=m[:, 20:22], in0=m[:, 10:12], in1=m[:, 12:14], op=ALU.add),
                       [t_r], [t_r])
                    dv(lambda e: e.tensor_tensor(out=m[:, 20:22], in0=m[:, 20:22], in1=m[:, 14:16], op=ALU.mult),
                       [t_r], [t_r])
                    dv(lambda e: e.tensor_scalar(out=m[:, 22:24], in0=m[:, 14:16], scalar1=-1.0, scalar2=-self.BIG,
                                                 op0=ALU.add, op1=ALU.mult), [t_r], [t_r])
                    dv(lambda e: e.tensor_tensor(out=m[:, 20:22], in0=m[:, 20:22], in1=m[:, 22:24], op=ALU.add),
                       [t_r], [t_r])
                    dv(lambda e: e.tensor_copy(out=dest_all[:, tti, :], in_=m[:, 20:22]), [t_r], [t_route])
                    for k in range(2):
                        s.dma("pool", None, None, reads=[t_H2, t_route], writes=[],
                              fn=lambda e: e.indirect_dma_start(
                                  out=xs_d[:, :],
                                  out_offset=bass.IndirectOffsetOnAxis(ap=dest_all[:, tti, k:k + 1], axis=0),
                                  in_=H2[:, :], in_offset=None, bounds_check=self.breg(), oob_is_err=False))
            if "dbgmerge" in (self.phases or ()):
                s.dma("sp", self.dbg_gates[:, :, :], gates_all[:, :, :], reads=[t_route])
                s.dma("sp", self.dbg_dest[:, :, :], dest_all[:, :, :], reads=[t_route])
            s.barrier()

    def phase_experts(self):
        nc, s = self.nc, self.s
        w1_d = self.din("w1", [NEXP, D, DE])
        w3_d = self.din("w3", [NEXP, D, DE])
        w2_d = self.din("w2", [NEXP, DE, D])
        ys_d = self.ys_d = self.dscr("ys_d", [self.NSLOT, D])
        xs_d = self.xs_d
        CAPB, CAPR = self.CAPB, self.CAPR
        with ExitStack() as es:
            def sb(name, shape, dt=F32):
                return es.enter_context(nc.sbuf_tensor("e_" + name, shape, dt))
            ident = sb("ident", [128, 128])
            xr = [sb(f"xr{i}", [128, D]) for i in range(2)]
            xT = sb("xT", [128, 16, CAPR], F32R)
            w1p = [sb(f"w1p{i}", [128, 16, 128], F32R) for i in range(3)]
            w3p = [sb(f"w3p{i}", [128, 16, 128], F32R) for i in range(3)]
            w2p = [sb(f"w2p{i}", [128, 4, 512], F32R) for i in range(2)]
            sil = sb("sil", [128, CAPR])
            GT = sb("GT", [128, 4, CAPR], F32R)
            yst = [sb(f"yst{i}", [128, 512]) for i in range(2)]
            ps, t_ps = self.ps, self.t_ps
            t_c = T()
            s.dma("sp", ident[:], self.ident_d[:, :], writes=[t_c])
            t_xr = [T(), T()]; t_w1 = [T(), T(), T()]; t_w3 = [T(), T(), T()]; t_w2 = [T(), T()]; t_yst = [T(), T()]
            t_xT, t_sil, t_GT = T(), T(), T()
            self.eps = 0

            def nps():
                i = self.eps
                self.eps = (i + 1) % 8
                return ps[i], t_ps[i]
            xi = wi = w2i = yi = 0
            ev = 0
            for e_ in range(NEXP):
                base = e_ * CAPR
                for blk in range(CAPB):
                    b = xi % 2
                    xi += 1
                    s.dma("sp", xr[b][:, :], xs_d[base + blk * 128:base + (blk + 1) * 128, :], writes=[t_xr[b]])
                    for k4 in range(4):
                        p_, t_p = nps()
                        for j in range(4):
                            kc = k4 * 4 + j
                            s.op("pe", lambda e: e.transpose(out=p_[:, j * 128:(j + 1) * 128],
                                                             in_=xr[b][:, kc * 128:(kc + 1) * 128],
                                                             identity=ident[:, :]), [t_xr[b], t_c], [t_p])
                        ev += 1
                        o_ = xT[:, k4 * 4:k4 * 4 + 4, blk * 128:(blk + 1) * 128]
                        i_ = p_[:, :].rearrange("p (a b) -> p a b", b=128)
                        if ev % 2:
                            s.op("act", lambda e: e.activation(out=o_, in_=i_, func=AF.Copy), [t_p], [t_xT])
                        else:
                            s.op("dve", lambda e: e.tensor_copy(out=o_, in_=i_), [t_p], [t_xT])
                for fc in range(4):
                    b = wi % 3
                    wi += 1
                    s.dma("sp", w1p[b][:, :, :], r32(w1_d[e_, :, fc * 128:(fc + 1) * 128]).rearrange(
                        "(kc p) f -> p kc f", p=128), writes=[t_w1[b]])
                    s.dma("sp", w3p[b][:, :, :], r32(w3_d[e_, :, fc * 128:(fc + 1) * 128]).rearrange(
                        "(kc p) f -> p kc f", p=128), writes=[t_w3[b]])
                    p1, t_p1 = nps()
                    for kc in range(16):
                        s.op("pe", lambda e: e.matmul(p1[:, :], lhsT=w1p[b][:, kc, :], rhs=xT[:, kc, :],
                                                      start=(kc == 0), stop=(kc == 15)), [t_w1[b], t_xT], [t_p1])
                    p3, t_p3 = nps()
                    for kc in range(16):
                        s.op("pe", lambda e: e.matmul(p3[:, :], lhsT=w3p[b][:, kc, :], rhs=xT[:, kc, :],
                                                      start=(kc == 0), stop=(kc == 15)), [t_w3[b], t_xT], [t_p3])
                    s.op("act", lambda e: e.activation(out=sil[:, :], in_=p1[:, :], func=AF.Silu), [t_p1], [t_sil])
                    s.op("dve", lambda e: e.tensor_tensor(out=GT[:, fc, :], in0=p3[:, :], in1=sil[:, :], op=ALU.mult),
                         [t_p3, t_sil], [t_GT])
                for cs in range(4):
                    b = w2i % 2
                    w2i += 1
                    s.dma("sp", w2p[b][:, :, :], r32(w2_d[e_, :, cs * 512:(cs + 1) * 512]).rearrange(
                        "(fc p) c -> p fc c", p=128), writes=[t_w2[b]])
                    for blk in range(CAPB):
                        p_, t_p = nps()
                        for fc in range(4):
                            s.op("pe", lambda e: e.matmul(p_[:, :], lhsT=GT[:, fc, blk * 128:(blk + 1) * 128],
                                                          rhs=w2p[b][:, fc, :], start=(fc == 0), stop=(fc == 3)),
                                 [t_GT, t_w2[b]], [t_p])
                        yb = yi % 2
                        yi += 1
                        ev += 1
                        if ev % 2:
                            s.op("act", lambda e: e.activation(out=yst[yb][:, :], in_=p_[:, :], func=AF.Copy),
                                 [t_p], [t_yst[yb]])
                        else:
                            s.op("dve", lambda e: e.tensor_copy(out=yst[yb][:, :], in_=p_[:, :]), [t_p], [t_yst[yb]])
                        s.dma(STQ, ys_d[base + blk * 128:base + (blk + 1) * 128, cs * 512:(cs + 1) * 512],
                              yst[yb][:, :], reads=[t_yst[yb]])
            s.barrier()

    def phase_final(self):
        nc, s = self.nc, self.s
        L, LS, PAST, TT = self.L, self.LS, self.PAST, self.TT
        p_all = self.din("p_all", [TT, PLE])
        wple_d = self.din("w_ple", [PLE, D])
        wpg_d = self.din("w_pg", [D, D])
        bpg_d = self.din("b_pg", [1, D])
        gfin_d = self.din("g_final", [1, D])
        y_all = self.dout("y_all", [TT, D])
        gates_all, dest_all, t_route = self.gates_all, self.dest_all, self.t_route
        ys_d, x2_d = self.ys_d, self.x2_d
        with ExitStack() as es:
            def sb(name, shape, dt=F32):
                return es.enter_context(nc.sbuf_tensor("f_" + name, shape, dt))
            ident = sb("ident", [128, 128])
            bpbc = sb("bpbc", [128, D]); gfbc = sb("gfbc", [128, D])
            X3 = sb("X3", [128, 4, D])
            y0s = [sb(f"y0{i}", [128, D]) for i in range(2)]; y1s = [sb(f"y1{i}", [128, D]) for i in range(2)]
            x3T = sb("x3T", [128, 16, 512], F32R)
            pts = [sb(f"pt{i}", [128, PLE]) for i in range(2)]; pT = sb("pT", [128, 2, 512], F32R)
            wpg = [sb(f"wpg{i}", [128, 16, 256], F32R) for i in range(2)]
            wpl = [sb(f"wpl{i}", [128, 2, 256], F32R) for i in range(2)]
            gt = sb("gt", [128, 256]); junk = sb("junk", [128, D]); m = sb("m", [128, 8])
            ps, t_ps = self.ps, self.t_ps
            t_c = T()
            s.dma("sp", ident[:], self.ident_d[:, :], writes=[t_c])
            s.dma("sp", bpbc[:], bpg_d[0:1, :].partition_broadcast(128), writes=[t_c])
            s.dma("sp", gfbc[:], gfin_d[0:1, :].partition_broadcast(128), writes=[t_c])
            t_X3, t_x3T, t_pT, t_gt, t_junk, t_m = (T() for _ in range(6))
            t_y0s = [T(), T()]; t_y1s = [T(), T()]; t_pts = [T(), T()]
            t_wpg = [T(), T()]; t_wpl = [T(), T()]
            self.fps = 0

            def nps():
                i = self.fps
                self.fps = (i + 1) % 8
                return ps[i], t_ps[i]
            wi = 0
            for (tok0, ntok) in self.groups():
                ntile = ntok // 128
                for ti in range(ntile):
                    r0 = tok0 + ti * 128
                    tti = r0 // 128
                    y0, y1, pt = y0s[tti % 2], y1s[tti % 2], pts[tti % 2]
                    t_y0, t_y1, t_pt = t_y0s[tti % 2], t_y1s[tti % 2], t_pts[tti % 2]
                    s.dma("sp", X3[:, ti, :], x2_d[r0:r0 + 128, :], writes=[t_X3])
                    s.dma("sp", pt[:, :], p_all[r0:r0 + 128, :], writes=[t_pt])
                    s.op("dve", lambda e: e.memset(y0[:, :], 0.0), [], [t_y0])
                    s.op("dve", lambda e: e.memset(y1[:, :], 0.0), [], [t_y1])
                    for k, (yk, t_yk) in enumerate(((y0, t_y0), (y1, t_y1))):
                        s.dma("pool", None, None, reads=[t_route], writes=[t_yk],
                              fn=lambda e: e.indirect_dma_start(
                                  out=yk[:, :], out_offset=None, in_=ys_d[:, :],
                                  in_offset=bass.IndirectOffsetOnAxis(ap=dest_all[:, tti, k:k + 1], axis=0),
                                  bounds_check=self.breg(), oob_is_err=False))
                        s.op("dve", lambda e: e.scalar_tensor_tensor(out=X3[:, ti, :], in0=yk[:, :],
                                                                     scalar=gates_all[:, tti, k:k + 1],
                                                                     in1=X3[:, ti, :], op0=ALU.mult, op1=ALU.add),
                             [t_yk, t_route, t_X3], [t_X3])
                    for k4 in range(4):
                        p_, t_p = nps()
                        for j in range(4):
                            kc = k4 * 4 + j
                            s.op("pe", lambda e: e.transpose(out=p_[:, j * 128:(j + 1) * 128],
                                                             in_=X3[:, ti, kc * 128:(kc + 1) * 128],
                                                             identity=ident[:, :]), [t_X3, t_c], [t_p])
                        s.op("act", lambda e: e.activation(out=x3T[:, k4 * 4:k4 * 4 + 4, ti * 128:(ti + 1) * 128],
                                                           in_=p_[:, :].rearrange("p (a b) -> p a b", b=128),
                                                           func=AF.Copy), [t_p], [t_x3T])
                    p_, t_p = nps()
                    for j in range(2):
                        s.op("pe", lambda e: e.transpose(out=p_[:, j * 128:(j + 1) * 128],
                                                         in_=pt[:, j * 128:(j + 1) * 128], identity=ident[:, :]),
                             [t_pt, t_c], [t_p])
                    s.op("dve", lambda e: e.tensor_copy(out=pT[:, :, ti * 128:(ti + 1) * 128],
                                                        in_=p_[:, 0:256].rearrange("p (a b) -> p a b", b=128)),
                         [t_p], [t_pT])
                for cs in range(8):
                    b = wi % 2
                    wi += 1
                    s.dma("sp", wpg[b][:, :, :], r32(wpg_d[:, cs * 256:(cs + 1) * 256]).rearrange(
                        "(kc p) c -> p kc c", p=128), writes=[t_wpg[b]])
                    s.dma("sp", wpl[b][:, :, :], r32(wple_d[:, cs * 256:(cs + 1) * 256]).rearrange(
                        "(kc p) c -> p kc c", p=128), writes=[t_wpl[b]])
                    for ti in range(ntile):
                        p1, t_p1 = nps()
                        for kc in range(16):
                            s.op("pe", lambda e: e.matmul(p1[:, 0:256], lhsT=x3T[:, kc, ti * 128:(ti + 1) * 128],
                                                          rhs=wpg[b][:, kc, :], start=(kc == 0), stop=(kc == 15)),
                                 [t_x3T, t_wpg[b]], [t_p1])
                        p2, t_p2 = nps()
                        for kc in range(2):
                            s.op("pe", lambda e: e.matmul(p2[:, 0:256], lhsT=pT[:, kc, ti * 128:(ti + 1) * 128],
                                                          rhs=wpl[b][:, kc, :], start=(kc == 0), stop=(kc == 1)),
                                 [t_pT, t_wpl[b]], [t_p2])
                        s.op("dve", lambda e: e.tensor_tensor(out=gt[:, :], in0=p1[:, 0:256],
                                                              in1=bpbc[:, cs * 256:(cs + 1) * 256], op=ALU.add),
                             [t_p1, t_c], [t_gt])
                        s.op("act", lambda e: e.activation(out=gt[:, :], in_=gt[:, :], func=AF.Sigmoid),
                             [t_gt], [t_gt])
                        s.op("dve", lambda e: e.tensor_tensor(out=gt[:, :], in0=p2[:, 0:256], in1=gt[:, :],
                                                              op=ALU.mult), [t_p2, t_gt], [t_gt])
                        s.op("dve", lambda e: e.tensor_tensor(out=X3[:, ti, cs * 256:(cs + 1) * 256], in0=gt[:, :],
                                                              in1=X3[:, ti, cs * 256:(cs + 1) * 256], op=ALU.add),
                             [t_gt, t_X3], [t_X3])
                for ti in range(ntile):
                    r0 = tok0 + ti * 128
                    s.op("act", lambda e: e.activation(out=junk[:, :], in_=X3[:, ti, :], func=AF.Square,
                                                       accum_out=m[:, 0:1]), [t_X3], [t_junk, t_m])
                    s.op("act", lambda e: e.activation(out=m[:, 1:2], in_=m[:, 0:1], func=AF.Sqrt,
                                                       scale=1.0 / D, bias=NORM_EPS), [t_m], [t_m])
                    s.op("dve", lambda e: e.reciprocal(out=m[:, 2:3], in_=m[:, 1:2]), [t_m], [t_m])
                    s.op("dve", lambda e: e.scalar_tensor_tensor(out=junk[:, :], in0=X3[:, ti, :], scalar=m[:, 2:3],
                                                                 in1=gfbc[:, :], op0=ALU.mult, op1=ALU.mult),
                         [t_X3, t_m, t_c, t_junk], [t_junk])
                    s.dma(STQ, y_all[r0:r0 + 128, :], junk[:, :], reads=[t_junk])
            s.barrier()


_CACHE = {}


def _consts():
    return {"ident": np.eye(128, dtype=np.float32),
            "triu": np.triu(np.ones((128, 128), dtype=np.float32)),
            "ones": np.ones((128, 128), dtype=np.float32),
            "m_su": np.tile(np.triu(np.ones((64, 64), np.float32), 1), (1, 8)),
            "m_sl": np.tile(np.tril(np.ones((64, 64), np.float32), -1), (1, 8)),
            "m_u": np.tile(np.triu(np.ones((64, 64), np.float32), 0), (1, 8)),
            "i8": np.tile(np.eye(64, dtype=np.float32), (1, 8)),
            "slt": np.triu(np.ones((128, 128), dtype=np.float32), 1),
            "ebase": np.tile((np.arange(NEXP, dtype=np.float32) * 512.0)[None, :], (128, 1))}


L_FULL, LS_FULL, PAST_FULL = 2048, 64, 2048


def run(inputs, L, LS, PAST, ncores, phases=None):
    key = (L, LS, PAST, None if phases is None else tuple(sorted(phases)))
    if key not in _CACHE:
        _CACHE[key] = Prog(L, LS, PAST, phases=phases)
    p = _CACHE[key]
    f32 = lambda a: np.ascontiguousarray(np.asarray(a, dtype=np.float32))
    xp, xs = f32(inputs["x_prompt"]), f32(inputs["x_sample"])
    pp, psm = f32(inputs["p_prompt"][0]), f32(inputs["p_sample"][0])
    shared = dict(_consts())
    for k in ("w_in", "w_w2", "w_a2", "w_g2", "w_fox_up", "w_rwkv_up", "w_o", "w_rg", "w_re", "w1", "w3", "w2",
              "w_ple", "w_pg"):
        shared[k] = f32(inputs[k][0])
    for k in ("g_mix", "b_f", "mu_shift", "w0", "a0", "k_k", "k_a", "lnx_g", "lnx_b", "g_ffn", "b_rg", "b_re", "b_pg"):
        shared[k] = f32(inputs[k]).reshape(1, -1)
    shared["r_k"] = f32(inputs["r_k"]).reshape(1, -1)
    shared["g_final"] = f32(inputs["g_final"]).reshape(1, -1)
    in_maps = []
    for c in range(ncores):
        m = dict(shared)
        m["x_all"] = np.concatenate([xp[2 * c], xp[2 * c + 1], xs[2 * c], xs[2 * c + 1]], axis=0)
        m["p_all"] = np.concatenate([pp[2 * c], pp[2 * c + 1], psm[2 * c], psm[2 * c + 1]], axis=0)
        m["cache_k"] = f32(inputs["cache_k"][0, 2 * c:2 * c + 2])
        m["cache_v"] = f32(inputs["cache_v"][0, 2 * c:2 * c + 2])
        m["cache_logf"] = f32(inputs["cache_logf"][0, 2 * c:2 * c + 2])
        m["state_wkv"] = f32(inputs["state_wkv"][0, 2 * c:2 * c + 2])
        m["state_shift"] = f32(inputs["state_shift"][0, 2 * c:2 * c + 2])
        in_maps.append({k: v for k, v in m.items() if k in p.ins})
    res = run_bass_kernel_spmd(p.nc, in_maps, core_ids=list(range(ncores)))
    R = res.results
    B = BS = 2 * ncores
    z = lambda *sh: np.zeros(sh, np.float32)
    yp, ys = z(B, L, D), z(BS, LS, D)
    nkp, nvp, nlp = z(1, B, L, NH, HD), z(1, B, L, NH, HD), z(1, B, L, NH)
    nks, nvs, nls = z(1, BS, LS, NH, HD), z(1, BS, LS, NH, HD), z(1, BS, LS, NH)
    shp, shs = z(1, B, RSW), z(1, BS, RSW)
    wkvp, wkvs = z(1, B, RH, RD, RD), z(1, BS, RH, RD, RD)
    for c in range(ncores):
        r = R[c]
        for i in range(2):
            b = 2 * c + i
            o = 2 * L + i * LS
            nkp[0, b] = r["k_all"][i * L:(i + 1) * L].reshape(L, NH, HD)
            nvp[0, b] = r["v_all"][i * L:(i + 1) * L].reshape(L, NH, HD)
            nlp[0, b] = r["logf_all"][i * L:(i + 1) * L]
            nks[0, b] = r["k_all"][o:o + LS].reshape(LS, NH, HD)
            nvs[0, b] = r["v_all"][o:o + LS].reshape(LS, NH, HD)
            nls[0, b] = r["logf_all"][o:o + LS]
            shp[0, b] = r["shift_out"][i]
            shs[0, b] = r["shift_out"][2 + i]
            if "wkv_out" in r:
                wkvp[0, b] = r["wkv_out"][i]
                wkvs[0, b] = r["wkv_out"][2 + i]
            if "y_all" in r:
                yp[b] = r["y_all"][i * L:(i + 1) * L]
                ys[b] = r["y_all"][o:o + LS]
    _CACHE["last"] = R
    return (yp, ys, nkp, nvp, nlp, wkvp, shp, nks, nvs, nls, wkvs, shs)


def kernel(**inputs):
    return run(inputs, L_FULL, LS_FULL, PAST_FULL, 8)
```

```python
import numpy as np
from contextlib import ExitStack
import concourse.bass as bass
import concourse.mybir as mybir
from concourse.bass_utils import run_bass_kernel_spmd

F32 = mybir.dt.float32
F32R = mybir.dt.float32r
I32 = mybir.dt.int32
U32 = mybir.dt.uint32
AF = mybir.ActivationFunctionType
ALU = mybir.AluOpType
AX = mybir.AxisListType

D = 2048
NH = 8
HD = 128
FW = 1024
RH = 16
RD = 64
RW = 1024
RSW = 3360
INW = 10536
OFF_Q, OFF_K, OFF_V, OFF_F, OFF_RW, OFF_G = 0, 1024, 2048, 3072, 3080, 6440
NEXP = 32
DE = 512
PLE = 256
NORM_EPS = 1e-6
STQ = "act"
GN_EPS = 64e-5
DECAY_SCALE = float(np.exp(-0.5))


class T:
    __slots__ = ("w", "r", "name")

    def __init__(self, name=""):
        self.w = None
        self.r = {}
        self.name = name


class Sched:
    NDS = 16

    def __init__(self, nc):
        self.nc = nc
        self.E = dict(pe=nc.tensor, act=nc.scalar, dve=nc.vector, pool=nc.gpsimd, sp=nc.sync)
        self.csem = {e: nc.alloc_semaphore(name=f"c_{e}") for e in ("pe", "act", "dve", "pool")}
        self.ccnt = {e: 0 for e in self.csem}
        self.dsem = {q: [nc.alloc_semaphore(name=f"d_{q}{i}") for i in range(self.NDS)]
                     for q in ("sp", "pool", "act")}
        self.dcnt = {q: [0] * self.NDS for q in self.dsem}
        self.dnext = {q: 0 for q in self.dsem}
        self.waited = {}
        self.nwaits = 0
        self.ninst = 0

    def _wait(self, eng, tok):
        sem, val, src = tok
        if src == eng and eng == "pe":
            return
        key = (eng, id(sem))
        if self.waited.get(key, 0) >= val:
            return
        self.E[eng].wait_ge(sem, val)
        self.nwaits += 1
        self.waited[key] = val

    def _deps(self, eng, reads, writes):
        for t in reads:
            if t.w is not None:
                self._wait(eng, t.w)
        for t in writes:
            if t.w is not None:
                self._wait(eng, t.w)
            for tok in t.r.values():
                self._wait(eng, tok)

    def _mark(self, tok, reads, writes):
        k = id(tok[0])
        for t in reads:
            t.r[k] = tok
        for t in writes:
            t.w = tok
            t.r = {}

    def op(self, eng, fn, reads=(), writes=()):
        self._deps(eng, reads, writes)
        inst = fn(self.E[eng])
        self.ccnt[eng] += 1
        inst.then_inc(self.csem[eng], 1)
        self.ninst += 1
        self._mark((self.csem[eng], self.ccnt[eng], eng), reads, writes)

    def dma(self, q, out, in_, reads=(), writes=(), fn=None, **kw):
        self._deps(q, reads, writes)
        i = self.dnext[q]
        self.dnext[q] = (i + 1) % self.NDS
        sem = self.dsem[q][i]
        if self.dcnt[q][i] > 0:
            self._wait(q, (sem, self.dcnt[q][i], "dma"))
        if fn is not None:
            inst = fn(self.E[q])
        else:
            inst = self.E[q].dma_start(out=out, in_=in_, **kw)
        self.dcnt[q][i] += 16
        inst.then_inc(sem, 16)
        self.ninst += 1
        self._mark((sem, self.dcnt[q][i], "dma"), reads, writes)

    def barrier(self, engines=("pe", "act", "dve", "pool", "sp")):
        for e in engines:
            for s, c in self.ccnt.items():
                if c > 0:
                    self._wait(e, (self.csem[s], c, "x"))
            for q in self.dsem:
                for i in range(self.NDS):
                    if self.dcnt[q][i] > 0:
                        self._wait(e, (self.dsem[q][i], self.dcnt[q][i], "dma"))


def r32(ap):
    return ap.bitcast(F32R)


class Prog:
    def __init__(self, L, LS, PAST, phases=None):
        self.L, self.LS, self.PAST = L, LS, PAST
        self.TT = 2 * L + 2 * LS
        self.phases = phases
        nc = bass.Bass("TRN2", target_bir_lowering=False)
        nc.dge_precook = False
        self.nc = nc
        self.s = Sched(nc)
        self.ins = {}
        self.outs = {}
        self.build()

    def din(self, name, shape, dt=F32):
        t = self.nc.dram_tensor(name, list(shape), dt, kind="ExternalInput").ap()
        self.ins[name] = t
        return t

    def cin(self, name, shape, dt=F32):
        if name in self.ins:
            return self.ins[name]
        return self.din(name, shape, dt)

    def dout(self, name, shape, dt=F32):
        t = self.nc.dram_tensor(name, list(shape), dt, kind="ExternalOutput").ap()
        self.outs[name] = t
        return t

    def dscr(self, name, shape, dt=F32):
        return self.nc.dram_tensor(name, list(shape), dt, kind="Internal").ap()

    def build(self):
        nc, s = self.nc, self.s
        L, LS, PAST, TT = self.L, self.LS, self.PAST, self.TT
        x_all = self.din("x_all", [TT, D])
        w_in = self.din("w_in", [D, INW])
        g_mix = self.din("g_mix", [1, D])
        b_f = self.din("b_f", [1, NH])
        ident_d = self.din("ident", [128, 128])
        k_all = self.dout("k_all", [TT, FW])
        v_all = self.dout("v_all", [TT, FW])
        logf_all = self.dout("logf_all", [TT, NH])
        shift_out = self.dout("shift_out", [4, RSW])
        ends = {L - 1: 0, 2 * L - 1: 1, 2 * L + LS - 1: 2, 2 * L + 2 * LS - 1: 3}
        qT_d = self.dscr("qT_d", [NH, 128, TT])
        kT_d = self.dscr("kT_d", [NH, 128, TT])
        rwT_d = self.dscr("rwT_d", [53, 64, TT])
        sgT_d = self.dscr("sgT_d", [32, 128, TT])
        self.dbg = {}
        if self.phases is not None and "dbg12" in self.phases:
            self.dbg["qT"] = self.dout("dbg_qT", [NH, 128, TT])
            self.dbg["rwT"] = self.dout("dbg_rwT", [53, 64, TT])
            self.dbg["sgT"] = self.dout("dbg_sgT", [32, 128, TT])
            qT_d, rwT_d, sgT_d = self.dbg["qT"], self.dbg["rwT"], self.dbg["sgT"]

        groups = []
        G = 512
        for sq in range(2):
            for g0 in range(0, L, G):
                groups.append((sq * L + g0, min(G, L - g0)))
        groups.append((2 * L, 2 * LS))

        with ExitStack() as es:
            ident = es.enter_context(nc.sbuf_tensor("sb_ident", [128, 128], F32))
            gbc = es.enter_context(nc.sbuf_tensor("sb_gbc", [128, D], F32))
            bfbc = es.enter_context(nc.sbuf_tensor("sb_bfbc", [128, NH], F32))
            xt0 = es.enter_context(nc.sbuf_tensor("sb_xt0", [128, D], F32))
            xt1 = es.enter_context(nc.sbuf_tensor("sb_xt1", [128, D], F32))
            xn = es.enter_context(nc.sbuf_tensor("sb_xn", [128, D], F32))
            junk = es.enter_context(nc.sbuf_tensor("sb_junk", [128, D], F32))
            stat = es.enter_context(nc.sbuf_tensor("sb_stat", [128, 8], F32))
            hT = es.enter_context(nc.sbuf_tensor("sb_hT", [128, 16, G], F32R))
            ws0 = es.enter_context(nc.sbuf_tensor("sb_ws0", [128, 16, 512], F32R))
            ws1 = es.enter_context(nc.sbuf_tensor("sb_ws1", [128, 16, 512], F32R))
            stg0 = es.enter_context(nc.sbuf_tensor("sb_stg0", [128, 512], F32))
            stg1 = es.enter_context(nc.sbuf_tensor("sb_stg1", [128, 512], F32))
            stg2 = es.enter_context(nc.sbuf_tensor("sb_stg2", [128, 512], F32))
            stg3 = es.enter_context(nc.sbuf_tensor("sb_stg3", [128, 512], F32))
            lf0 = es.enter_context(nc.sbuf_tensor("sb_lf0", [128, 16], F32))
            t_ident, t_gbc, t_bfbc = T("ident"), T("gbc"), T("bfbc")
            s.dma("sp", ident[:], ident_d[:, :], writes=[t_ident])
            s.dma("sp", gbc[:], g_mix[0:1, :].partition_broadcast(128), writes=[t_gbc])
            s.dma("sp", bfbc[:], b_f[0:1, :].partition_broadcast(128), writes=[t_bfbc])
            ps = self.ps = [nc.alloc_psum_tensor(f"ps{i}", [128, 512], F32) for i in range(8)]
            t_ps = self.t_ps = [T(f"ps{i}") for i in range(8)]
            self.psn = 0

            def next_ps():
                i = self.psn
                self.psn = (i + 1) % 8
                return ps[i], t_ps[i]

            xts = [(xt0, T("xt0")), (xt1, T("xt1"))]
            wss = [(ws0, T("ws0")), (ws1, T("ws1"))]
            stgs = [(stg0, T("stg0")), (stg1, T("stg1")), (stg2, T("stg2")), (stg3, T("stg3"))]
            t_xn, t_junk, t_stat, t_hT, t_lf = T("xn"), T("junk"), T("stat"), T("hT"), T("lf")
            self.stn = 0
            self.evn = 0

            def next_stg():
                i = self.stn
                self.stn = (i + 1) % 4
                return stgs[i]

            def evac(out_ap, in_ap, reads, writes, func=None):
                self.evn += 1
                if func is not None:
                    s.op("act", lambda e: e.activation(out=out_ap, in_=in_ap, func=func), reads, writes)
                elif self.evn % 2 == 0:
                    s.op("act", lambda e: e.activation(out=out_ap, in_=in_ap, func=AF.Copy), reads, writes)
                else:
                    s.op("dve", lambda e: e.tensor_copy(out=out_ap, in_=in_ap), reads, writes)

            slabs = []
            for c0 in range(0, 1024, 512):
                slabs.append((OFF_Q + c0, 512, "q"))
            for c0 in range(0, 1024, 512):
                slabs.append((OFF_K + c0, 512, "k"))
            for c0 in range(0, 1024, 512):
                slabs.append((OFF_V + c0, 512, "v"))
            slabs.append((OFF_F, 8, "f"))
            for c0 in range(0, RSW, 512):
                slabs.append((OFF_RW + c0, min(512, RSW - c0), "rw"))
            for c0 in range(0, 4096, 512):
                slabs.append((OFF_G + c0, 512, "g"))

            xi = 0
            wi = 0
            for (tok0, ntok) in groups:
                ntile = (ntok + 127) // 128
                for ti in range(ntile):
                    n = min(128, ntok - ti * 128)
                    xt, t_xt = xts[xi % 2]
                    xi += 1
                    r0 = tok0 + ti * 128
                    s.dma("sp", xt[0:n, :], x_all[r0:r0 + n, :], writes=[t_xt])
                    s.op("act", lambda e: e.activation(out=junk[0:n, :], in_=xt[0:n, :], func=AF.Square,
                                                       accum_out=stat[0:n, 0:1]),
                         reads=[t_xt], writes=[t_junk, t_stat])
                    s.op("act", lambda e: e.activation(out=stat[0:n, 1:2], in_=stat[0:n, 0:1], func=AF.Sqrt,
                                                       scale=1.0 / D, bias=NORM_EPS),
                         reads=[t_stat], writes=[t_stat])
                    s.op("dve", lambda e: e.reciprocal(out=stat[0:n, 2:3], in_=stat[0:n, 1:2]),
                         reads=[t_stat], writes=[t_stat])
                    s.op("dve", lambda e: e.scalar_tensor_tensor(out=xn[0:n, :], in0=xt[0:n, :],
                                                                 scalar=stat[0:n, 2:3], in1=gbc[0:n, :],
                                                                 op0=ALU.mult, op1=ALU.mult),
                         reads=[t_xt, t_stat, t_gbc], writes=[t_xn])
                    for k4 in range(4):
                        pt, t_pt = next_ps()
                        for j in range(4):
                            kc = k4 * 4 + j
                            s.op("pe", lambda e: e.transpose(out=pt[:, j * 128:j * 128 + n],
                                                             in_=xn[0:n, kc * 128:(kc + 1) * 128],
                                                             identity=ident[0:n, 0:n]),
                                 reads=[t_xn, t_ident], writes=[t_pt])
                        evac(hT[:, k4 * 4:k4 * 4 + 4, ti * 128:ti * 128 + n],
                             pt[:].rearrange("p (j t) -> p j t", j=4)[:, :, 0:n],
                             reads=[t_pt], writes=[t_hT])
                dbgf = self.phases or ()
                if "p1only" in dbgf:
                    s.dma("sp", self.dbg["rwT"][0:16, :, tok0:tok0 + ntok].rearrange("k p t -> p k t"),
                          hT[:, :, 0:ntok].bitcast(F32), reads=[t_hT])
                    continue
                for (c0, ncol, kind) in slabs:
                    if any(("only_" + kk) in dbgf for kk in "qkvfg") and ("only_" + kind[0]) not in dbgf:
                        continue
                    ws, t_ws = wss[wi % 2]
                    wi += 1
                    s.dma("sp", ws[:, :, 0:ncol],
                          r32(w_in[:, c0:c0 + ncol]).rearrange("(kc p) c -> p kc c", p=128),
                          writes=[t_ws])
                    if kind in ("q", "k", "rw", "g"):
                        for cb in range(0, ncol, 128):
                            m = min(128, ncol - cb)
                            pt, t_pt = next_ps()
                            for kc in range(16):
                                s.op("pe", lambda e: e.matmul(pt[:, 0:ntok], lhsT=ws[:, kc, cb:cb + 128],
                                                              rhs=hT[:, kc, 0:ntok],
                                                              start=(kc == 0), stop=(kc == 15)),
                                     reads=[t_ws, t_hT], writes=[t_pt])
                            stg, t_stg = next_stg()
                            evac(stg[0:m, 0:ntok], pt[0:m, 0:ntok], [t_pt], [t_stg],
                                 func=(AF.Sigmoid if kind == "g" else None))
                            if kind == "q":
                                dst = qT_d[(c0 - OFF_Q + cb) // 128, :, tok0:tok0 + ntok]
                            elif kind == "k":
                                dst = kT_d[(c0 - OFF_K + cb) // 128, :, tok0:tok0 + ntok]
                            elif kind == "rw":
                                hc = (c0 - OFF_RW + cb) // 64
                                dst = rwT_d[hc, 0:min(m, 64), tok0:tok0 + ntok]
                                if m > 64:
                                    s.dma(STQ, rwT_d[hc + 1, :, tok0:tok0 + ntok], stg[64:128, 0:ntok], reads=[t_stg])
                            else:
                                dst = sgT_d[(c0 - OFF_G + cb) // 128, :, tok0:tok0 + ntok]
                            s.dma(STQ, dst, stg[0:(min(m, 64) if kind == "rw" else m), 0:ntok], reads=[t_stg])
                            if kind == "rw":
                                for te, sidx in ends.items():
                                    if tok0 <= te < tok0 + ntok:
                                        cc = c0 - OFF_RW + cb
                                        s.dma(STQ, shift_out[sidx, cc:cc + m].rearrange("(p o) -> p o", o=1),
                                              stg[0:m, te - tok0:te - tok0 + 1], reads=[t_stg])
                    if kind in ("k", "v", "f"):
                        for ti in range(ntile):
                            n = min(128, ntok - ti * 128)
                            pt, t_pt = next_ps()
                            for kc in range(16):
                                s.op("pe", lambda e: e.matmul(pt[0:n, 0:ncol],
                                                              lhsT=hT[:, kc, ti * 128:ti * 128 + n],
                                                              rhs=ws[:, kc, 0:ncol],
                                                              start=(kc == 0), stop=(kc == 15)),
                                     reads=[t_ws, t_hT], writes=[t_pt])
                            r0 = tok0 + ti * 128
                            if kind == "f":
                                s.op("dve", lambda e: e.tensor_tensor(out=lf0[0:n, 0:8], in0=pt[0:n, 0:8],
                                                                      in1=bfbc[0:n, :], op=ALU.add),
                                     reads=[t_pt, t_bfbc], writes=[t_lf])
                                s.op("act", lambda e: e.activation(out=lf0[0:n, 0:8], in_=lf0[0:n, 0:8],
                                                                   func=AF.Exp, scale=-1.0),
                                     reads=[t_lf], writes=[t_lf])
                                s.op("act", lambda e: e.activation(out=lf0[0:n, 0:8], in_=lf0[0:n, 0:8],
                                                                   func=AF.Ln, bias=1.0),
                                     reads=[t_lf], writes=[t_lf])
                                s.op("dve", lambda e: e.tensor_scalar(out=lf0[0:n, 8:16], in0=lf0[0:n, 0:8],
                                                                      scalar1=-1.0, scalar2=None, op0=ALU.mult),
                                     reads=[t_lf], writes=[t_lf])
                                s.dma(STQ, logf_all[r0:r0 + n, :], lf0[0:n, 8:16], reads=[t_lf])
                            else:
                                stg, t_stg = next_stg()
                                evac(stg[0:n, 0:ncol], pt[0:n, 0:ncol], [t_pt], [t_stg])
                                dst = (k_all if kind == "k" else v_all)
                                cc = c0 - (OFF_K if kind == "k" else OFF_V)
                                s.dma(STQ, dst[r0:r0 + n, cc:cc + ncol], stg[0:n, 0:ncol], reads=[t_stg])
            s.barrier()
        self.qT_d, self.kT_d, self.rwT_d, self.sgT_d = qT_d, kT_d, rwT_d, sgT_d
        self.k_all, self.v_all, self.logf_all, self.ident_d = k_all, v_all, logf_all, ident_d
        self.x_all = x_all
        ph = self.phases or ()
        if "stop12" not in ph:
            if "noattn" not in ph:
                self.phase_attn()
            if "norwkv" not in ph:
                self.phase_rwkv()
            if "stoprw" not in ph:
                self.phase_merge()
                if "stopmerge" not in ph:
                    self.phase_experts()
                    if "stopexp" not in ph:
                        self.phase_final()
        s.barrier(engines=("sp",))

    def phase_attn(self):
        nc, s = self.nc, self.s
        L, LS, PAST, TT = self.L, self.LS, self.PAST, self.TT
        scale = float(HD) ** -0.5
        cache_k = self.din("cache_k", [2, PAST, NH, HD])
        cache_v = self.din("cache_v", [2, PAST, NH, HD])
        cache_logf = self.din("cache_logf", [2, PAST, NH])
        triu_d = self.cin("triu", [128, 128])
        ones_d = self.cin("ones", [128, 128])
        Lkmax = max(L, PAST + LS)
        nkbmax = (Lkmax + 127) // 128
        nkbp = ((nkbmax + 15) // 16) * 16
        Lqmax = max(L, LS)
        FThi_d = self.dscr("FThi_d", [4, NH, nkbp * 128], F32R)
        FTlo_d = self.dscr("FTlo_d", [4, NH, nkbp * 128], F32R)
        foxT_d = self.dscr("foxT_d", [NH, 128, TT])
        if "dbgattn" in (self.phases or ()):
            foxT_d = self.dout("dbg_foxT", [NH, 128, TT])
        self.foxT_d = foxT_d
        seqs = [(0, L, 0, None), (L, L, 0, None), (2 * L, LS, PAST, 0), (2 * L + LS, LS, PAST, 1)]
        with ExitStack() as es:
            def sb(name, shape, dt=F32):
                return es.enter_context(nc.sbuf_tensor("a_" + name, shape, dt))
            ident = sb("ident", [128, 128])
            triu = sb("triu", [128, 128])
            ones = sb("ones", [128, 128], F32R)
            onesf = sb("onesf", [128, 128])
            LF = sb("LF", [128, nkbp, NH])
            tot = sb("tot", [128, nkbp, NH])
            inc = sb("inc", [128, nkbp, NH])
            Fall = sb("Fall", [128, nkbp, NH])
            negF = sb("negF", [128, nkbp, NH])
            FThi = sb("FThi", [128, (nkbp // 16) * 128], F32R)
            FTlo = sb("FTlo", [128, (nkbp // 16) * 128], F32R)
            qT = sb("qT", [128, Lqmax], F32R)
            kT = sb("kT", [128, nkbmax * 128], F32R)
            V = sb("V", [128, nkbmax, 128], F32R)
            kc = sb("kc", [128, max(PAST // 128, 1), 128])
            Fq = sb("Fq", [128, Lqmax], F32R)
            PT0 = sb("PT0", [128, 512], F32R)
            PT1 = sb("PT1", [128, 512], F32R)
            PT2 = sb("PT2", [128, 512], F32R)
            rinv = sb("rinv", [128, 512])
            ost0 = sb("ost0", [128, 512])
            ost1 = sb("ost1", [128, 512])
            ps, t_ps = self.ps, self.t_ps
            t_c = T("consts")
            s.dma("sp", ident[:], self.ident_d[:, :], writes=[t_c])
            s.dma("sp", triu[:], triu_d[:, :], writes=[t_c])
            s.dma("sp", ones[:], r32(ones_d[:, :]), writes=[t_c])
            s.dma("sp", onesf[:], ones_d[:, :], writes=[t_c])
            t_LF, t_tot, t_inc, t_Fall, t_negF, t_FThi, t_FTlo, t_FTd = (T() for _ in range(8))
            t_qT, t_kT, t_V, t_kc, t_Fq = (T() for _ in range(5))
            s.op("dve", lambda e: e.memset(Fq[:, :].bitcast(F32), 0.0), writes=[t_Fq])
            s.op("dve", lambda e: e.memset(kT[:, :].bitcast(F32), 0.0), writes=[t_kT])
            s.op("dve", lambda e: e.memset(V[:, :, :].bitcast(F32), 0.0), writes=[t_V])
            s.op("dve", lambda e: e.memset(Fall[:, :, :], 0.0), writes=[t_Fall])
            PTs = [(PT0, T()), (PT1, T()), (PT2, T())]
            osts = [(ost0, T()), (ost1, T())]
            t_rinv = T()
            pti = 0
            psi = 0
            qbi = 0
            for si, (tok0, Lq, off, ci) in enumerate(seqs):
                Lk = off + Lq
                nkb = (Lk + 127) // 128
                s.op("dve", lambda e: e.memset(LF[:, :, :], 0.0), writes=[t_LF])
                if off:
                    s.dma("sp", LF[:, 0:off // 128, :], cache_logf[ci].rearrange("(kb p) h -> p kb h", p=128),
                          writes=[t_LF])
                for j in range(0, Lq, 128):
                    n = min(128, Lq - j)
                    s.dma("sp", LF[0:n, (off + j) // 128, :], self.logf_all[tok0 + j:tok0 + j + n, :], writes=[t_LF])
                N8 = nkb * NH
                psA, t_psA = ps[7], t_ps[7]
                psB, t_psB = ps[6], t_ps[6]
                s.op("pe", lambda e: e.matmul(psA[:, 0:N8], lhsT=triu[:, :],
                                              rhs=LF[:, 0:nkb, :].rearrange("p k h -> p (k h)"),
                                              start=True, stop=True),
                     reads=[t_c, t_LF], writes=[t_psA])
                s.op("pe", lambda e: e.matmul(psB[:, 0:N8], lhsT=onesf[:, :],
                                              rhs=LF[:, 0:nkb, :].rearrange("p k h -> p (k h)"),
                                              start=True, stop=True),
                     reads=[t_c, t_LF], writes=[t_psB])
                s.op("act", lambda e: e.activation(out=tot[:, 0:nkb, :].rearrange("p k h -> p (k h)"),
                                                   in_=psB[:, 0:N8], func=AF.Copy),
                     reads=[t_psB], writes=[t_tot])
                for h in range(NH):
                    s.op("dve", lambda e: e.tensor_tensor_scan(out=inc[:, 0:nkb, h], data0=onesf[:, 0:nkb],
                                                               data1=tot[:, 0:nkb, h], initial=0.0,
                                                               op0=ALU.mult, op1=ALU.add),
                         reads=[t_tot, t_c], writes=[t_inc])
                s.op("dve", lambda e: e.tensor_tensor(out=inc[:, 0:nkb, :], in0=inc[:, 0:nkb, :],
                                                      in1=tot[:, 0:nkb, :], op=ALU.subtract),
                     reads=[t_inc, t_tot], writes=[t_inc])
                s.op("dve", lambda e: e.tensor_tensor(out=Fall[:, 0:nkb, :].rearrange("p k h -> p (k h)"),
                                                      in0=psA[:, 0:N8],
                                                      in1=inc[:, 0:nkb, :].rearrange("p k h -> p (k h)"),
                                                      op=ALU.add),
                     reads=[t_psA, t_inc], writes=[t_Fall])
                s.op("dve", lambda e: e.tensor_scalar(out=negF[:, 0:nkb, :], in0=Fall[:, 0:nkb, :], scalar1=-1.0,
                                                      scalar2=None, op0=ALU.mult),
                     reads=[t_Fall], writes=[t_negF])
                ng = (nkb + 15) // 16
                for g in range(ng):
                    s.op("pe", lambda e: e.transpose(out=psA[:, g * 128:(g + 1) * 128],
                                                     in_=Fall[:, g * 16:(g + 1) * 16, :].rearrange("p k h -> p (k h)"),
                                                     identity=ident[:, :]),
                         reads=[t_Fall, t_c], writes=[t_psA])
                s.op("dve", lambda e: e.tensor_scalar(out=FThi[:, 0:ng * 128], in0=psA[:, 0:ng * 128],
                                                      scalar1=1.0 / scale, scalar2=None, op0=ALU.mult),
                     reads=[t_psA], writes=[t_FThi])
                s.op("dve", lambda e: e.scalar_tensor_tensor(out=FTlo[:, 0:ng * 128], in0=psA[:, 0:ng * 128],
                                                             scalar=1.0 / scale,
                                                             in1=FThi[:, 0:ng * 128].bitcast(F32),
                                                             op0=ALU.mult, op1=ALU.subtract),
                     reads=[t_psA, t_FThi], writes=[t_FTlo])
                for kb in range(nkb):
                    g, kl = kb // 16, kb % 16
                    s.dma("sp", FThi_d[si, :, kb * 128:(kb + 1) * 128],
                          FThi[kl * 8:(kl + 1) * 8, g * 128:(g + 1) * 128], reads=[t_FThi], writes=[t_FTd])
                    s.dma("sp", FTlo_d[si, :, kb * 128:(kb + 1) * 128],
                          FTlo[kl * 8:(kl + 1) * 8, g * 128:(g + 1) * 128], reads=[t_FTlo], writes=[t_FTd])
                if "attn_stopF" in (self.phases or ()):
                    continue
                for h in range(NH):
                    s.dma("sp", qT[:, 0:Lq], r32(self.qT_d[h, :, tok0:tok0 + Lq]), writes=[t_qT])
                    s.dma("sp", kT[:, off:Lk], r32(self.kT_d[h, :, tok0:tok0 + Lq]), writes=[t_kT])
                    if off:
                        s.dma("sp", kc[:, 0:off // 128, :],
                              cache_k[ci, :, h, :].rearrange("(kb p) d -> p kb d", p=128), writes=[t_kc])
                        s.dma("sp", V[:, 0:off // 128, :],
                              r32(cache_v[ci, :, h, :]).rearrange("(kb p) d -> p kb d", p=128), writes=[t_V])
                    if Lq % 128 == 0:
                        s.dma("sp", V[:, off // 128:off // 128 + Lq // 128, :],
                              r32(self.v_all[tok0:tok0 + Lq, h * 128:(h + 1) * 128]).rearrange(
                                  "(kb p) d -> p kb d", p=128), writes=[t_V])
                    else:
                        for j in range(0, Lq, 128):
                            n = min(128, Lq - j)
                            s.dma("sp", V[0:n, (off + j) // 128, :],
                                  r32(self.v_all[tok0 + j:tok0 + j + n, h * 128:(h + 1) * 128]), writes=[t_V])
                    s.dma("sp", Fq[0:1, 0:Lq], FThi_d[si, h:h + 1, off:Lk], reads=[t_FTd], writes=[t_Fq])
                    s.dma("sp", Fq[1:2, 0:Lq], FTlo_d[si, h:h + 1, off:Lk], reads=[t_FTd], writes=[t_Fq])
                    if off:
                        for k4 in range(0, off // 128, 4):
                            pt, t_pt = ps[7], t_ps[7]
                            nb = min(4, off // 128 - k4)
                            for j in range(nb):
                                s.op("pe", lambda e: e.transpose(out=pt[:, j * 128:(j + 1) * 128],
                                                                 in_=kc[:, k4 + j, :], identity=ident[:, :]),
                                     reads=[t_kc, t_c], writes=[t_pt])
                            s.op("dve", lambda e: e.tensor_copy(out=kT[:, k4 * 128:(k4 + nb) * 128],
                                                                in_=pt[:, 0:nb * 128]),
                                 reads=[t_pt], writes=[t_kT])
                    if "attn_noblocks" in (self.phases or ()):
                        continue
                    if "attn_only_prompt" in (self.phases or ()) and off:
                        continue
                    if "attn_only_sample" in (self.phases or ()) and not off:
                        continue
                    for t0 in range(0, Lq, 512):
                        nq = min(512, Lq - t0)
                        psO, t_psO = ps[3 + qbi % 2], t_ps[3 + qbi % 2]
                        psR, t_psR = ps[5], t_ps[5]
                        ost, t_ost = osts[qbi % 2]
                        qbi += 1
                        blocks = []
                        for kb in range(nkb):
                            s0 = kb * 128
                            ns = min(128, Lk - s0)
                            dlt = off + t0 - s0
                            if dlt >= ns - 1:
                                blocks.append((kb, s0, 0, False))
                            else:
                                cs = -dlt
                                assert cs >= 0
                                if cs < nq:
                                    blocks.append((kb, s0, cs, True))
                        for bi, (kb, s0, cs, diag) in enumerate(blocks):
                            first, last = bi == 0, bi == len(blocks) - 1
                            psS, t_psS = ps[psi % 3], t_ps[psi % 3]
                            psi += 1
                            PT, t_PT = PTs[pti % 3]
                            pti += 1
                            s.op("pe", lambda e: e.matmul(psS[:, cs:nq], lhsT=kT[:, s0:s0 + 128],
                                                          rhs=qT[:, t0 + cs:t0 + nq], start=True, stop=False),
                                 reads=[t_kT, t_qT], writes=[t_psS])
                            s.op("pe", lambda e: e.matmul(psS[:, cs:nq], lhsT=ones[:, :],
                                                          rhs=Fq[:, t0 + cs:t0 + nq], start=False, stop=True),
                                 reads=[t_c, t_Fq], writes=[t_psS])
                            for hh in range(1):
                                s.op("act", lambda e: e.activation(out=PT[:, cs:nq], in_=psS[:, cs:nq],
                                                                   func=AF.Exp, scale=scale,
                                                                   bias=negF[:, kb, h:h + 1]),
                                     reads=[t_psS, t_negF], writes=[t_PT])
                            if diag:
                                w = min(128, nq - cs)
                                s.op("dve", lambda e: e.tensor_tensor(out=PT[:, cs:cs + w],
                                                                      in0=PT[:, cs:cs + w].bitcast(F32),
                                                                      in1=triu[:, 0:w], op=ALU.mult),
                                     reads=[t_PT, t_c], writes=[t_PT])
                            s.op("pe", lambda e: e.matmul(psO[:, cs:nq], lhsT=V[:, kb, :],
                                                          rhs=PT[:, cs:nq], start=first, stop=last),
                                 reads=[t_V, t_PT], writes=[t_psO])
                            s.op("pe", lambda e: e.matmul(psR[:, cs:nq], lhsT=ones[:, :],
                                                          rhs=PT[:, cs:nq], start=first, stop=last),
                                 reads=[t_c, t_PT], writes=[t_psR])
                        s.op("dve", lambda e: e.reciprocal(out=rinv[:, 0:nq], in_=psR[:, 0:nq]),
                             reads=[t_psR], writes=[t_rinv])
                        s.op("dve", lambda e: e.tensor_tensor(out=ost[:, 0:nq], in0=psO[:, 0:nq],
                                                              in1=rinv[:, 0:nq], op=ALU.mult),
                             reads=[t_psO, t_rinv], writes=[t_ost])
                        s.dma(STQ, foxT_d[h, :, tok0 + t0:tok0 + t0 + nq], ost[:, 0:nq], reads=[t_ost])
            s.barrier()


    def phase_rwkv(self):
        nc, s = self.nc, self.s
        L, LS, PAST, TT = self.L, self.LS, self.PAST, self.TT
        C = 64
        state_wkv = self.din("state_wkv", [2, RH, RD, RD])
        state_shift = self.din("state_shift", [2, RSW])
        mu_d = self.din("mu_shift", [1, RSW])
        w0_d = self.din("w0", [1, RW])
        a0_d = self.din("a0", [1, RW])
        kk_d = self.din("k_k", [1, RW])
        ka_d = self.din("k_a", [1, RW])
        rk_d = self.din("r_k", [1, RW])
        lg_d = self.din("lnx_g", [1, RW])
        lb_d = self.din("lnx_b", [1, RW])
        ww2_d = self.din("w_w2", [64, RW])
        wa2_d = self.din("w_a2", [64, RW])
        wg2_d = self.din("w_g2", [160, RW])
        msu_d = self.din("m_su", [64, 512])
        msl_d = self.din("m_sl", [64, 512])
        mu8_d = self.din("m_u", [64, 512])
        i8_d = self.din("i8", [64, 512])
        ones_d = self.cin("ones", [128, 128])
        wkv_out = self.dout("wkv_out", [4, RH, RD, RD])
        rwoT_d = self.dscr("rwoT_d", [8, 128, TT])
        if "dbgrw" in (self.phases or ()):
            rwoT_d = self.dout("dbg_rwoT", [8, 128, TT])
        self.rwoT_d = rwoT_d
        rwT_d = self.rwT_d
        seqs = [(0, L, None), (L, L, None), (2 * L, LS, 0), (2 * L + LS, LS, 1)]
        with ExitStack() as es:
            def sb(name, shape, dt=F32):
                return es.enter_context(nc.sbuf_tensor("r_" + name, shape, dt))
            ident = sb("ident", [128, 128])
            ones = sb("ones", [64, 64])
            msu = sb("msu", [64, 8, 64]); msl = sb("msl", [64, 8, 64]); mu8 = sb("mu8", [64, 8, 64]); i8 = sb("i8", [64, 8, 64])
            mu = sb("mu", [64, 53]); w0 = sb("w0", [64, 16]); a0 = sb("a0", [64, 16]); k_k = sb("k_k", [64, 16])
            k_a = sb("k_a", [64, 16]); r_k = sb("r_k", [64, 16])
            lgb = sb("lgb", [64, RW]); lbb = sb("lbb", [64, RW])
            ww2 = sb("ww2", [64, RW]); wa2 = sb("wa2", [64, RW]); wg2 = sb("wg2", [64, 3, RW])
            ST = sb("ST", [64, 16, 64]); S0 = sb("S0", [64, 16, 64])
            Xs = [sb(f"X{i}", [64, 53, C + 1]) for i in range(2)]; xm = sb("xm", [64, 53, C])
            txws = [sb(f"txw{i}", [64, C]) for i in range(2)]; sxgs = [sb(f"sxg{i}", [64, 3, C]) for i in range(2)]
            names = ["lw", "a", "kkn", "krep", "Lc", "Ep", "rt", "En", "bh", "bg", "kg", "rkp", "tmp",
                     "Vtok", "bgT", "kgT", "N", "NT", "AK", "QK", "QB", "P0", "P1", "PT0", "PT1",
                     "X0", "X1"]
            alias = {"yc": "P0", "sq": "P1", "rwtok": "NT", "W0": "PT1", "U": "N"}
            tls, tts, st8s, rwoTs, t_st8s, t_rwoTs = [], [], [], [], [], []
            HG = 8
            NG = 16 // HG
            for g_ in range(NG):
                tl_ = {n: sb(f"{n}_{g_}", [64, HG, 64]) for n in names}
                tt_ = {n: T(n) for n in names}
                for k_, v_ in alias.items():
                    tl_[k_] = tl_[v_]
                    tt_[k_] = tt_[v_]
                tls.append(tl_)
                tts.append(tt_)
                st8s.append(sb(f"st8_{g_}", [64, 32]))
                rwoTs.append(sb(f"rwoT_{g_}", [128, HG // 2, 64]))
                t_st8s.append(T())
                t_rwoTs.append(T())
            ps, t_ps = self.ps, self.t_ps
            t_c = T("rconsts")
            s.dma("sp", ident[:], self.ident_d[:, :], writes=[t_c])
            s.dma("sp", ones[:], ones_d[0:64, 0:64], writes=[t_c])
            for tile_, d_ in ((msu, msu_d), (msl, msl_d), (mu8, mu8_d), (i8, i8_d)):
                s.dma("sp", tile_[:, :, :].rearrange("p a b -> p (a b)"), d_[:, :], writes=[t_c])
            s.dma("sp", mu[:, 0:52], mu_d[0, 0:52 * 64].rearrange("(j p) -> p j", p=64), writes=[t_c],
                  allow_slow_non_contiguous=True)
            s.op("dve", lambda e: e.memset(mu[:, 52:53], 0.0), writes=[t_c])
            s.dma("sp", mu[0:32, 52:53], mu_d[0, 52 * 64:RSW].rearrange("(p o) -> p o", o=1), writes=[t_c],
                  allow_slow_non_contiguous=True)
            for tile_, d_ in ((w0, w0_d), (a0, a0_d), (k_k, kk_d), (k_a, ka_d), (r_k, rk_d)):
                s.dma("sp", tile_[:, :], d_[0, :].rearrange("(j p) -> p j", p=64), writes=[t_c],
                      allow_slow_non_contiguous=True)
            s.dma("sp", lgb[:, :], lg_d[0:1, :].partition_broadcast(64), writes=[t_c])
            s.dma("sp", lbb[:, :], lb_d[0:1, :].partition_broadcast(64), writes=[t_c])
            s.dma("sp", ww2[:, :], ww2_d[:, :], writes=[t_c])
            s.dma("sp", wa2[:, :], wa2_d[:, :], writes=[t_c])
            s.dma("sp", wg2[:, 0, :], wg2_d[0:64, :], writes=[t_c])
            s.dma("sp", wg2[:, 1, :], wg2_d[64:128, :], writes=[t_c])
            s.dma("sp", wg2[0:32, 2, :], wg2_d[128:160, :], writes=[t_c])
            t_S0, t_xm = T(), T()
            t_Xs = [T(), T()]; t_Xps = [T(), T()]
            t_txws = [T(), T()]; t_sxgs = [T(), T()]
            t_STs = [T() for _ in range(NG)]
            self.rps = 0

            psv = []
            for i_ in range(8):
                psv.append((ps[i_], t_ps[i_]))
            psh = []
            for i_ in range(8):
                for j_ in range(2):
                    psh.append((ps[i_][:, j_ * 256:(j_ + 1) * 256], T()))
            self.rph = 0

            def nps():
                i = self.rps
                self.rps = (i + 1) % 8
                return psv[i]

            def nph():
                if "halfbank" not in (self.phases or ()):
                    return nps()
                i = self.rph
                self.rph = (i + 1) % 16
                return psh[i]

            def v3g(p_):
                return p_[0:64, 0:HG * 64].rearrange("p (a b) -> p a b", b=64)

            def v3(p_):
                return p_[0:64, :].rearrange("p (a b) -> p a b", b=64)

            def bc(ap2, n):
                return ap2.unsqueeze(2).to_broadcast([64, ap2.shape[1], n])

            def dve(fn, reads, writes):
                s.op("dve", fn, reads, writes)

            def act(fn, reads, writes):
                s.op("act", fn, reads, writes)

            def mm8(lhs, t_l, rhs, t_r, lslice=None):
                p_, t_p = nps()
                for hh in range(8):
                    s.op("pe", lambda e: e.matmul(p_[0:64, hh * 64:(hh + 1) * 64], lhsT=lhs(hh), rhs=rhs(hh),
                                                  start=True, stop=True),
                         reads=t_l + t_r, writes=[t_p])
                return p_, t_p

            def mm8h(lhs, t_l, rhs, t_r):
                p_, t_p = nph()
                for hh in range(HG):
                    s.op("pe", lambda e: e.matmul(p_[0:64, hh * 64:(hh + 1) * 64], lhsT=lhs(hh), rhs=rhs(hh),
                                                  start=True, stop=True),
                         reads=t_l + t_r, writes=[t_p])
                return p_, t_p

            for si, (tok0, Lq, ci) in enumerate(seqs):
                if ci is None:
                    dve(lambda e: e.memset(ST[:, :, :], 0.0), [], t_STs)
                else:
                    s.dma("sp", S0[:, :, :], state_wkv[ci].rearrange("h v k -> v h k"), writes=[t_S0])
                    for g in range(2):
                        p_, t_p = nps()
                        for hh in range(8):
                            s.op("pe", lambda e: e.matmul(p_[0:64, hh * 64:(hh + 1) * 64], lhsT=S0[:, g * 8 + hh, :],
                                                          rhs=ident[0:64, 0:64], start=True, stop=True),
                                 reads=[t_S0, t_c], writes=[t_p])
                        dve(lambda e: e.tensor_copy(out=ST[:, g * 8:g * 8 + 8, :], in_=v3(p_)), [t_p], t_STs)
                def load_x(c):
                    t0 = tok0 + c * C
                    X, t_X = Xs[c % 2], t_Xs[c % 2]
                    for j0 in range(0, 53, 14):
                        j1 = min(53, j0 + 14)
                        if j1 == 53:
                            s.dma("sp", X[:, j0:52, 1:C + 1],
                                  rwT_d[j0:52, :, t0:t0 + C].rearrange("j p t -> p j t"), writes=[t_X])
                            s.dma("sp", X[0:32, 52, 1:C + 1], rwT_d[52, 0:32, t0:t0 + C], writes=[t_X])
                        else:
                            s.dma("sp", X[:, j0:j1, 1:C + 1],
                                  rwT_d[j0:j1, :, t0:t0 + C].rearrange("j p t -> p j t"), writes=[t_X])

                def prologue(c):
                    t0 = tok0 + c * C
                    txw, sxg, t_txw, t_sxg = txws[c % 2], sxgs[c % 2], t_txws[c % 2], t_sxgs[c % 2]
                    X, t_X, t_Xp = Xs[c % 2], t_Xs[c % 2], t_Xps[c % 2]
                    Xo, t_Xo = Xs[(c + 1) % 2], t_Xs[(c + 1) % 2]
                    if c == 0:
                        load_x(0)
                    if c == 0:
                        if ci is None:
                            dve(lambda e: e.memset(X[:, :, 0:1], 0.0), [], [t_Xp])
                        else:
                            dve(lambda e: e.memset(X[:, :, 0:1], 0.0), [], [t_Xp])
                            for j0 in range(0, 52, 13):
                                s.dma("sp", X[:, j0:j0 + 13, 0],
                                      state_shift[ci, j0 * 64:(j0 + 13) * 64].rearrange("(j p) -> p j", p=64),
                                      writes=[t_Xp], allow_slow_non_contiguous=True)
                            s.dma("sp", X[0:32, 52, 0:1], state_shift[ci, 52 * 64:RSW].rearrange("(p o) -> p o", o=1),
                                  writes=[t_Xp], allow_slow_non_contiguous=True)
                    else:
                        dve(lambda e: e.tensor_copy(out=X[:, :, 0:1], in_=Xo[:, :, C:C + 1]), [t_Xo], [t_Xp])
                    if c + 1 < Lq // C:
                        load_x(c + 1)
                    if c == 0 and si == 0:
                        pass
                    for ja, jb in ((0, 14), (14, 28), (28, 42), (42, 53)):
                        nj = jb - ja
                        dve(lambda e: e.tensor_tensor(out=xm[:, ja:jb, :], in0=X[:, ja:jb, 0:C], in1=X[:, ja:jb, 1:C + 1],
                                                      op=ALU.subtract), [t_X, t_Xp], [t_xm])
                        yield
                        dve(lambda e: e.tensor_tensor(out=xm[:, ja:jb, :], in0=xm[:, ja:jb, :],
                                                      in1=mu[:, ja:jb].unsqueeze(2).to_broadcast([64, nj, C]),
                                                      op=ALU.mult), [t_xm, t_c], [t_xm])
                        yield
                        dve(lambda e: e.tensor_tensor(out=xm[:, ja:jb, :], in0=xm[:, ja:jb, :], in1=X[:, ja:jb, 1:C + 1],
                                                      op=ALU.add), [t_xm, t_X], [t_xm])
                        yield
                    act(lambda e: e.activation(out=txw[:, :], in_=xm[:, 48, :], func=AF.Tanh), [t_xm], [t_txw])
                    act(lambda e: e.activation(out=sxg[:, :, :], in_=xm[:, 50:53, :], func=AF.Sigmoid),
                        [t_xm], [t_sxg])

                def body(c, g):
                    t0 = tok0 + c * C
                    txw, sxg, t_txw, t_sxg = txws[c % 2], sxgs[c % 2], t_txws[c % 2], t_sxgs[c % 2]
                    h0 = g * HG
                    A, tt, st8, rwoT = tls[g], tts[g], st8s[g], rwoTs[g]
                    t_ST, t_st8, t_rwoT = t_STs[g], t_st8s[g], t_rwoTs[g]
                    r_ = xm[:, h0:h0 + HG, :]
                    k_ = xm[:, 16 + h0:16 + h0 + HG, :]
                    v_ = xm[:, 32 + h0:32 + h0 + HG, :]
                    yield
                    p_, t_p = mm8h(lambda hh: ww2[:, (h0 + hh) * 64:(h0 + hh + 1) * 64], [t_c],
                                  lambda hh: txw[:, :], [t_txw])
                    for hh in range(HG):
                        act(lambda e: e.activation(out=A["lw"][:, hh, :], in_=p_[0:64, hh * 64:(hh + 1) * 64],
                                                   func=AF.Sigmoid, bias=w0[:, h0 + hh:h0 + hh + 1]),
                            [t_p, t_c], [tt["lw"]])
                    yield
                    p_, t_p = mm8h(lambda hh: wa2[:, (h0 + hh) * 64:(h0 + hh + 1) * 64], [t_c],
                                  lambda hh: xm[:, 49, :], [t_xm])
                    for hh in range(HG):
                        act(lambda e: e.activation(out=A["a"][:, hh, :], in_=p_[0:64, hh * 64:(hh + 1) * 64],
                                                   func=AF.Sigmoid, bias=a0[:, h0 + hh:h0 + hh + 1]),
                            [t_p, t_c], [tt["a"]])
                    dve(lambda e: e.tensor_tensor(out=A["kkn"][:, :, :], in0=k_, in1=bc(k_k[:, h0:h0 + HG], C),
                                                  op=ALU.mult), [t_xm, t_c], [tt["kkn"]])
                    dve(lambda e: e.tensor_tensor(out=A["tmp"][:, :, :], in0=A["kkn"][:, :, :],
                                                  in1=A["kkn"][:, :, :], op=ALU.mult), [tt["kkn"]], [tt["tmp"]])
                    yield
                    yield
                    p_, t_p = nph()
                    s.op("pe", lambda e: e.matmul(p_[0:64, 0:HG * 64], lhsT=ones[:, :],
                                                  rhs=A["tmp"][:, :, :].rearrange("p a b -> p (a b)"),
                                                  start=True, stop=True), [t_c, tt["tmp"]], [t_p])
                    act(lambda e: e.activation(out=A["tmp"][:, :, :], in_=v3g(p_), func=AF.Ln, bias=1e-24),
                        [t_p], [tt["tmp"]])
                    act(lambda e: e.activation(out=A["tmp"][:, :, :], in_=A["tmp"][:, :, :], func=AF.Exp, scale=-0.5),
                        [tt["tmp"]], [tt["tmp"]])
                    yield
                    dve(lambda e: e.tensor_tensor(out=A["kkn"][:, :, :], in0=A["kkn"][:, :, :],
                                                  in1=A["tmp"][:, :, :], op=ALU.mult),
                        [tt["kkn"], tt["tmp"]], [tt["kkn"]])
                    dve(lambda e: e.scalar_tensor_tensor(out=A["tmp"][:, :, :], in0=A["a"][:, :, :], scalar=-1.0,
                                                         in1=bc(k_a[:, h0:h0 + HG], C), op0=ALU.add,
                                                         op1=ALU.mult), [tt["a"], t_c], [tt["tmp"]])
                    yield
                    dve(lambda e: e.scalar_tensor_tensor(out=A["krep"][:, :, :], in0=A["tmp"][:, :, :], scalar=1.0,
                                                         in1=k_, op0=ALU.add, op1=ALU.mult),
                        [tt["tmp"], t_xm], [tt["krep"]])
                    for hh in range(HG):
                        dve(lambda e: e.tensor_tensor_scan(out=A["Lc"][:, hh, :], data0=ones[:, 0:C],
                                                           data1=A["lw"][:, hh, :], initial=0.0,
                                                           op0=ALU.mult, op1=ALU.add),
                            [tt["lw"], t_c], [tt["Lc"]])
                    act(lambda e: e.activation(out=A["Ep"][:, :, :], in_=A["Lc"][:, :, :], func=AF.Exp,
                                               scale=-DECAY_SCALE), [tt["Lc"]], [tt["Ep"]])
                    yield
                    act(lambda e: e.activation(out=A["En"][:, :, :], in_=A["Lc"][:, :, :], func=AF.Exp,
                                               scale=DECAY_SCALE), [tt["Lc"]], [tt["En"]])
                    dve(lambda e: e.tensor_tensor(out=A["rt"][:, :, :], in0=r_, in1=A["Ep"][:, :, :], op=ALU.mult),
                        [t_xm, tt["Ep"]], [tt["rt"]])
                    yield
                    dve(lambda e: e.tensor_tensor(out=A["Lc"][:, :, :], in0=A["Lc"][:, :, :], in1=A["lw"][:, :, :],
                                                  op=ALU.subtract), [tt["Lc"], tt["lw"]], [tt["Lc"]])
                    act(lambda e: e.activation(out=A["Lc"][:, :, :], in_=A["Lc"][:, :, :], func=AF.Exp,
                                               scale=-DECAY_SCALE), [tt["Lc"]], [tt["Lc"]])
                    yield
                    dve(lambda e: e.scalar_tensor_tensor(out=A["Lc"][:, :, :], in0=A["kkn"][:, :, :], scalar=-1.0,
                                                         in1=A["Lc"][:, :, :], op0=ALU.mult, op1=ALU.mult),
                        [tt["kkn"], tt["Lc"]], [tt["Lc"]])
                    at, t_at = A["Lc"], tt["Lc"]
                    dve(lambda e: e.tensor_tensor(out=A["bh"][:, :, :], in0=A["a"][:, :, :], in1=A["kkn"][:, :, :],
                                                  op=ALU.mult), [tt["a"], tt["kkn"]], [tt["bh"]])
                    yield
                    dve(lambda e: e.tensor_tensor(out=A["bh"][:, :, :], in0=A["bh"][:, :, :], in1=A["En"][:, :, :],
                                                  op=ALU.mult), [tt["bh"], tt["En"]], [tt["bh"]])
                    dve(lambda e: e.tensor_tensor(out=A["En"][:, :, :], in0=A["krep"][:, :, :],
                                                  in1=A["En"][:, :, :], op=ALU.mult),
                        [tt["krep"], tt["En"]], [tt["En"]])
                    yield
                    kh, t_kh = A["En"], tt["En"]
                    gC = A["Ep"][:, :, C - 1]
                    dve(lambda e: e.tensor_tensor(out=A["bg"][:, :, :], in0=A["bh"][:, :, :], in1=bc(gC, C),
                                                  op=ALU.mult), [tt["bh"], tt["Ep"]], [tt["bg"]])
                    dve(lambda e: e.tensor_tensor(out=A["kg"][:, :, :], in0=kh[:, :, :], in1=bc(gC, C),
                                                  op=ALU.mult), [t_kh, tt["Ep"]], [tt["kg"]])
                    yield
                    dve(lambda e: e.tensor_tensor(out=A["rkp"][:, :, :], in0=r_, in1=A["krep"][:, :, :],
                                                  op=ALU.mult), [t_xm, tt["krep"]], [tt["rkp"]])
                    dve(lambda e: e.tensor_tensor(out=A["rkp"][:, :, :], in0=A["rkp"][:, :, :],
                                                  in1=bc(r_k[:, h0:h0 + HG], C), op=ALU.mult),
                        [tt["rkp"], t_c], [tt["rkp"]])
                    yield
                    for src, t_src, dst in ((lambda hh: v_[:, hh, :], t_xm, "Vtok"),
                                            (lambda hh: A["bg"][:, hh, :], tt["bg"], "bgT"),
                                            (lambda hh: A["kg"][:, hh, :], tt["kg"], "kgT")):
                        yield
                        p_, t_p = mm8h(src, [t_src], lambda hh: ident[0:64, 0:64], [t_c])
                        act(lambda e: e.activation(out=A[dst][:, :, :], in_=v3g(p_), func=AF.Copy),
                            [t_p], [tt[dst]])
                    yield "PREP_DONE"
                    for lh, t_lh, rh, t_rh, msk, dst in (
                            (A["bh"], tt["bh"], at, t_at, msu, "N"),
                            (at, t_at, A["bh"], tt["bh"], msl, "NT"),
                            (kh, t_kh, at, t_at, msu, "AK"),
                            (kh, t_kh, A["rt"], tt["rt"], mu8, "QK"),
                            (A["bh"], tt["bh"], A["rt"], tt["rt"], mu8, "QB")):
                        yield
                        p_, t_p = mm8h(lambda hh: lh[:, hh, :], [t_lh], lambda hh: rh[:, hh, :], [t_rh])
                        dve(lambda e: e.tensor_tensor(out=A[dst][:, :, :], in0=v3g(p_), in1=msk[:, 0:HG, :],
                                                      op=ALU.mult), [t_p, t_c], [tt[dst]])
                    dve(lambda e: e.tensor_tensor(out=A["X0"][:, :, :], in0=A["N"][:, :, :], in1=i8[:, 0:HG, :],
                                                  op=ALU.add), [tt["N"], t_c], [tt["X0"]])
                    Pn, PTn, Xn = "N", "NT", "X0"
                    for lvl in range(1, 6):
                        Pd, PTd, Xd = ("P0", "PT0", "X1") if lvl % 2 else ("P1", "PT1", "X0")
                        if lvl < 5:
                            yield
                            p_, t_p = mm8h(lambda hh: A[PTn][:, hh, :], [tt[PTn]],
                                          lambda hh: A[Pn][:, hh, :], [tt[Pn]])
                            act(lambda e: e.activation(out=A[Pd][:, :, :], in_=v3g(p_), func=AF.Copy),
                                [t_p], [tt[Pd]])
                        yield
                        p_, t_p = mm8h(lambda hh: A[Pn][:, hh, :], [tt[Pn]],
                                      lambda hh: A[PTn][:, hh, :], [tt[PTn]])
                        act(lambda e: e.activation(out=A[PTd][:, :, :], in_=v3g(p_), func=AF.Copy), [t_p], [tt[PTd]])
                        yield
                        p_, t_p = mm8h(lambda hh: A[PTd][:, hh, :], [tt[PTd]],
                                      lambda hh: A[Xn][:, hh, :], [tt[Xn]])
                        dve(lambda e: e.tensor_tensor(out=A[Xd][:, :, :], in0=v3g(p_), in1=A[Xn][:, :, :],
                                                      op=ALU.add), [t_p, tt[Xn]], [tt[Xd]])
                        Pn, PTn, Xn = Pd, PTd, Xd
                    yield
                    p_, t_p = nph()
                    for hh in range(HG):
                        o_ = p_[0:64, hh * 64:(hh + 1) * 64]
                        s.op("pe", lambda e: e.matmul(o_, lhsT=at[:, hh, :], rhs=ST[:, h0 + hh, :],
                                                      start=True, stop=False), [t_at, t_ST], [t_p])
                        s.op("pe", lambda e: e.matmul(o_, lhsT=A["AK"][:, hh, :], rhs=A["Vtok"][:, hh, :],
                                                      start=False, stop=True), [tt["AK"], tt["Vtok"]], [t_p])
                    act(lambda e: e.activation(out=A["W0"][:, :, :], in_=v3g(p_), func=AF.Copy), [t_p], [tt["W0"]])
                    yield
                    yield
                    p_, t_p = mm8h(lambda hh: A[Xn][:, hh, :], [tt[Xn]], lambda hh: A["W0"][:, hh, :], [tt["W0"]])
                    act(lambda e: e.activation(out=A["U"][:, :, :], in_=v3g(p_), func=AF.Copy), [t_p], [tt["U"]])
                    yield
                    pY, t_pY = nph()
                    for hh in range(HG):
                        o_ = pY[0:64, hh * 64:(hh + 1) * 64]
                        s.op("pe", lambda e: e.matmul(o_, lhsT=A["rt"][:, hh, :], rhs=ST[:, h0 + hh, :],
                                                      start=True, stop=False), [tt["rt"], t_ST], [t_pY])
                        s.op("pe", lambda e: e.matmul(o_, lhsT=A["QK"][:, hh, :], rhs=A["Vtok"][:, hh, :],
                                                      start=False, stop=False), [tt["QK"], tt["Vtok"]], [t_pY])
                        s.op("pe", lambda e: e.matmul(o_, lhsT=A["QB"][:, hh, :], rhs=A["U"][:, hh, :],
                                                      start=False, stop=True), [tt["QB"], tt["U"]], [t_pY])
                    yield
                    pS, t_pS = nph()
                    for hh in range(HG):
                        o_ = pS[0:64, hh * 64:(hh + 1) * 64]
                        s.op("pe", lambda e: e.matmul(o_, lhsT=A["bgT"][:, hh, :], rhs=A["U"][:, hh, :],
                                                      start=True, stop=False), [tt["bgT"], tt["U"]], [t_pS])
                        s.op("pe", lambda e: e.matmul(o_, lhsT=A["kgT"][:, hh, :], rhs=A["Vtok"][:, hh, :],
                                                      start=False, stop=True), [tt["kgT"], tt["Vtok"]], [t_pS])
                    dve(lambda e: e.tensor_tensor(out=ST[:, h0:h0 + HG, :], in0=ST[:, h0:h0 + HG, :], in1=bc(gC, 64),
                                                  op=ALU.mult), [t_ST, tt["Ep"]], [t_ST])
                    yield
                    dve(lambda e: e.tensor_tensor(out=ST[:, h0:h0 + HG, :], in0=ST[:, h0:h0 + HG, :], in1=v3g(pS),
                                                  op=ALU.add), [t_ST, t_pS], [t_ST])
                    dve(lambda e: e.tensor_reduce(out=st8[:, 0:HG], in_=v3g(pY), axis=AX.X, op=ALU.add),
                        [t_pY], [t_st8])
                    yield
                    dve(lambda e: e.tensor_scalar(out=st8[:, 0:HG], in0=st8[:, 0:HG], scalar1=1.0 / 64,
                                                  scalar2=None, op0=ALU.mult), [t_st8], [t_st8])
                    dve(lambda e: e.tensor_tensor(out=A["yc"][:, :, :], in0=v3g(pY), in1=bc(st8[:, 0:HG], 64),
                                                  op=ALU.subtract), [t_pY, t_st8], [tt["yc"]])
                    yield
                    dve(lambda e: e.tensor_tensor(out=A["sq"][:, :, :], in0=A["yc"][:, :, :], in1=A["yc"][:, :, :],
                                                  op=ALU.mult), [tt["yc"]], [tt["sq"]])
                    dve(lambda e: e.tensor_reduce(out=st8[:, 8:8 + HG], in_=A["sq"][:, :, :], axis=AX.X, op=ALU.add),
                        [tt["sq"]], [t_st8])
                    yield
                    act(lambda e: e.activation(out=st8[:, 16:16 + HG], in_=st8[:, 8:8 + HG], func=AF.Sqrt, scale=1.0 / 64,
                                               bias=GN_EPS), [t_st8], [t_st8])
                    dve(lambda e: e.reciprocal(out=st8[:, 24:24 + HG], in_=st8[:, 16:16 + HG]), [t_st8], [t_st8])
                    yield
                    dve(lambda e: e.tensor_tensor(out=A["yc"][:, :, :], in0=A["yc"][:, :, :],
                                                  in1=bc(st8[:, 24:24 + HG], 64), op=ALU.mult),
                        [tt["yc"], t_st8], [tt["yc"]])
                    lg3 = lgb[:, h0 * 64:(h0 + HG) * 64].rearrange("p (a b) -> p a b", b=64)
                    lb3 = lbb[:, h0 * 64:(h0 + HG) * 64].rearrange("p (a b) -> p a b", b=64)
                    dve(lambda e: e.tensor_tensor(out=A["yc"][:, :, :], in0=A["yc"][:, :, :], in1=lg3, op=ALU.mult),
                        [tt["yc"], t_c], [tt["yc"]])
                    yield
                    dve(lambda e: e.tensor_tensor(out=A["yc"][:, :, :], in0=A["yc"][:, :, :], in1=lb3, op=ALU.add),
                        [tt["yc"], t_c], [tt["yc"]])
                    yield
                    pC, t_pC = nph()
                    for hh in range(HG):
                        s.op("pe", lambda e: e.matmul(pC[0:64, hh * 2:hh * 2 + 2], lhsT=A["rkp"][:, hh, :],
                                                      rhs=ones[:, 0:2], start=True, stop=True),
                             [tt["rkp"], t_c], [t_pC])
                    dve(lambda e: e.tensor_copy(out=st8[:, 0:HG], in_=pC[0:64, 0:2 * HG:2]), [t_pC], [t_st8])
                    yield
                    dve(lambda e: e.tensor_tensor(out=A["sq"][:, :, :], in0=A["Vtok"][:, :, :],
                                                  in1=bc(st8[:, 0:HG], 64), op=ALU.mult),
                        [tt["Vtok"], t_st8], [tt["sq"]])
                    dve(lambda e: e.tensor_tensor(out=A["yc"][:, :, :], in0=A["yc"][:, :, :], in1=A["sq"][:, :, :],
                                                  op=ALU.add), [tt["yc"], tt["sq"]], [tt["yc"]])
                    yield
                    yield
                    pG, t_pG = nph()
                    for part, kp in ((0, 64), (1, 64), (2, 32)):
                        s.op("pe", lambda e: e.matmul(pG[0:64, 0:HG * 64], lhsT=sxg[0:kp, part, :],
                                                      rhs=wg2[0:kp, part, h0 * 64:(h0 + HG) * 64],
                                                      start=(part == 0), stop=(part == 2)),
                             [t_sxg, t_c], [t_pG])
                    dve(lambda e: e.tensor_tensor(out=A["rwtok"][:, :, :], in0=v3g(pG), in1=A["yc"][:, :, :],
                                                  op=ALU.mult), [t_pG, tt["yc"]], [tt["rwtok"]])
                    yield
                    pT, t_pT = nph()
                    for jj in range(HG // 2):
                        s.op("pe", lambda e: e.matmul(pT[:, jj * 64:(jj + 1) * 64],
                                                      lhsT=A["rwtok"][:, 2 * jj:2 * jj + 2, :].rearrange(
                                                          "p a b -> p (a b)"),
                                                      rhs=ident[0:64, 0:64], start=True, stop=True),
                             [tt["rwtok"], t_c], [t_pT])
                    act(lambda e: e.activation(out=rwoT[:, :, :].rearrange("p a b -> p (a b)"), in_=pT[:, 0:(HG // 2) * 64],
                                               func=AF.Copy), [t_pT], [t_rwoT])
                    yield
                    s.dma(STQ, rwoT_d[(HG // 2) * g:(HG // 2) * (g + 1), :, t0:t0 + C].rearrange("j p t -> p j t"),
                          rwoT[:, :, :], reads=[t_rwoT])
                order = [(c_, g_) for c_ in range(Lq // C) for g_ in range(NG)]
                active = []
                running = set()
                state = {"oi": 0}
                pro_started, pro_done = set(), set()

                def try_start():
                    oi_ = state["oi"]
                    if oi_ >= len(order):
                        return False
                    c_, g_ = order[oi_]
                    bodies = [e_ for e_ in active if e_[1] != "P"]
                    if g_ == 0 and c_ not in pro_started:
                        if any(not e_[2] for e_ in bodies):
                            return False
                        active.append([prologue(c_), "P", True, c_])
                        pro_started.add(c_)
                        return True
                    if g_ == 0 and c_ not in pro_done:
                        return False
                    if g_ in running:
                        return False
                    if any(not e_[2] for e_ in bodies):
                        return False
                    if len(bodies) >= 2:
                        return False
                    active.append([body(c_, g_), g_, False])
                    running.add(g_)
                    state["oi"] = oi_ + 1
                    return True

                try_start()
                while active:
                    for ent in list(active):
                        try:
                            v_ = next(ent[0])
                            if v_ == "PREP_DONE":
                                ent[2] = True
                        except StopIteration:
                            active.remove(ent)
                            if ent[1] == "P":
                                pro_done.add(ent[3])
                            else:
                                running.discard(ent[1])
                        try_start()
                    if not active:
                        try_start()
                for g in range(2):
                    p_, t_p = nps()
                    for hh in range(8):
                        s.op("pe", lambda e: e.matmul(p_[0:64, hh * 64:(hh + 1) * 64], lhsT=ST[:, g * 8 + hh, :],
                                                      rhs=ident[0:64, 0:64], start=True, stop=True),
                             t_STs + [t_c], [t_p])
                    dve(lambda e: e.tensor_copy(out=S0[:, g * 8:g * 8 + 8, :], in_=v3(p_)), [t_p], [t_S0])
                s.dma(STQ, wkv_out[si].rearrange("h v k -> v h k"), S0[:, :, :], reads=[t_S0])
            s.barrier()


    CAPB = 4
    CAPR = 512
    NSLOT = NEXP * 512
    BIG = 1.0e6

    def breg(self):
        if not hasattr(self, "_breg"):
            self._breg = self.nc.gpsimd.to_reg(self.NSLOT - 1)
        return self._breg

    def groups(self):
        L, LS = self.L, self.LS
        gs = []
        for sq in range(2):
            for g0 in range(0, L, 512):
                gs.append((sq * L + g0, min(512, L - g0)))
        gs.append((2 * L, 2 * LS))
        return gs

    def phase_merge(self):
        nc, s = self.nc, self.s
        L, LS, PAST, TT = self.L, self.LS, self.PAST, self.TT
        wfu_d = self.din("w_fox_up", [FW, D])
        wru_d = self.din("w_rwkv_up", [RW, D])
        wo_d = self.din("w_o", [D, D])
        gffn_d = self.din("g_ffn", [1, D])
        wrg_d = self.din("w_rg", [D, 4])
        brg_d = self.din("b_rg", [1, 4])
        wre_d = self.din("w_re", [D, NEXP])
        bre_d = self.din("b_re", [1, NEXP])
        slt_d = self.din("slt", [128, 128])
        ebase_d = self.din("ebase", [128, NEXP])
        ones_d = self.cin("ones", [128, 128])
        x2_d = self.x2_d = self.dscr("x2_d", [TT, D])
        xs_d = self.xs_d = self.dscr("xs_d", [self.NSLOT, D])
        if "dbgmerge" in (self.phases or ()):
            x2_d = self.x2_d = self.dout("dbg_x2", [TT, D])
            self.dbg_gates = self.dout("dbg_gates", [128, TT // 128, 2])
            self.dbg_dest = self.dout("dbg_dest", [128, TT // 128, 2], I32)
            xs_d = self.xs_d = self.dout("dbg_xs", [self.NSLOT, D])
        NTt = TT // 128
        self.gates_all = nc.alloc_sbuf_tensor("gates_all", [128, NTt, 2], F32)
        self.dest_all = nc.alloc_sbuf_tensor("dest_all", [128, NTt, 2], I32)
        self.t_route = T("route")
        gates_all, dest_all, t_route = self.gates_all, self.dest_all, self.t_route
        with ExitStack() as es:
            def sb(name, shape, dt=F32):
                return es.enter_context(nc.sbuf_tensor("m_" + name, shape, dt))
            ident = sb("ident", [128, 128]); slt = sb("slt", [128, 128]); onesf = sb("onesf", [128, 128])
            ebase = sb("ebase", [128, NEXP]); gfbc = sb("gfbc", [128, D]); brbc = sb("brbc", [128, 36])
            wr = sb("wr", [128, 16, 36]); cnt = sb("cnt", [128, NEXP])
            foxT = sb("foxT", [128, 8, 512], F32R); rwoT = sb("rwoT", [128, 8, 512], F32R)
            wfu = [sb(f"wfu{i}", [128, 8, 128], F32R) for i in range(2)]
            wru = [sb(f"wru{i}", [128, 8, 128], F32R) for i in range(2)]
            sgf = [sb(f"sgf{i}", [128, 512]) for i in range(2)]
            sgr = [sb(f"sgr{i}", [128, 512]) for i in range(2)]
            tmpA = sb("tmpA", [128, 512]); tmpB = sb("tmpB", [128, 512])
            mT = sb("mT", [128, 16, 512], F32R)
            X2 = sb("X2", [128, 4, D])
            wo = [sb(f"wo{i}", [128, 16, 256], F32R) for i in range(2)]
            H2s = [sb(f"H2{i}", [128, D]) for i in range(2)]; h2T = sb("h2T", [128, 16, 128])
            junk = h2T[:, :, :].rearrange("p a b -> p (a b)")
            lg = sb("lg", [128, 36]); G = sb("G", [128, 4]); pen = sb("pen", [128, 4]); ge = sb("ge", [128, 4])
            ml = sb("ml", [128, NEXP]); ml2 = sb("ml2", [128, NEXP]); oh0 = sb("oh0", [128, NEXP])
            oh1 = sb("oh1", [128, NEXP]); oh = sb("oh", [128, NEXP]); pos = sb("pos", [128, NEXP])
            t32 = sb("t32", [128, NEXP]); m = sb("m", [128, 32])
            ps, t_ps = self.ps, self.t_ps
            t_c = T("mconsts")
            s.dma("sp", ident[:], self.ident_d[:, :], writes=[t_c])
            s.dma("sp", slt[:], slt_d[:, :], writes=[t_c])
            s.dma("sp", onesf[:], ones_d[:, :], writes=[t_c])
            s.dma("sp", ebase[:], ebase_d[:, :], writes=[t_c])
            s.dma("sp", gfbc[:], gffn_d[0:1, :].partition_broadcast(128), writes=[t_c])
            s.dma("sp", brbc[:, 0:4], brg_d[0:1, :].partition_broadcast(128), writes=[t_c])
            s.dma("sp", brbc[:, 4:36], bre_d[0:1, :].partition_broadcast(128), writes=[t_c])
            s.dma("sp", wr[:, :, 0:4], wrg_d[:, :].rearrange("(kc p) g -> p kc g", p=128), writes=[t_c])
            s.dma("sp", wr[:, :, 4:36], wre_d[:, :].rearrange("(kc p) g -> p kc g", p=128), writes=[t_c])
            t_cnt = T()
            s.op("dve", lambda e: e.memset(cnt[:, :], 0.0), writes=[t_cnt])
            t_foxT, t_rwoT, t_tmpA, t_tmpB, t_mT, t_X2, t_h2T, t_r = (T() for _ in range(8))
            t_H2s = [T(), T()]
            t_junk = t_h2T
            t_wfu = [T(), T()]; t_wru = [T(), T()]; t_sgf = [T(), T()]; t_sgr = [T(), T()]; t_wo = [T(), T()]
            self.mps = 0

            def nps():
                i = self.mps
                self.mps = (i + 1) % 8
                return ps[i], t_ps[i]
            wi = 0
            woi = 0
            for (tok0, ntok) in self.groups():
                ntile = ntok // 128
                s.dma("sp", foxT[:, :, 0:ntok], r32(self.foxT_d[:, :, tok0:tok0 + ntok]).rearrange("h p t -> p h t"),
                      writes=[t_foxT])
                s.dma("sp", rwoT[:, :, 0:ntok], r32(self.rwoT_d[:, :, tok0:tok0 + ntok]).rearrange("h p t -> p h t"),
                      writes=[t_rwoT])
                s.dma("sp", X2[:, 0:ntile, :], self.x_all[tok0:tok0 + ntok, :].rearrange("(a p) c -> p a c", p=128),
                      writes=[t_X2])
                for cb in range(16):
                    b = wi % 2
                    wi += 1
                    s.dma("sp", wfu[b][:, :, :], r32(wfu_d[:, cb * 128:(cb + 1) * 128]).rearrange(
                        "(kc p) c -> p kc c", p=128), writes=[t_wfu[b]])
                    s.dma("sp", wru[b][:, :, :], r32(wru_d[:, cb * 128:(cb + 1) * 128]).rearrange(
                        "(kc p) c -> p kc c", p=128), writes=[t_wru[b]])
                    s.dma("sp", sgf[b][:, 0:ntok], self.sgT_d[cb, :, tok0:tok0 + ntok], writes=[t_sgf[b]])
                    s.dma("sp", sgr[b][:, 0:ntok], self.sgT_d[16 + cb, :, tok0:tok0 + ntok], writes=[t_sgr[b]])
                    pF, t_pF = nps()
                    for kc in range(8):
                        s.op("pe", lambda e: e.matmul(pF[:, 0:ntok], lhsT=wfu[b][:, kc, :], rhs=foxT[:, kc, 0:ntok],
                                                      start=(kc == 0), stop=(kc == 7)),
                             [t_wfu[b], t_foxT], [t_pF])
                    pR, t_pR = nps()
                    for kc in range(8):
                        s.op("pe", lambda e: e.matmul(pR[:, 0:ntok], lhsT=wru[b][:, kc, :], rhs=rwoT[:, kc, 0:ntok],
                                                      start=(kc == 0), stop=(kc == 7)),
                             [t_wru[b], t_rwoT], [t_pR])
                    s.op("dve", lambda e: e.tensor_tensor(out=tmpA[:, 0:ntok], in0=pF[:, 0:ntok], in1=sgf[b][:, 0:ntok],
                                                          op=ALU.mult), [t_pF, t_sgf[b]], [t_tmpA])
                    s.op("dve", lambda e: e.tensor_tensor(out=tmpB[:, 0:ntok], in0=pR[:, 0:ntok], in1=sgr[b][:, 0:ntok],
                                                          op=ALU.mult), [t_pR, t_sgr[b]], [t_tmpB])
                    s.op("dve", lambda e: e.tensor_tensor(out=mT[:, cb, 0:ntok], in0=tmpA[:, 0:ntok],
                                                          in1=tmpB[:, 0:ntok], op=ALU.add),
                         [t_tmpA, t_tmpB], [t_mT])
                for cs in range(8):
                    b = woi % 2
                    woi += 1
                    s.dma("sp", wo[b][:, :, :], r32(wo_d[:, cs * 256:(cs + 1) * 256]).rearrange(
                        "(kc p) c -> p kc c", p=128), writes=[t_wo[b]])
                    for ti in range(ntile):
                        p_, t_p = nps()
                        for kc in range(16):
                            s.op("pe", lambda e: e.matmul(p_[:, 0:256], lhsT=mT[:, kc, ti * 128:(ti + 1) * 128],
                                                          rhs=wo[b][:, kc, :], start=(kc == 0), stop=(kc == 15)),
                                 [t_mT, t_wo[b]], [t_p])
                        s.op("dve", lambda e: e.tensor_tensor(out=X2[:, ti, cs * 256:(cs + 1) * 256], in0=p_[:, 0:256],
                                                              in1=X2[:, ti, cs * 256:(cs + 1) * 256], op=ALU.add),
                             [t_p, t_X2], [t_X2])
                for ti in range(ntile):
                    tti = (tok0 + ti * 128) // 128
                    r0 = tok0 + ti * 128
                    H2, t_H2 = H2s[tti % 2], t_H2s[tti % 2]
                    s.dma(STQ, x2_d[r0:r0 + 128, :], X2[:, ti, :], reads=[t_X2])
                    s.op("act", lambda e: e.activation(out=junk, in_=X2[:, ti, :], func=AF.Square,
                                                       accum_out=m[:, 16:17]), [t_X2], [t_junk, t_r])
                    s.op("act", lambda e: e.activation(out=m[:, 17:18], in_=m[:, 16:17], func=AF.Sqrt,
                                                       scale=1.0 / D, bias=NORM_EPS), [t_r], [t_r])
                    s.op("dve", lambda e: e.reciprocal(out=m[:, 18:19], in_=m[:, 17:18]), [t_r], [t_r])
                    s.op("dve", lambda e: e.scalar_tensor_tensor(out=H2[:, :], in0=X2[:, ti, :], scalar=m[:, 18:19],
                                                                 in1=gfbc[:, :], op0=ALU.mult, op1=ALU.mult),
                         [t_X2, t_r, t_c], [t_H2])
                    for k4 in range(4):
                        p_, t_p = nps()
                        for j in range(4):
                            kc = k4 * 4 + j
                            s.op("pe", lambda e: e.transpose(out=p_[:, j * 128:(j + 1) * 128],
                                                             in_=H2[:, kc * 128:(kc + 1) * 128], identity=ident[:, :]),
                                 [t_H2, t_c], [t_p])
                        s.op("act", lambda e: e.activation(out=h2T[:, k4 * 4:k4 * 4 + 4, :].rearrange("p a b -> p (a b)"),
                                                           in_=p_[:, :], func=AF.Copy), [t_p], [t_h2T])
                    pL, t_pL = nps()
                    for kc in range(16):
                        s.op("pe", lambda e: e.matmul(pL[:, 0:36], lhsT=h2T[:, kc, :], rhs=wr[:, kc, :],
                                                      start=(kc == 0), stop=(kc == 15)), [t_h2T, t_c], [t_pL])
                    dv = lambda fn, rd, wrt: s.op("dve", fn, rd, wrt)
                    dv(lambda e: e.tensor_tensor(out=lg[:, :], in0=pL[:, 0:36], in1=brbc[:, :], op=ALU.add),
                       [t_pL, t_c], [t_r])
                    dv(lambda e: e.tensor_reduce(out=m[:, 0:1], in_=lg[:, 0:4], axis=AX.X, op=ALU.max), [t_r], [t_r])
                    dv(lambda e: e.tensor_scalar(out=G[:, :], in0=lg[:, 0:4], scalar1=m[:, 0:1], scalar2=None,
                                                 op0=ALU.is_equal), [t_r], [t_r])
                    dv(lambda e: e.tensor_scalar(out=m[:, 1:2], in0=m[:, 0:1], scalar1=-1.0, scalar2=None,
                                                 op0=ALU.mult), [t_r], [t_r])
                    s.op("act", lambda e: e.activation(out=ge[:, :], in_=lg[:, 0:4], func=AF.Exp, bias=m[:, 1:2],
                                                       accum_out=m[:, 2:3]), [t_r], [t_r])
                    dv(lambda e: e.reciprocal(out=m[:, 3:4], in_=m[:, 2:3]), [t_r], [t_r])
                    dv(lambda e: e.tensor_scalar(out=pen[:, :], in0=G[:, :], scalar1=-1.0, scalar2=1.0e30,
                                                 op0=ALU.add, op1=ALU.mult), [t_r], [t_r])
                    dv(lambda e: e.tensor_tensor(out=ml[:, :].rearrange("p (a b) -> p a b", b=8),
                                                 in0=lg[:, 4:36].rearrange("p (a b) -> p a b", b=8),
                                                 in1=pen[:, :].unsqueeze(2).to_broadcast([128, 4, 8]), op=ALU.add),
                       [t_r], [t_r])
                    dv(lambda e: e.tensor_reduce(out=m[:, 4:5], in_=ml[:, :], axis=AX.X, op=ALU.max), [t_r], [t_r])
                    dv(lambda e: e.tensor_scalar(out=oh0[:, :], in0=ml[:, :], scalar1=m[:, 4:5], scalar2=None,
                                                 op0=ALU.is_equal), [t_r], [t_r])
                    dv(lambda e: e.scalar_tensor_tensor(out=ml2[:, :], in0=oh0[:, :], scalar=-1.0e30, in1=ml[:, :],
                                                        op0=ALU.mult, op1=ALU.add), [t_r], [t_r])
                    dv(lambda e: e.tensor_reduce(out=m[:, 5:6], in_=ml2[:, :], axis=AX.X, op=ALU.max), [t_r], [t_r])
                    dv(lambda e: e.tensor_scalar(out=oh1[:, :], in0=ml2[:, :], scalar1=m[:, 5:6], scalar2=None,
                                                 op0=ALU.is_equal), [t_r], [t_r])
                    dv(lambda e: e.tensor_tensor(out=m[:, 6:7], in0=m[:, 5:6], in1=m[:, 4:5], op=ALU.subtract),
                       [t_r], [t_r])
                    s.op("act", lambda e: e.activation(out=m[:, 7:8], in_=m[:, 6:7], func=AF.Exp), [t_r], [t_r])
                    dv(lambda e: e.tensor_scalar(out=m[:, 8:9], in0=m[:, 7:8], scalar1=1.0, scalar2=None,
                                                 op0=ALU.add), [t_r], [t_r])
                    dv(lambda e: e.reciprocal(out=m[:, 9:10], in_=m[:, 8:9]), [t_r], [t_r])
                    dv(lambda e: e.tensor_tensor(out=gates_all[:, tti, 0:1], in0=m[:, 3:4], in1=m[:, 9:10],
                                                 op=ALU.mult), [t_r], [t_route])
                    dv(lambda e: e.tensor_tensor(out=gates_all[:, tti, 1:2], in0=gates_all[:, tti, 0:1],
                                                 in1=m[:, 7:8], op=ALU.mult), [t_r, t_route], [t_route])
                    dv(lambda e: e.tensor_tensor(out=oh[:, :], in0=oh0[:, :], in1=oh1[:, :], op=ALU.add), [t_r], [t_r])
                    pP, t_pP = nps()
                    s.op("pe", lambda e: e.matmul(pP[:, 0:NEXP], lhsT=slt[:, :], rhs=oh[:, :], start=True, stop=True),
                         [t_c, t_r], [t_pP])
                    s.op("pe", lambda e: e.matmul(pP[:, 64:64 + NEXP], lhsT=onesf[:, :], rhs=oh[:, :],
                                                  start=True, stop=True), [t_c, t_r], [t_pP])
                    dv(lambda e: e.tensor_tensor(out=pos[:, :], in0=pP[:, 0:NEXP], in1=cnt[:, :], op=ALU.add),
                       [t_pP, t_cnt], [t_r])
                    dv(lambda e: e.tensor_tensor(out=cnt[:, :], in0=pP[:, 64:64 + NEXP], in1=cnt[:, :], op=ALU.add),
                       [t_pP, t_cnt, t_r], [t_cnt])
                    for k, ohk in ((0, oh0), (1, oh1)):
                        dv(lambda e: e.tensor_tensor(out=t32[:, :], in0=ohk[:, :], in1=pos[:, :], op=ALU.mult),
                           [t_r], [t_r])
                        dv(lambda e: e.tensor_reduce(out=m[:, 10 + k:11 + k], in_=t32[:, :], axis=AX.X, op=ALU.add),
                           [t_r], [t_r])
                        dv(lambda e: e.tensor_tensor(out=t32[:, :], in0=ohk[:, :], in1=ebase[:, :], op=ALU.mult),
                           [t_r, t_c], [t_r])
                        dv(lambda e: e.tensor_reduce(out=m[:, 12 + k:13 + k], in_=t32[:, :], axis=AX.X, op=ALU.add),
                           [t_r], [t_r])
                    dv(lambda e: e.tensor_scalar(out=m[:, 14:16], in0=m[:, 10:12], scalar1=float(self.CAPR),
                                                 scalar2=None, op0=ALU.is_lt), [t_r], [t_r])
                    dv(lambda e: e.tensor_tensor(out=m[:, 20:22], in0=m[:, 10:12], in1=m[:, 12:14], op=ALU.add),
                       [t_r], [t_r])
                    dv(lambda e: e.tensor_tensor(out=m[:, 20:22], in0=m[:, 20:22], in1=m[:, 14:16], op=ALU.mult),
                       [t_r], [t_r])
                    dv(lambda e: e.tensor_scalar(out=m[:, 22:24], in0=m[:, 14:16], scalar1=-1.0, scalar2=-self.BIG,
                                                 op0=ALU.add, op1=ALU.mult), [t_r], [t_r])
                    dv(lambda e: e.tensor_tensor(out=m[:, 20:22], in0=m[:, 20:22], in1=m[:, 22:24], op=ALU.add),
                       [t_r], [t_r])
                    dv(lambda e: e.tensor_copy(out=dest_all[:, tti, :], in_=m[:, 20:22]), [t_r], [t_route])
                    for k in range(2):
                        s.dma("pool", None, None, reads=[t_H2, t_route], writes=[],
                              fn=lambda e: e.indirect_dma_start(
                                  out=xs_d[:, :],
                                  out_offset=bass.IndirectOffsetOnAxis(ap=dest_all[:, tti, k:k + 1], axis=0),
                                  in_=H2[:, :], in_offset=None, bounds_check=self.breg(), oob_is_err=False))
            if "dbgmerge" in (self.phases or ()):
                s.dma("sp", self.dbg_gates[:, :, :], gates_all[:, :, :], reads=[t_route])
                s.dma("sp", self.dbg_dest[:, :, :], dest_all[:, :, :], reads=[t_route])
            s.barrier()

    def phase_experts(self):
        nc, s = self.nc, self.s
        w1_d = self.din("w1", [NEXP, D, DE])
        w3_d = self.din("w3", [NEXP, D, DE])
        w2_d = self.din("w2", [NEXP, DE, D])
        ys_d = self.ys_d = self.dscr("ys_d", [self.NSLOT, D])
        xs_d = self.xs_d
        CAPB, CAPR = self.CAPB, self.CAPR
        with ExitStack() as es:
            def sb(name, shape, dt=F32):
                return es.enter_context(nc.sbuf_tensor("e_" + name, shape, dt))
            ident = sb("ident", [128, 128])
            xr = [sb(f"xr{i}", [128, D]) for i in range(2)]
            xT = sb("xT", [128, 16, CAPR], F32R)
            w1p = [sb(f"w1p{i}", [128, 16, 128], F32R) for i in range(3)]
            w3p = [sb(f"w3p{i}", [128, 16, 128], F32R) for i in range(3)]
            w2p = [sb(f"w2p{i}", [128, 4, 512], F32R) for i in range(2)]
            sil = sb("sil", [128, CAPR])
            GT = sb("GT", [128, 4, CAPR], F32R)
            yst = [sb(f"yst{i}", [128, 512]) for i in range(2)]
            ps, t_ps = self.ps, self.t_ps
            t_c = T()
            s.dma("sp", ident[:], self.ident_d[:, :], writes=[t_c])
            t_xr = [T(), T()]; t_w1 = [T(), T(), T()]; t_w3 = [T(), T(), T()]; t_w2 = [T(), T()]; t_yst = [T(), T()]
            t_xT, t_sil, t_GT = T(), T(), T()
            self.eps = 0

            def nps():
                i = self.eps
                self.eps = (i + 1) % 8
                return ps[i], t_ps[i]
            xi = wi = w2i = yi = 0
            ev = 0
            for e_ in range(NEXP):
                base = e_ * CAPR
                for blk in range(CAPB):
                    b = xi % 2
                    xi += 1
                    s.dma("sp", xr[b][:, :], xs_d[base + blk * 128:base + (blk + 1) * 128, :], writes=[t_xr[b]])
                    for k4 in range(4):
                        p_, t_p = nps()
                        for j in range(4):
                            kc = k4 * 4 + j
                            s.op("pe", lambda e: e.transpose(out=p_[:, j * 128:(j + 1) * 128],
                                                             in_=xr[b][:, kc * 128:(kc + 1) * 128],
                                                             identity=ident[:, :]), [t_xr[b], t_c], [t_p])
                        ev += 1
                        o_ = xT[:, k4 * 4:k4 * 4 + 4, blk * 128:(blk + 1) * 128]
                        i_ = p_[:, :].rearrange("p (a b) -> p a b", b=128)
                        if ev % 2:
                            s.op("act", lambda e: e.activation(out=o_, in_=i_, func=AF.Copy), [t_p], [t_xT])
                        else:
                            s.op("dve", lambda e: e.tensor_copy(out=o_, in_=i_), [t_p], [t_xT])
                for fc in range(4):
                    b = wi % 3
                    wi += 1
                    s.dma("sp", w1p[b][:, :, :], r32(w1_d[e_, :, fc * 128:(fc + 1) * 128]).rearrange(
                        "(kc p) f -> p kc f", p=128), writes=[t_w1[b]])
                    s.dma("sp", w3p[b][:, :, :], r32(w3_d[e_, :, fc * 128:(fc + 1) * 128]).rearrange(
                        "(kc p) f -> p kc f", p=128), writes=[t_w3[b]])
                    p1, t_p1 = nps()
                    for kc in range(16):
                        s.op("pe", lambda e: e.matmul(p1[:, :], lhsT=w1p[b][:, kc, :], rhs=xT[:, kc, :],
                                                      start=(kc == 0), stop=(kc == 15)), [t_w1[b], t_xT], [t_p1])
                    p3, t_p3 = nps()
                    for kc in range(16):
                        s.op("pe", lambda e: e.matmul(p3[:, :], lhsT=w3p[b][:, kc, :], rhs=xT[:, kc, :],
                                                      start=(kc == 0), stop=(kc == 15)), [t_w3[b], t_xT], [t_p3])
                    s.op("act", lambda e: e.activation(out=sil[:, :], in_=p1[:, :], func=AF.Silu), [t_p1], [t_sil])
                    s.op("dve", lambda e: e.tensor_tensor(out=GT[:, fc, :], in0=p3[:, :], in1=sil[:, :], op=ALU.mult),
                         [t_p3, t_sil], [t_GT])
                for cs in range(4):
                    b = w2i % 2
                    w2i += 1
                    s.dma("sp", w2p[b][:, :, :], r32(w2_d[e_, :, cs * 512:(cs + 1) * 512]).rearrange(
                        "(fc p) c -> p fc c", p=128), writes=[t_w2[b]])
                    for blk in range(CAPB):
                        p_, t_p = nps()
                        for fc in range(4):
                            s.op("pe", lambda e: e.matmul(p_[:, :], lhsT=GT[:, fc, blk * 128:(blk + 1) * 128],
                                                          rhs=w2p[b][:, fc, :], start=(fc == 0), stop=(fc == 3)),
                                 [t_GT, t_w2[b]], [t_p])
                        yb = yi % 2
                        yi += 1
                        ev += 1
                        if ev % 2:
                            s.op("act", lambda e: e.activation(out=yst[yb][:, :], in_=p_[:, :], func=AF.Copy),
                                 [t_p], [t_yst[yb]])
                        else:
                            s.op("dve", lambda e: e.tensor_copy(out=yst[yb][:, :], in_=p_[:, :]), [t_p], [t_yst[yb]])
                        s.dma(STQ, ys_d[base + blk * 128:base + (blk + 1) * 128, cs * 512:(cs + 1) * 512],
                              yst[yb][:, :], reads=[t_yst[yb]])
            s.barrier()

    def phase_final(self):
        nc, s = self.nc, self.s
        L, LS, PAST, TT = self.L, self.LS, self.PAST, self.TT
        p_all = self.din("p_all", [TT, PLE])
        wple_d = self.din("w_ple", [PLE, D])
        wpg_d = self.din("w_pg", [D, D])
        bpg_d = self.din("b_pg", [1, D])
        gfin_d = self.din("g_final", [1, D])
        y_all = self.dout("y_all", [TT, D])
        gates_all, dest_all, t_route = self.gates_all, self.dest_all, self.t_route
        ys_d, x2_d = self.ys_d, self.x2_d
        with ExitStack() as es:
            def sb(name, shape, dt=F32):
                return es.enter_context(nc.sbuf_tensor("f_" + name, shape, dt))
            ident = sb("ident", [128, 128])
            bpbc = sb("bpbc", [128, D]); gfbc = sb("gfbc", [128, D])
            X3 = sb("X3", [128, 4, D])
            y0s = [sb(f"y0{i}", [128, D]) for i in range(2)]; y1s = [sb(f"y1{i}", [128, D]) for i in range(2)]
            x3T = sb("x3T", [128, 16, 512], F32R)
            pts = [sb(f"pt{i}", [128, PLE]) for i in range(2)]; pT = sb("pT", [128, 2, 512], F32R)
            wpg = [sb(f"wpg{i}", [128, 16, 256], F32R) for i in range(2)]
            wpl = [sb(f"wpl{i}", [128, 2, 256], F32R) for i in range(2)]
            gt = sb("gt", [128, 256]); junk = sb("junk", [128, D]); m = sb("m", [128, 8])
            ps, t_ps = self.ps, self.t_ps
            t_c = T()
            s.dma("sp", ident[:], self.ident_d[:, :], writes=[t_c])
            s.dma("sp", bpbc[:], bpg_d[0:1, :].partition_broadcast(128), writes=[t_c])
            s.dma("sp", gfbc[:], gfin_d[0:1, :].partition_broadcast(128), writes=[t_c])
            t_X3, t_x3T, t_pT, t_gt, t_junk, t_m = (T() for _ in range(6))
            t_y0s = [T(), T()]; t_y1s = [T(), T()]; t_pts = [T(), T()]
            t_wpg = [T(), T()]; t_wpl = [T(), T()]
            self.fps = 0

            def nps():
                i = self.fps
                self.fps = (i + 1) % 8
                return ps[i], t_ps[i]
            wi = 0
            for (tok0, ntok) in self.groups():
                ntile = ntok // 128
                for ti in range(ntile):
                    r0 = tok0 + ti * 128
                    tti = r0 // 128
                    y0, y1, pt = y0s[tti % 2], y1s[tti % 2], pts[tti % 2]
                    t_y0, t_y1, t_pt = t_y0s[tti % 2], t_y1s[tti % 2], t_pts[tti % 2]
                    s.dma("sp", X3[:, ti, :], x2_d[r0:r0 + 128, :], writes=[t_X3])
                    s.dma("sp", pt[:, :], p_all[r0:r0 + 128, :], writes=[t_pt])
                    s.op("dve", lambda e: e.memset(y0[:, :], 0.0), [], [t_y0])
                    s.op("dve", lambda e: e.memset(y1[:, :], 0.0), [], [t_y1])
                    for k, (yk, t_yk) in enumerate(((y0, t_y0), (y1, t_y1))):
                        s.dma("pool", None, None, reads=[t_route], writes=[t_yk],
                              fn=lambda e: e.indirect_dma_start(
                                  out=yk[:, :], out_offset=None, in_=ys_d[:, :],
                                  in_offset=bass.IndirectOffsetOnAxis(ap=dest_all[:, tti, k:k + 1], axis=0),
                                  bounds_check=self.breg(), oob_is_err=False))
                        s.op("dve", lambda e: e.scalar_tensor_tensor(out=X3[:, ti, :], in0=yk[:, :],
                                                                     scalar=gates_all[:, tti, k:k + 1],
                                                                     in1=X3[:, ti, :], op0=ALU.mult, op1=ALU.add),
                             [t_yk, t_route, t_X3], [t_X3])
                    for k4 in range(4):
                        p_, t_p = nps()
                        for j in range(4):
                            kc = k4 * 4 + j
                            s.op("pe", lambda e: e.transpose(out=p_[:, j * 128:(j + 1) * 128],
                                                             in_=X3[:, ti, kc * 128:(kc + 1) * 128],
                                                             identity=ident[:, :]), [t_X3, t_c], [t_p])
                        s.op("act", lambda e: e.activation(out=x3T[:, k4 * 4:k4 * 4 + 4, ti * 128:(ti + 1) * 128],
                                                           in_=p_[:, :].rearrange("p (a b) -> p a b", b=128),
                                                           func=AF.Copy), [t_p], [t_x3T])
                    p_, t_p = nps()
                    for j in range(2):
                        s.op("pe", lambda e: e.transpose(out=p_[:, j * 128:(j + 1) * 128],
                                                         in_=pt[:, j * 128:(j + 1) * 128], identity=ident[:, :]),
                             [t_pt, t_c], [t_p])
                    s.op("dve", lambda e: e.tensor_copy(out=pT[:, :, ti * 128:(ti + 1) * 128],
                                                        in_=p_[:, 0:256].rearrange("p (a b) -> p a b", b=128)),
                         [t_p], [t_pT])
                for cs in range(8):
                    b = wi % 2
                    wi += 1
                    s.dma("sp", wpg[b][:, :, :], r32(wpg_d[:, cs * 256:(cs + 1) * 256]).rearrange(
                        "(kc p) c -> p kc c", p=128), writes=[t_wpg[b]])
                    s.dma("sp", wpl[b][:, :, :], r32(wple_d[:, cs * 256:(cs + 1) * 256]).rearrange(
                        "(kc p) c -> p kc c", p=128), writes=[t_wpl[b]])
                    for ti in range(ntile):
                        p1, t_p1 = nps()
                        for kc in range(16):
                            s.op("pe", lambda e: e.matmul(p1[:, 0:256], lhsT=x3T[:, kc, ti * 128:(ti + 1) * 128],
                                                          rhs=wpg[b][:, kc, :], start=(kc == 0), stop=(kc == 15)),
                                 [t_x3T, t_wpg[b]], [t_p1])
                        p2, t_p2 = nps()
                        for kc in range(2):
                            s.op("pe", lambda e: e.matmul(p2[:, 0:256], lhsT=pT[:, kc, ti * 128:(ti + 1) * 128],
                                                          rhs=wpl[b][:, kc, :], start=(kc == 0), stop=(kc == 1)),
                                 [t_pT, t_wpl[b]], [t_p2])
                        s.op("dve", lambda e: e.tensor_tensor(out=gt[:, :], in0=p1[:, 0:256],
                                                              in1=bpbc[:, cs * 256:(cs + 1) * 256], op=ALU.add),
                             [t_p1, t_c], [t_gt])
                        s.op("act", lambda e: e.activation(out=gt[:, :], in_=gt[:, :], func=AF.Sigmoid),
                             [t_gt], [t_gt])
                        s.op("dve", lambda e: e.tensor_tensor(out=gt[:, :], in0=p2[:, 0:256], in1=gt[:, :],
                                                              op=ALU.mult), [t_p2, t_gt], [t_gt])
                        s.op("dve", lambda e: e.tensor_tensor(out=X3[:, ti, cs * 256:(cs + 1) * 256], in0=gt[:, :],
                                                              in1=X3[:, ti, cs * 256:(cs + 1) * 256], op=ALU.add),
                             [t_gt, t_X3], [t_X3])
                for ti in range(ntile):
                    r0 = tok0 + ti * 128
                    s.op("act", lambda e: e.activation(out=junk[:, :], in_=X3[:, ti, :], func=AF.Square,
                                                       accum_out=m[:, 0:1]), [t_X3], [t_junk, t_m])
                    s.op("act", lambda e: e.activation(out=m[:, 1:2], in_=m[:, 0:1], func=AF.Sqrt,
                                                       scale=1.0 / D, bias=NORM_EPS), [t_m], [t_m])
                    s.op("dve", lambda e: e.reciprocal(out=m[:, 2:3], in_=m[:, 1:2]), [t_m], [t_m])
                    s.op("dve", lambda e: e.scalar_tensor_tensor(out=junk[:, :], in0=X3[:, ti, :], scalar=m[:, 2:3],
                                                                 in1=gfbc[:, :], op0=ALU.mult, op1=ALU.mult),
                         [t_X3, t_m, t_c, t_junk], [t_junk])
                    s.dma(STQ, y_all[r0:r0 + 128, :], junk[:, :], reads=[t_junk])
            s.barrier()


_CACHE = {}


def _consts():
    return {"ident": np.eye(128, dtype=np.float32),
            "triu": np.triu(np.ones((128, 128), dtype=np.float32)),
            "ones": np.ones((128, 128), dtype=np.float32),
            "m_su": np.tile(np.triu(np.ones((64, 64), np.float32), 1), (1, 8)),
            "m_sl": np.tile(np.tril(np.ones((64, 64), np.float32), -1), (1, 8)),
            "m_u": np.tile(np.triu(np.ones((64, 64), np.float32), 0), (1, 8)),
            "i8": np.tile(np.eye(64, dtype=np.float32), (1, 8)),
            "slt": np.triu(np.ones((128, 128), dtype=np.float32), 1),
            "ebase": np.tile((np.arange(NEXP, dtype=np.float32) * 512.0)[None, :], (128, 1))}


L_FULL, LS_FULL, PAST_FULL = 2048, 64, 2048


def run(inputs, L, LS, PAST, ncores, phases=None):
    key = (L, LS, PAST, None if phases is None else tuple(sorted(phases)))
    if key not in _CACHE:
        _CACHE[key] = Prog(L, LS, PAST, phases=phases)
    p = _CACHE[key]
    f32 = lambda a: np.ascontiguousarray(np.asarray(a, dtype=np.float32))
    xp, xs = f32(inputs["x_prompt"]), f32(inputs["x_sample"])
    pp, psm = f32(inputs["p_prompt"][0]), f32(inputs["p_sample"][0])
    shared = dict(_consts())
    for k in ("w_in", "w_w2", "w_a2", "w_g2", "w_fox_up", "w_rwkv_up", "w_o", "w_rg", "w_re", "w1", "w3", "w2",
              "w_ple", "w_pg"):
        shared[k] = f32(inputs[k][0])
    for k in ("g_mix", "b_f", "mu_shift", "w0", "a0", "k_k", "k_a", "lnx_g", "lnx_b", "g_ffn", "b_rg", "b_re", "b_pg"):
        shared[k] = f32(inputs[k]).reshape(1, -1)
    shared["r_k"] = f32(inputs["r_k"]).reshape(1, -1)
    shared["g_final"] = f32(inputs["g_final"]).reshape(1, -1)
    in_maps = []
    for c in range(ncores):
        m = dict(shared)
        m["x_all"] = np.concatenate([xp[2 * c], xp[2 * c + 1], xs[2 * c], xs[2 * c + 1]], axis=0)
        m["p_all"] = np.concatenate([pp[2 * c], pp[2 * c + 1], psm[2 * c], psm[2 * c + 1]], axis=0)
        m["cache_k"] = f32(inputs["cache_k"][0, 2 * c:2 * c + 2])
        m["cache_v"] = f32(inputs["cache_v"][0, 2 * c:2 * c + 2])
        m["cache_logf"] = f32(inputs["cache_logf"][0, 2 * c:2 * c + 2])
        m["state_wkv"] = f32(inputs["state_wkv"][0, 2 * c:2 * c + 2])
        m["state_shift"] = f32(inputs["state_shift"][0, 2 * c:2 * c + 2])
        in_maps.append({k: v for k, v in m.items() if k in p.ins})
    res = run_bass_kernel_spmd(p.nc, in_maps, core_ids=list(range(ncores)))
    R = res.results
    B = BS = 2 * ncores
    z = lambda *sh: np.zeros(sh, np.float32)
    yp, ys = z(B, L, D), z(BS, LS, D)
    nkp, nvp, nlp = z(1, B, L, NH, HD), z(1, B, L, NH, HD), z(1, B, L, NH)
    nks, nvs, nls = z(1, BS, LS, NH, HD), z(1, BS, LS, NH, HD), z(1, BS, LS, NH)
    shp, shs = z(1, B, RSW), z(1, BS, RSW)
    wkvp, wkvs = z(1, B, RH, RD, RD), z(1, BS, RH, RD, RD)
    for c in range(ncores):
        r = R[c]
        for i in range(2):
            b = 2 * c + i
            o = 2 * L + i * LS
            nkp[0, b] = r["k_all"][i * L:(i + 1) * L].reshape(L, NH, HD)
            nvp[0, b] = r["v_all"][i * L:(i + 1) * L].reshape(L, NH, HD)
            nlp[0, b] = r["logf_all"][i * L:(i + 1) * L]
            nks[0, b] = r["k_all"][o:o + LS].reshape(LS, NH, HD)
            nvs[0, b] = r["v_all"][o:o + LS].reshape(LS, NH, HD)
            nls[0, b] = r["logf_all"][o:o + LS]
            shp[0, b] = r["shift_out"][i]
            shs[0, b] = r["shift_out"][2 + i]
            if "wkv_out" in r:
                wkvp[0, b] = r["wkv_out"][i]
                wkvs[0, b] = r["wkv_out"][2 + i]
            if "y_all" in r:
                yp[b] = r["y_all"][i * L:(i + 1) * L]
                ys[b] = r["y_all"][o:o + LS]
    _CACHE["last"] = R
    return (yp, ys, nkp, nvp, nlp, wkvp, shp, nks, nvs, nls, wkvs, shs)


def kernel(**inputs):
    return run(inputs, L_FULL, LS_FULL, PAST_FULL, 8)
```

```python
import numpy as np
from contextlib import ExitStack
import concourse.bass as bass
import concourse.mybir as mybir
from concourse.bass_utils import run_bass_kernel_spmd

F32 = mybir.dt.float32
F32R = mybir.dt.float32r
I32 = mybir.dt.int32
U32 = mybir.dt.uint32
AF = mybir.ActivationFunctionType
ALU = mybir.AluOpType
AX = mybir.AxisListType

D = 2048
NH = 8
HD = 128
FW = 1024
RH = 16
RD = 64
RW = 1024
RSW = 3360
INW = 10536
OFF_Q, OFF_K, OFF_V, OFF_F, OFF_RW, OFF_G = 0, 1024, 2048, 3072, 3080, 6440
NEXP = 32
DE = 512
PLE = 256
NORM_EPS = 1e-6
STQ = "act"
GN_EPS = 64e-5
DECAY_SCALE = float(np.exp(-0.5))


class T:
    __slots__ = ("w", "r", "name")

    def __init__(self, name=""):
        self.w = None
        self.r = {}
        self.name = name


class Sched:
    NDS = 16

    def __init__(self, nc):
        self.nc = nc
        self.E = dict(pe=nc.tensor, act=nc.scalar, dve=nc.vector, pool=nc.gpsimd, sp=nc.sync)
        self.csem = {e: nc.alloc_semaphore(name=f"c_{e}") for e in ("pe", "act", "dve", "pool")}
        self.ccnt = {e: 0 for e in self.csem}
        self.dsem = {q: [nc.alloc_semaphore(name=f"d_{q}{i}") for i in range(self.NDS)]
                     for q in ("sp", "pool", "act")}
        self.dcnt = {q: [0] * self.NDS for q in self.dsem}
        self.dnext = {q: 0 for q in self.dsem}
        self.waited = {}
        self.nwaits = 0
        self.ninst = 0

    def _wait(self, eng, tok):
        sem, val, src = tok
        if src == eng and eng == "pe":
            return
        key = (eng, id(sem))
        if self.waited.get(key, 0) >= val:
            return
        self.E[eng].wait_ge(sem, val)
        self.nwaits += 1
        self.waited[key] = val

    def _deps(self, eng, reads, writes):
        for t in reads:
            if t.w is not None:
                self._wait(eng, t.w)
        for t in writes:
            if t.w is not None:
                self._wait(eng, t.w)
            for tok in t.r.values():
                self._wait(eng, tok)

    def _mark(self, tok, reads, writes):
        k = id(tok[0])
        for t in reads:
            t.r[k] = tok
        for t in writes:
            t.w = tok
            t.r = {}

    def op(self, eng, fn, reads=(), writes=()):
        self._deps(eng, reads, writes)
        inst = fn(self.E[eng])
        self.ccnt[eng] += 1
        inst.then_inc(self.csem[eng], 1)
        self.ninst += 1
        self._mark((self.csem[eng], self.ccnt[eng], eng), reads, writes)

    def dma(self, q, out, in_, reads=(), writes=(), fn=None, **kw):
        self._deps(q, reads, writes)
        i = self.dnext[q]
        self.dnext[q] = (i + 1) % self.NDS
        sem = self.dsem[q][i]
        if self.dcnt[q][i] > 0:
            self._wait(q, (sem, self.dcnt[q][i], "dma"))
        if fn is not None:
            inst = fn(self.E[q])
        else:
            inst = self.E[q].dma_start(out=out, in_=in_, **kw)
        self.dcnt[q][i] += 16
        inst.then_inc(sem, 16)
        self.ninst += 1
        self._mark((sem, self.dcnt[q][i], "dma"), reads, writes)

    def barrier(self, engines=("pe", "act", "dve", "pool", "sp")):
        for e in engines:
            for s, c in self.ccnt.items():
                if c > 0:
                    self._wait(e, (self.csem[s], c, "x"))
            for q in self.dsem:
                for i in range(self.NDS):
                    if self.dcnt[q][i] > 0:
                        self._wait(e, (self.dsem[q][i], self.dcnt[q][i], "dma"))


def r32(ap):
    return ap.bitcast(F32R)


class Prog:
    def __init__(self, L, LS, PAST, phases=None):
        self.L, self.LS, self.PAST = L, LS, PAST
        self.TT = 2 * L + 2 * LS
        self.phases = phases
        nc = bass.Bass("TRN2", target_bir_lowering=False)
        nc.dge_precook = False
        self.nc = nc
        self.s = Sched(nc)
        self.ins = {}
        self.outs = {}
        self.build()

    def din(self, name, shape, dt=F32):
        t = self.nc.dram_tensor(name, list(shape), dt, kind="ExternalInput").ap()
        self.ins[name] = t
        return t

    def cin(self, name, shape, dt=F32):
        if name in self.ins:
            return self.ins[name]
        return self.din(name, shape, dt)

    def dout(self, name, shape, dt=F32):
        t = self.nc.dram_tensor(name, list(shape), dt, kind="ExternalOutput").ap()
        self.outs[name] = t
        return t

    def dscr(self, name, shape, dt=F32):
        return self.nc.dram_tensor(name, list(shape), dt, kind="Internal").ap()

    def build(self):
        nc, s = self.nc, self.s
        L, LS, PAST, TT = self.L, self.LS, self.PAST, self.TT
        x_all = self.din("x_all", [TT, D])
        w_in = self.din("w_in", [D, INW])
        g_mix = self.din("g_mix", [1, D])
        b_f = self.din("b_f", [1, NH])
        ident_d = self.din("ident", [128, 128])
        k_all = self.dout("k_all", [TT, FW])
        v_all = self.dout("v_all", [TT, FW])
        logf_all = self.dout("logf_all", [TT, NH])
        shift_out = self.dout("shift_out", [4, RSW])
        ends = {L - 1: 0, 2 * L - 1: 1, 2 * L + LS - 1: 2, 2 * L + 2 * LS - 1: 3}
        qT_d = self.dscr("qT_d", [NH, 128, TT])
        kT_d = self.dscr("kT_d", [NH, 128, TT])
        rwT_d = self.dscr("rwT_d", [53, 64, TT])
        sgT_d = self.dscr("sgT_d", [32, 128, TT])
        self.dbg = {}
        if self.phases is not None and "dbg12" in self.phases:
            self.dbg["qT"] = self.dout("dbg_qT", [NH, 128, TT])
            self.dbg["rwT"] = self.dout("dbg_rwT", [53, 64, TT])
            self.dbg["sgT"] = self.dout("dbg_sgT", [32, 128, TT])
            qT_d, rwT_d, sgT_d = self.dbg["qT"], self.dbg["rwT"], self.dbg["sgT"]

        groups = []
        G = 512
        for sq in range(2):
            for g0 in range(0, L, G):
                groups.append((sq * L + g0, min(G, L - g0)))
        groups.append((2 * L, 2 * LS))

        with ExitStack() as es:
            ident = es.enter_context(nc.sbuf_tensor("sb_ident", [128, 128], F32))
            gbc = es.enter_context(nc.sbuf_tensor("sb_gbc", [128, D], F32))
            bfbc = es.enter_context(nc.sbuf_tensor("sb_bfbc", [128, NH], F32))
            xt0 = es.enter_context(nc.sbuf_tensor("sb_xt0", [128, D], F32))
            xt1 = es.enter_context(nc.sbuf_tensor("sb_xt1", [128, D], F32))
            xn = es.enter_context(nc.sbuf_tensor("sb_xn", [128, D], F32))
            junk = es.enter_context(nc.sbuf_tensor("sb_junk", [128, D], F32))
            stat = es.enter_context(nc.sbuf_tensor("sb_stat", [128, 8], F32))
            hT = es.enter_context(nc.sbuf_tensor("sb_hT", [128, 16, G], F32R))
            ws0 = es.enter_context(nc.sbuf_tensor("sb_ws0", [128, 16, 512], F32R))
            ws1 = es.enter_context(nc.sbuf_tensor("sb_ws1", [128, 16, 512], F32R))
            stg0 = es.enter_context(nc.sbuf_tensor("sb_stg0", [128, 512], F32))
            stg1 = es.enter_context(nc.sbuf_tensor("sb_stg1", [128, 512], F32))
            stg2 = es.enter_context(nc.sbuf_tensor("sb_stg2", [128, 512], F32))
            stg3 = es.enter_context(nc.sbuf_tensor("sb_stg3", [128, 512], F32))
            lf0 = es.enter_context(nc.sbuf_tensor("sb_lf0", [128, 16], F32))
            t_ident, t_gbc, t_bfbc = T("ident"), T("gbc"), T("bfbc")
            s.dma("sp", ident[:], ident_d[:, :], writes=[t_ident])
            s.dma("sp", gbc[:], g_mix[0:1, :].partition_broadcast(128), writes=[t_gbc])
            s.dma("sp", bfbc[:], b_f[0:1, :].partition_broadcast(128), writes=[t_bfbc])
            ps = self.ps = [nc.alloc_psum_tensor(f"ps{i}", [128, 512], F32) for i in range(8)]
            t_ps = self.t_ps = [T(f"ps{i}") for i in range(8)]
            self.psn = 0

            def next_ps():
                i = self.psn
                self.psn = (i + 1) % 8
                return ps[i], t_ps[i]

            xts = [(xt0, T("xt0")), (xt1, T("xt1"))]
            wss = [(ws0, T("ws0")), (ws1, T("ws1"))]
            stgs = [(stg0, T("stg0")), (stg1, T("stg1")), (stg2, T("stg2")), (stg3, T("stg3"))]
            t_xn, t_junk, t_stat, t_hT, t_lf = T("xn"), T("junk"), T("stat"), T("hT"), T("lf")
            self.stn = 0
            self.evn = 0

            def next_stg():
                i = self.stn
                self.stn = (i + 1) % 4
                return stgs[i]

            def evac(out_ap, in_ap, reads, writes, func=None):
                self.evn += 1
                if func is not None:
                    s.op("act", lambda e: e.activation(out=out_ap, in_=in_ap, func=func), reads, writes)
                elif self.evn % 2 == 0:
                    s.op("act", lambda e: e.activation(out=out_ap, in_=in_ap, func=AF.Copy), reads, writes)
                else:
                    s.op("dve", lambda e: e.tensor_copy(out=out_ap, in_=in_ap), reads, writes)

            slabs = []
            for c0 in range(0, 1024, 512):
                slabs.append((OFF_Q + c0, 512, "q"))
            for c0 in range(0, 1024, 512):
                slabs.append((OFF_K + c0, 512, "k"))
            for c0 in range(0, 1024, 512):
                slabs.append((OFF_V + c0, 512, "v"))
            slabs.append((OFF_F, 8, "f"))
            for c0 in range(0, RSW, 512):
                slabs.append((OFF_RW + c0, min(512, RSW - c0), "rw"))
            for c0 in range(0, 4096, 512):
                slabs.append((OFF_G + c0, 512, "g"))

            xi = 0
            wi = 0
            for (tok0, ntok) in groups:
                ntile = (ntok + 127) // 128
                for ti in range(ntile):
                    n = min(128, ntok - ti * 128)
                    xt, t_xt = xts[xi % 2]
                    xi += 1
                    r0 = tok0 + ti * 128
                    s.dma("sp", xt[0:n, :], x_all[r0:r0 + n, :], writes=[t_xt])
                    s.op("act", lambda e: e.activation(out=junk[0:n, :], in_=xt[0:n, :], func=AF.Square,
                                                       accum_out=stat[0:n, 0:1]),
                         reads=[t_xt], writes=[t_junk, t_stat])
                    s.op("act", lambda e: e.activation(out=stat[0:n, 1:2], in_=stat[0:n, 0:1], func=AF.Sqrt,
                                                       scale=1.0 / D, bias=NORM_EPS),
                         reads=[t_stat], writes=[t_stat])
                    s.op("dve", lambda e: e.reciprocal(out=stat[0:n, 2:3], in_=stat[0:n, 1:2]),
                         reads=[t_stat], writes=[t_stat])
                    s.op("dve", lambda e: e.scalar_tensor_tensor(out=xn[0:n, :], in0=xt[0:n, :],
                                                                 scalar=stat[0:n, 2:3], in1=gbc[0:n, :],
                                                                 op0=ALU.mult, op1=ALU.mult),
                         reads=[t_xt, t_stat, t_gbc], writes=[t_xn])
                    for k4 in range(4):
                        pt, t_pt = next_ps()
                        for j in range(4):
                            kc = k4 * 4 + j
                            s.op("pe", lambda e: e.transpose(out=pt[:, j * 128:j * 128 + n],
                                                             in_=xn[0:n, kc * 128:(kc + 1) * 128],
                                                             identity=ident[0:n, 0:n]),
                                 reads=[t_xn, t_ident], writes=[t_pt])
                        evac(hT[:, k4 * 4:k4 * 4 + 4, ti * 128:ti * 128 + n],
                             pt[:].rearrange("p (j t) -> p j t", j=4)[:, :, 0:n],
                             reads=[t_pt], writes=[t_hT])
                dbgf = self.phases or ()
                if "p1only" in dbgf:
                    s.dma("sp", self.dbg["rwT"][0:16, :, tok0:tok0 + ntok].rearrange("k p t -> p k t"),
                          hT[:, :, 0:ntok].bitcast(F32), reads=[t_hT])
                    continue
                for (c0, ncol, kind) in slabs:
                    if any(("only_" + kk) in dbgf for kk in "qkvfg") and ("only_" + kind[0]) not in dbgf:
                        continue
                    ws, t_ws = wss[wi % 2]
                    wi += 1
                    s.dma("sp", ws[:, :, 0:ncol],
                          r32(w_in[:, c0:c0 + ncol]).rearrange("(kc p) c -> p kc c", p=128),
                          writes=[t_ws])
                    if kind in ("q", "k", "rw", "g"):
                        for cb in range(0, ncol, 128):
                            m = min(128, ncol - cb)
                            pt, t_pt = next_ps()
                            for kc in range(16):
                                s.op("pe", lambda e: e.matmul(pt[:, 0:ntok], lhsT=ws[:, kc, cb:cb + 128],
                                                              rhs=hT[:, kc, 0:ntok],
                                                              start=(kc == 0), stop=(kc == 15)),
                                     reads=[t_ws, t_hT], writes=[t_pt])
                            stg, t_stg = next_stg()
                            evac(stg[0:m, 0:ntok], pt[0:m, 0:ntok], [t_pt], [t_stg],
                                 func=(AF.Sigmoid if kind == "g" else None))
                            if kind == "q":
                                dst = qT_d[(c0 - OFF_Q + cb) // 128, :, tok0:tok0 + ntok]
                            elif kind == "k":
                                dst = kT_d[(c0 - OFF_K + cb) // 128, :, tok0:tok0 + ntok]
                            elif kind == "rw":
                                hc = (c0 - OFF_RW + cb) // 64
                                dst = rwT_d[hc, 0:min(m, 64), tok0:tok0 + ntok]
                                if m > 64:
                                    s.dma(STQ, rwT_d[hc + 1, :, tok0:tok0 + ntok], stg[64:128, 0:ntok], reads=[t_stg])
                            else:
                                dst = sgT_d[(c0 - OFF_G + cb) // 128, :, tok0:tok0 + ntok]
                            s.dma(STQ, dst, stg[0:(min(m, 64) if kind == "rw" else m), 0:ntok], reads=[t_stg])
                            if kind == "rw":
                                for te, sidx in ends.items():
                                    if tok0 <= te < tok0 + ntok:
                                        cc = c0 - OFF_RW + cb
                                        s.dma(STQ, shift_out[sidx, cc:cc + m].rearrange("(p o) -> p o", o=1),
                                              stg[0:m, te - tok0:te - tok0 + 1], reads=[t_stg])
                    if kind in ("k", "v", "f"):
                        for ti in range(ntile):
                            n = min(128, ntok - ti * 128)
                            pt, t_pt = next_ps()
                            for kc in range(16):
                                s.op("pe", lambda e: e.matmul(pt[0:n, 0:ncol],
                                                              lhsT=hT[:, kc, ti * 128:ti * 128 + n],
                                                              rhs=ws[:, kc, 0:ncol],
                                                              start=(kc == 0), stop=(kc == 15)),
                                     reads=[t_ws, t_hT], writes=[t_pt])
                            r0 = tok0 + ti * 128
                            if kind == "f":
                                s.op("dve", lambda e: e.tensor_tensor(out=lf0[0:n, 0:8], in0=pt[0:n, 0:8],
                                                                      in1=bfbc[0:n, :], op=ALU.add),
                                     reads=[t_pt, t_bfbc], writes=[t_lf])
                                s.op("act", lambda e: e.activation(out=lf0[0:n, 0:8], in_=lf0[0:n, 0:8],
                                                                   func=AF.Exp, scale=-1.0),
                                     reads=[t_lf], writes=[t_lf])
                                s.op("act", lambda e: e.activation(out=lf0[0:n, 0:8], in_=lf0[0:n, 0:8],
                                                                   func=AF.Ln, bias=1.0),
                                     reads=[t_lf], writes=[t_lf])
                                s.op("dve", lambda e: e.tensor_scalar(out=lf0[0:n, 8:16], in0=lf0[0:n, 0:8],
                                                                      scalar1=-1.0, scalar2=None, op0=ALU.mult),
                                     reads=[t_lf], writes=[t_lf])
                                s.dma(STQ, logf_all[r0:r0 + n, :], lf0[0:n, 8:16], reads=[t_lf])
                            else:
                                stg, t_stg = next_stg()
                                evac(stg[0:n, 0:ncol], pt[0:n, 0:ncol], [t_pt], [t_stg])
                                dst = (k_all if kind == "k" else v_all)
                                cc = c0 - (OFF_K if kind == "k" else OFF_V)
                                s.dma(STQ, dst[r0:r0 + n, cc:cc + ncol], stg[0:n, 0:ncol], reads=[t_stg])
            s.barrier()
        self.qT_d, self.kT_d, self.rwT_d, self.sgT_d = qT_d, kT_d, rwT_d, sgT_d
        self.k_all, self.v_all, self.logf_all, self.ident_d = k_all, v_all, logf_all, ident_d
        self.x_all = x_all
        ph = self.phases or ()
        if "stop12" not in ph:
            if "noattn" not in ph:
                self.phase_attn()
            if "norwkv" not in ph:
                self.phase_rwkv()
            if "stoprw" not in ph:
                self.phase_merge()
                if "stopmerge" not in ph:
                    self.phase_experts()
                    if "stopexp" not in ph:
                        self.phase_final()
        s.barrier(engines=("sp",))

    def phase_attn(self):
        nc, s = self.nc, self.s
        L, LS, PAST, TT = self.L, self.LS, self.PAST, self.TT
        scale = float(HD) ** -0.5
        cache_k = self.din("cache_k", [2, PAST, NH, HD])
        cache_v = self.din("cache_v", [2, PAST, NH, HD])
        cache_logf = self.din("cache_logf", [2, PAST, NH])
        triu_d = self.cin("triu", [128, 128])
        ones_d = self.cin("ones", [128, 128])
        Lkmax = max(L, PAST + LS)
        nkbmax = (Lkmax + 127) // 128
        nkbp = ((nkbmax + 15) // 16) * 16
        Lqmax = max(L, LS)
        FThi_d = self.dscr("FThi_d", [4, NH, nkbp * 128], F32R)
        FTlo_d = self.dscr("FTlo_d", [4, NH, nkbp * 128], F32R)
        foxT_d = self.dscr("foxT_d", [NH, 128, TT])
        if "dbgattn" in (self.phases or ()):
            foxT_d = self.dout("dbg_foxT", [NH, 128, TT])
        self.foxT_d = foxT_d
        seqs = [(0, L, 0, None), (L, L, 0, None), (2 * L, LS, PAST, 0), (2 * L + LS, LS, PAST, 1)]
        with ExitStack() as es:
            def sb(name, shape, dt=F32):
                return es.enter_context(nc.sbuf_tensor("a_" + name, shape, dt))
            ident = sb("ident", [128, 128])
            triu = sb("triu", [128, 128])
            ones = sb("ones", [128, 128], F32R)
            onesf = sb("onesf", [128, 128])
            LF = sb("LF", [128, nkbp, NH])
            tot = sb("tot", [128, nkbp, NH])
            inc = sb("inc", [128, nkbp, NH])
            Fall = sb("Fall", [128, nkbp, NH])
            negF = sb("negF", [128, nkbp, NH])
            FThi = sb("FThi", [128, (nkbp // 16) * 128], F32R)
            FTlo = sb("FTlo", [128, (nkbp // 16) * 128], F32R)
            qT = sb("qT", [128, Lqmax], F32R)
            kT = sb("kT", [128, nkbmax * 128], F32R)
            V = sb("V", [128, nkbmax, 128], F32R)
            kc = sb("kc", [128, max(PAST // 128, 1), 128])
            Fq = sb("Fq", [128, Lqmax], F32R)
            PT0 = sb("PT0", [128, 512], F32R)
            PT1 = sb("PT1", [128, 512], F32R)
            PT2 = sb("PT2", [128, 512], F32R)
            rinv = sb("rinv", [128, 512])
            ost0 = sb("ost0", [128, 512])
            ost1 = sb("ost1", [128, 512])
            ps, t_ps = self.ps, self.t_ps
            t_c = T("consts")
            s.dma("sp", ident[:], self.ident_d[:, :], writes=[t_c])
            s.dma("sp", triu[:], triu_d[:, :], writes=[t_c])
            s.dma("sp", ones[:], r32(ones_d[:, :]), writes=[t_c])
            s.dma("sp", onesf[:], ones_d[:, :], writes=[t_c])
            t_LF, t_tot, t_inc, t_Fall, t_negF, t_FThi, t_FTlo, t_FTd = (T() for _ in range(8))
            t_qT, t_kT, t_V, t_kc, t_Fq = (T() for _ in range(5))
            s.op("dve", lambda e: e.memset(Fq[:, :].bitcast(F32), 0.0), writes=[t_Fq])
            s.op("dve", lambda e: e.memset(kT[:, :].bitcast(F32), 0.0), writes=[t_kT])
            s.op("dve", lambda e: e.memset(V[:, :, :].bitcast(F32), 0.0), writes=[t_V])
            s.op("dve", lambda e: e.memset(Fall[:, :, :], 0.0), writes=[t_Fall])
            PTs = [(PT0, T()), (PT1, T()), (PT2, T())]
            osts = [(ost0, T()), (ost1, T())]
            t_rinv = T()
            pti = 0
            psi = 0
            qbi = 0
            for si, (tok0, Lq, off, ci) in enumerate(seqs):
                Lk = off + Lq
                nkb = (Lk + 127) // 128
                s.op("dve", lambda e: e.memset(LF[:, :, :], 0.0), writes=[t_LF])
                if off:
                    s.dma("sp", LF[:, 0:off // 128, :], cache_logf[ci].rearrange("(kb p) h -> p kb h", p=128),
                          writes=[t_LF])
                for j in range(0, Lq, 128):
                    n = min(128, Lq - j)
                    s.dma("sp", LF[0:n, (off + j) // 128, :], self.logf_all[tok0 + j:tok0 + j + n, :], writes=[t_LF])
                N8 = nkb * NH
                psA, t_psA = ps[7], t_ps[7]
                psB, t_psB = ps[6], t_ps[6]
                s.op("pe", lambda e: e.matmul(psA[:, 0:N8], lhsT=triu[:, :],
                                              rhs=LF[:, 0:nkb, :].rearrange("p k h -> p (k h)"),
                                              start=True, stop=True),
                     reads=[t_c, t_LF], writes=[t_psA])
                s.op("pe", lambda e: e.matmul(psB[:, 0:N8], lhsT=onesf[:, :],
                                              rhs=LF[:, 0:nkb, :].rearrange("p k h -> p (k h)"),
                                              start=True, stop=True),
                     reads=[t_c, t_LF], writes=[t_psB])
                s.op("act", lambda e: e.activation(out=tot[:, 0:nkb, :].rearrange("p k h -> p (k h)"),
                                                   in_=psB[:, 0:N8], func=AF.Copy),
                     reads=[t_psB], writes=[t_tot])
                for h in range(NH):
                    s.op("dve", lambda e: e.tensor_tensor_scan(out=inc[:, 0:nkb, h], data0=onesf[:, 0:nkb],
                                                               data1=tot[:, 0:nkb, h], initial=0.0,
                                                               op0=ALU.mult, op1=ALU.add),
                         reads=[t_tot, t_c], writes=[t_inc])
                s.op("dve", lambda e: e.tensor_tensor(out=inc[:, 0:nkb, :], in0=inc[:, 0:nkb, :],
                                                      in1=tot[:, 0:nkb, :], op=ALU.subtract),
                     reads=[t_inc, t_tot], writes=[t_inc])
                s.op("dve", lambda e: e.tensor_tensor(out=Fall[:, 0:nkb, :].rearrange("p k h -> p (k h)"),
                                                      in0=psA[:, 0:N8],
                                                      in1=inc[:, 0:nkb, :].rearrange("p k h -> p (k h)"),
                                                      op=ALU.add),
                     reads=[t_psA, t_inc], writes=[t_Fall])
                s.op("dve", lambda e: e.tensor_scalar(out=negF[:, 0:nkb, :], in0=Fall[:, 0:nkb, :], scalar1=-1.0,
                                                      scalar2=None, op0=ALU.mult),
                     reads=[t_Fall], writes=[t_negF])
                ng = (nkb + 15) // 16
                for g in range(ng):
                    s.op("pe", lambda e: e.transpose(out=psA[:, g * 128:(g + 1) * 128],
                                                     in_=Fall[:, g * 16:(g + 1) * 16, :].rearrange("p k h -> p (k h)"),
                                                     identity=ident[:, :]),
                         reads=[t_Fall, t_c], writes=[t_psA])
                s.op("dve", lambda e: e.tensor_scalar(out=FThi[:, 0:ng * 128], in0=psA[:, 0:ng * 128],
                                                      scalar1=1.0 / scale, scalar2=None, op0=ALU.mult),
                     reads=[t_psA], writes=[t_FThi])
                s.op("dve", lambda e: e.scalar_tensor_tensor(out=FTlo[:, 0:ng * 128], in0=psA[:, 0:ng * 128],
                                                             scalar=1.0 / scale,
                                                             in1=FThi[:, 0:ng * 128].bitcast(F32),
                                                             op0=ALU.mult, op1=ALU.subtract),
                     reads=[t_psA, t_FThi], writes=[t_FTlo])
                for kb in range(nkb):
                    g, kl = kb // 16, kb % 16
                    s.dma("sp", FThi_d[si, :, kb * 128:(kb + 1) * 128],
                          FThi[kl * 8:(kl + 1) * 8, g * 128:(g + 1) * 128], reads=[t_FThi], writes=[t_FTd])
                    s.dma("sp", FTlo_d[si, :, kb * 128:(kb + 1) * 128],
                          FTlo[kl * 8:(kl + 1) * 8, g * 128:(g + 1) * 128], reads=[t_FTlo], writes=[t_FTd])
                if "attn_stopF" in (self.phases or ()):
                    continue
                for h in range(NH):
                    s.dma("sp", qT[:, 0:Lq], r32(self.qT_d[h, :, tok0:tok0 + Lq]), writes=[t_qT])
                    s.dma("sp", kT[:, off:Lk], r32(self.kT_d[h, :, tok0:tok0 + Lq]), writes=[t_kT])
                    if off:
                        s.dma("sp", kc[:, 0:off // 128, :],
                              cache_k[ci, :, h, :].rearrange("(kb p) d -> p kb d", p=128), writes=[t_kc])
                        s.dma("sp", V[:, 0:off // 128, :],
                              r32(cache_v[ci, :, h, :]).rearrange("(kb p) d -> p kb d", p=128), writes=[t_V])
                    if Lq % 128 == 0:
                        s.dma("sp", V[:, off // 128:off // 128 + Lq // 128, :],
                              r32(self.v_all[tok0:tok0 + Lq, h * 128:(h + 1) * 128]).rearrange(
                                  "(kb p) d -> p kb d", p=128), writes=[t_V])
                    else:
                        for j in range(0, Lq, 128):
                            n = min(128, Lq - j)
                            s.dma("sp", V[0:n, (off + j) // 128, :],
                                  r32(self.v_all[tok0 + j:tok0 + j + n, h * 128:(h + 1) * 128]), writes=[t_V])
                    s.dma("sp", Fq[0:1, 0:Lq], FThi_d[si, h:h + 1, off:Lk], reads=[t_FTd], writes=[t_Fq])
                    s.dma("sp", Fq[1:2, 0:Lq], FTlo_d[si, h:h + 1, off:Lk], reads=[t_FTd], writes=[t_Fq])
                    if off:
                        for k4 in range(0, off // 128, 4):
                            pt, t_pt = ps[7], t_ps[7]
                            nb = min(4, off // 128 - k4)
                            for j in range(nb):
                                s.op("pe", lambda e: e.transpose(out=pt[:, j * 128:(j + 1) * 128],
                                                                 in_=kc[:, k4 + j, :], identity=ident[:, :]),
                                     reads=[t_kc, t_c], writes=[t_pt])
                            s.op("dve", lambda e: e.tensor_copy(out=kT[:, k4 * 128:(k4 + nb) * 128],
                                                                in_=pt[:, 0:nb * 128]),
                                 reads=[t_pt], writes=[t_kT])
                    if "attn_noblocks" in (self.phases or ()):
                        continue
                    if "attn_only_prompt" in (self.phases or ()) and off:
                        continue
                    if "attn_only_sample" in (self.phases or ()) and not off:
                        continue
                    for t0 in range(0, Lq, 512):
                        nq = min(512, Lq - t0)
                        psO, t_psO = ps[3 + qbi % 2], t_ps[3 + qbi % 2]
                        psR, t_psR = ps[5], t_ps[5]
                        ost, t_ost = osts[qbi % 2]
                        qbi += 1
                        blocks = []
                        for kb in range(nkb):
                            s0 = kb * 128
                            ns = min(128, Lk - s0)
                            dlt = off + t0 - s0
                            if dlt >= ns - 1:
                                blocks.append((kb, s0, 0, False))
                            else:
                                cs = -dlt
                                assert cs >= 0
                                if cs < nq:
                                    blocks.append((kb, s0, cs, True))
                        for bi, (kb, s0, cs, diag) in enumerate(blocks):
                            first, last = bi == 0, bi == len(blocks) - 1
                            psS, t_psS = ps[psi % 3], t_ps[psi % 3]
                            psi += 1
                            PT, t_PT = PTs[pti % 3]
                            pti += 1
                            s.op("pe", lambda e: e.matmul(psS[:, cs:nq], lhsT=kT[:, s0:s0 + 128],
                                                          rhs=qT[:, t0 + cs:t0 + nq], start=True, stop=False),
                                 reads=[t_kT, t_qT], writes=[t_psS])
                            s.op("pe", lambda e: e.matmul(psS[:, cs:nq], lhsT=ones[:, :],
                                                          rhs=Fq[:, t0 + cs:t0 + nq], start=False, stop=True),
                                 reads=[t_c, t_Fq], writes=[t_psS])
                            for hh in range(1):
                                s.op("act", lambda e: e.activation(out=PT[:, cs:nq], in_=psS[:, cs:nq],
                                                                   func=AF.Exp, scale=scale,
                                                                   bias=negF[:, kb, h:h + 1]),
                                     reads=[t_psS, t_negF], writes=[t_PT])
                            if diag:
                                w = min(128, nq - cs)
                                s.op("dve", lambda e: e.tensor_tensor(out=PT[:, cs:cs + w],
                                                                      in0=PT[:, cs:cs + w].bitcast(F32),
                                                                      in1=triu[:, 0:w], op=ALU.mult),
                                     reads=[t_PT, t_c], writes=[t_PT])
                            s.op("pe", lambda e: e.matmul(psO[:, cs:nq], lhsT=V[:, kb, :],
                                                          rhs=PT[:, cs:nq], start=first, stop=last),
                                 reads=[t_V, t_PT], writes=[t_psO])
                            s.op("pe", lambda e: e.matmul(psR[:, cs:nq], lhsT=ones[:, :],
                                                          rhs=PT[:, cs:nq], start=first, stop=last),
                                 reads=[t_c, t_PT], writes=[t_psR])
                        s.op("dve", lambda e: e.reciprocal(out=rinv[:, 0:nq], in_=psR[:, 0:nq]),
                             reads=[t_psR], writes=[t_rinv])
                        s.op("dve", lambda e: e.tensor_tensor(out=ost[:, 0:nq], in0=psO[:, 0:nq],
                                                              in1=rinv[:, 0:nq], op=ALU.mult),
                             reads=[t_psO, t_rinv], writes=[t_ost])
                        s.dma(STQ, foxT_d[h, :, tok0 + t0:tok0 + t0 + nq], ost[:, 0:nq], reads=[t_ost])
            s.barrier()


    def phase_rwkv(self):
        nc, s = self.nc, self.s
        L, LS, PAST, TT = self.L, self.LS, self.PAST, self.TT
        C = 64
        state_wkv = self.din("state_wkv", [2, RH, RD, RD])
        state_shift = self.din("state_shift", [2, RSW])
        mu_d = self.din("mu_shift", [1, RSW])
        w0_d = self.din("w0", [1, RW])
        a0_d = self.din("a0", [1, RW])
        kk_d = self.din("k_k", [1, RW])
        ka_d = self.din("k_a", [1, RW])
        rk_d = self.din("r_k", [1, RW])
        lg_d = self.din("lnx_g", [1, RW])
        lb_d = self.din("lnx_b", [1, RW])
        ww2_d = self.din("w_w2", [64, RW])
        wa2_d = self.din("w_a2", [64, RW])
        wg2_d = self.din("w_g2", [160, RW])
        msu_d = self.din("m_su", [64, 512])
        msl_d = self.din("m_sl", [64, 512])
        mu8_d = self.din("m_u", [64, 512])
        i8_d = self.din("i8", [64, 512])
        ones_d = self.cin("ones", [128, 128])
        wkv_out = self.dout("wkv_out", [4, RH, RD, RD])
        rwoT_d = self.dscr("rwoT_d", [8, 128, TT])
        if "dbgrw" in (self.phases or ()):
            rwoT_d = self.dout("dbg_rwoT", [8, 128, TT])
        self.rwoT_d = rwoT_d
        rwT_d = self.rwT_d
        seqs = [(0, L, None), (L, L, None), (2 * L, LS, 0), (2 * L + LS, LS, 1)]
        with ExitStack() as es:
            def sb(name, shape, dt=F32):
                return es.enter_context(nc.sbuf_tensor("r_" + name, shape, dt))
            ident = sb("ident", [128, 128])
            ones = sb("ones", [64, 64])
            msu = sb("msu", [64, 8, 64]); msl = sb("msl", [64, 8, 64]); mu8 = sb("mu8", [64, 8, 64]); i8 = sb("i8", [64, 8, 64])
            mu = sb("mu", [64, 53]); w0 = sb("w0", [64, 16]); a0 = sb("a0", [64, 16]); k_k = sb("k_k", [64, 16])
            k_a = sb("k_a", [64, 16]); r_k = sb("r_k", [64, 16])
            lgb = sb("lgb", [64, RW]); lbb = sb("lbb", [64, RW])
            ww2 = sb("ww2", [64, RW]); wa2 = sb("wa2", [64, RW]); wg2 = sb("wg2", [64, 3, RW])
            ST = sb("ST", [64, 16, 64]); S0 = sb("S0", [64, 16, 64])
            Xs = [sb(f"X{i}", [64, 53, C + 1]) for i in range(2)]; xm = sb("xm", [64, 53, C])
            txws = [sb(f"txw{i}", [64, C]) for i in range(2)]; sxgs = [sb(f"sxg{i}", [64, 3, C]) for i in range(2)]
            names = ["lw", "a", "kkn", "krep", "Lc", "Ep", "rt", "En", "bh", "bg", "kg", "rkp", "tmp",
                     "Vtok", "bgT", "kgT", "N", "NT", "AK", "QK", "QB", "P0", "P1", "PT0", "PT1",
                     "X0", "X1"]
            alias = {"yc": "P0", "sq": "P1", "rwtok": "NT", "W0": "PT1", "U": "N"}
            tls, tts, st8s, rwoTs, t_st8s, t_rwoTs = [], [], [], [], [], []
            HG = 8
            NG = 16 // HG
            for g_ in range(NG):
                tl_ = {n: sb(f"{n}_{g_}", [64, HG, 64]) for n in names}
                tt_ = {n: T(n) for n in names}
                for k_, v_ in alias.items():
                    tl_[k_] = tl_[v_]
                    tt_[k_] = tt_[v_]
                tls.append(tl_)
                tts.append(tt_)
                st8s.append(sb(f"st8_{g_}", [64, 32]))
                rwoTs.append(sb(f"rwoT_{g_}", [128, HG // 2, 64]))
                t_st8s.append(T())
                t_rwoTs.append(T())
            ps, t_ps = self.ps, self.t_ps
            t_c = T("rconsts")
            s.dma("sp", ident[:], self.ident_d[:, :], writes=[t_c])
            s.dma("sp", ones[:], ones_d[0:64, 0:64], writes=[t_c])
            for tile_, d_ in ((msu, msu_d), (msl, msl_d), (mu8, mu8_d), (i8, i8_d)):
                s.dma("sp", tile_[:, :, :].rearrange("p a b -> p (a b)"), d_[:, :], writes=[t_c])
            s.dma("sp", mu[:, 0:52], mu_d[0, 0:52 * 64].rearrange("(j p) -> p j", p=64), writes=[t_c],
                  allow_slow_non_contiguous=True)
            s.op("dve", lambda e: e.memset(mu[:, 52:53], 0.0), writes=[t_c])
            s.dma("sp", mu[0:32, 52:53], mu_d[0, 52 * 64:RSW].rearrange("(p o) -> p o", o=1), writes=[t_c],
                  allow_slow_non_contiguous=True)
            for tile_, d_ in ((w0, w0_d), (a0, a0_d), (k_k, kk_d), (k_a, ka_d), (r_k, rk_d)):
                s.dma("sp", tile_[:, :], d_[0, :].rearrange("(j p) -> p j", p=64), writes=[t_c],
                      allow_slow_non_contiguous=True)
            s.dma("sp", lgb[:, :], lg_d[0:1, :].partition_broadcast(64), writes=[t_c])
            s.dma("sp", lbb[:, :], lb_d[0:1, :].partition_broadcast(64), writes=[t_c])
            s.dma("sp", ww2[:, :], ww2_d[:, :], writes=[t_c])
            s.dma("sp", wa2[:, :], wa2_d[:, :], writes=[t_c])
            s.dma("sp", wg2[:, 0, :], wg2_d[0:64, :], writes=[t_c])
            s.dma("sp", wg2[:, 1, :], wg2_d[64:128, :], writes=[t_c])
            s.dma("sp", wg2[0:32, 2, :], wg2_d[128:160, :], writes=[t_c])
            t_S0, t_xm = T(), T()
            t_Xs = [T(), T()]; t_Xps = [T(), T()]
            t_txws = [T(), T()]; t_sxgs = [T(), T()]
            t_STs = [T() for _ in range(NG)]
            self.rps = 0

            psv = []
            for i_ in range(8):
                psv.append((ps[i_], t_ps[i_]))
            psh = []
            for i_ in range(8):
                for j_ in range(2):
                    psh.append((ps[i_][:, j_ * 256:(j_ + 1) * 256], T()))
            self.rph = 0

            def nps():
                i = self.rps
                self.rps = (i + 1) % 8
                return psv[i]

            def nph():
                if "halfbank" not in (self.phases or ()):
                    return nps()
                i = self.rph
                self.rph = (i + 1) % 16
                return psh[i]

            def v3g(p_):
                return p_[0:64, 0:HG * 64].rearrange("p (a b) -> p a b", b=64)

            def v3(p_):
                return p_[0:64, :].rearrange("p (a b) -> p a b", b=64)

            def bc(ap2, n):
                return ap2.unsqueeze(2).to_broadcast([64, ap2.shape[1], n])

            def dve(fn, reads, writes):
                s.op("dve", fn, reads, writes)

            def act(fn, reads, writes):
                s.op("act", fn, reads, writes)

            def mm8(lhs, t_l, rhs, t_r, lslice=None):
                p_, t_p = nps()
                for hh in range(8):
                    s.op("pe", lambda e: e.matmul(p_[0:64, hh * 64:(hh + 1) * 64], lhsT=lhs(hh), rhs=rhs(hh),
                                                  start=True, stop=True),
                         reads=t_l + t_r, writes=[t_p])
                return p_, t_p

            def mm8h(lhs, t_l, rhs, t_r):
                p_, t_p = nph()
                for hh in range(HG):
                    s.op("pe", lambda e: e.matmul(p_[0:64, hh * 64:(hh + 1) * 64], lhsT=lhs(hh), rhs=rhs(hh),
                                                  start=True, stop=True),
                         reads=t_l + t_r, writes=[t_p])
                return p_, t_p

            for si, (tok0, Lq, ci) in enumerate(seqs):
                if ci is None:
                    dve(lambda e: e.memset(ST[:, :, :], 0.0), [], t_STs)
                else:
                    s.dma("sp", S0[:, :, :], state_wkv[ci].rearrange("h v k -> v h k"), writes=[t_S0])
                    for g in range(2):
                        p_, t_p = nps()
                        for hh in range(8):
                            s.op("pe", lambda e: e.matmul(p_[0:64, hh * 64:(hh + 1) * 64], lhsT=S0[:, g * 8 + hh, :],
                                                          rhs=ident[0:64, 0:64], start=True, stop=True),
                                 reads=[t_S0, t_c], writes=[t_p])
                        dve(lambda e: e.tensor_copy(out=ST[:, g * 8:g * 8 + 8, :], in_=v3(p_)), [t_p], t_STs)
                def load_x(c):
                    t0 = tok0 + c * C
                    X, t_X = Xs[c % 2], t_Xs[c % 2]
                    for j0 in range(0, 53, 14):
                        j1 = min(53, j0 + 14)
                        if j1 == 53:
                            s.dma("sp", X[:, j0:52, 1:C + 1],
                                  rwT_d[j0:52, :, t0:t0 + C].rearrange("j p t -> p j t"), writes=[t_X])
                            s.dma("sp", X[0:32, 52, 1:C + 1], rwT_d[52, 0:32, t0:t0 + C], writes=[t_X])
                        else:
                            s.dma("sp", X[:, j0:j1, 1:C + 1],
                                  rwT_d[j0:j1, :, t0:t0 + C].rearrange("j p t -> p j t"), writes=[t_X])

                def prologue(c):
                    t0 = tok0 + c * C
                    txw, sxg, t_txw, t_sxg = txws[c % 2], sxgs[c % 2], t_txws[c % 2], t_sxgs[c % 2]
                    X, t_X, t_Xp = Xs[c % 2], t_Xs[c % 2], t_Xps[c % 2]
                    Xo, t_Xo = Xs[(c + 1) % 2], t_Xs[(c + 1) % 2]
                    if c == 0:
                        load_x(0)
                    if c == 0:
                        if ci is None:
                            dve(lambda e: e.memset(X[:, :, 0:1], 0.0), [], [t_Xp])
                        else:
                            dve(lambda e: e.memset(X[:, :, 0:1], 0.0), [], [t_Xp])
                            for j0 in range(0, 52, 13):
                                s.dma("sp", X[:, j0:j0 + 13, 0],
                                      state_shift[ci, j0 * 64:(j0 + 13) * 64].rearrange("(j p) -> p j", p=64),
                                      writes=[t_Xp], allow_slow_non_contiguous=True)
                            s.dma("sp", X[0:32, 52, 0:1], state_shift[ci, 52 * 64:RSW].rearrange("(p o) -> p o", o=1),
                                  writes=[t_Xp], allow_slow_non_contiguous=True)
                    else:
                        dve(lambda e: e.tensor_copy(out=X[:, :, 0:1], in_=Xo[:, :, C:C + 1]), [t_Xo], [t_Xp])
                    if c + 1 < Lq // C:
                        load_x(c + 1)
                    if c == 0 and si == 0:
                        pass
                    for ja, jb in ((0, 14), (14, 28), (28, 42), (42, 53)):
                        nj = jb - ja
                        dve(lambda e: e.tensor_tensor(out=xm[:, ja:jb, :], in0=X[:, ja:jb, 0:C], in1=X[:, ja:jb, 1:C + 1],
                                                      op=ALU.subtract), [t_X, t_Xp], [t_xm])
                        yield
                        dve(lambda e: e.tensor_tensor(out=xm[:, ja:jb, :], in0=xm[:, ja:jb, :],
                                                      in1=mu[:, ja:jb].unsqueeze(2).to_broadcast([64, nj, C]),
                                                      op=ALU.mult), [t_xm, t_c], [t_xm])
                        yield
                        dve(lambda e: e.tensor_tensor(out=xm[:, ja:jb, :], in0=xm[:, ja:jb, :], in1=X[:, ja:jb, 1:C + 1],
                                                      op=ALU.add), [t_xm, t_X], [t_xm])
                        yield
                    act(lambda e: e.activation(out=txw[:, :], in_=xm[:, 48, :], func=AF.Tanh), [t_xm], [t_txw])
                    act(lambda e: e.activation(out=sxg[:, :, :], in_=xm[:, 50:53, :], func=AF.Sigmoid),
                        [t_xm], [t_sxg])

                def body(c, g):
                    t0 = tok0 + c * C
                    txw, sxg, t_txw, t_sxg = txws[c % 2], sxgs[c % 2], t_txws[c % 2], t_sxgs[c % 2]
                    h0 = g * HG
                    A, tt, st8, rwoT = tls[g], tts[g], st8s[g], rwoTs[g]
                    t_ST, t_st8, t_rwoT = t_STs[g], t_st8s[g], t_rwoTs[g]
                    r_ = xm[:, h0:h0 + HG, :]
                    k_ = xm[:, 16 + h0:16 + h0 + HG, :]
                    v_ = xm[:, 32 + h0:32 + h0 + HG, :]
                    yield
                    p_, t_p = mm8h(lambda hh: ww2[:, (h0 + hh) * 64:(h0 + hh + 1) * 64], [t_c],
                                  lambda hh: txw[:, :], [t_txw])
                    for hh in range(HG):
                        act(lambda e: e.activation(out=A["lw"][:, hh, :], in_=p_[0:64, hh * 64:(hh + 1) * 64],
                                                   func=AF.Sigmoid, bias=w0[:, h0 + hh:h0 + hh + 1]),
                            [t_p, t_c], [tt["lw"]])
                    yield
                    p_, t_p = mm8h(lambda hh: wa2[:, (h0 + hh) * 64:(h0 + hh + 1) * 64], [t_c],
                                  lambda hh: xm[:, 49, :], [t_xm])
                    for hh in range(HG):
                        act(lambda e: e.activation(out=A["a"][:, hh, :], in_=p_[0:64, hh * 64:(hh + 1) * 64],
                                                   func=AF.Sigmoid, bias=a0[:, h0 + hh:h0 + hh + 1]),
                            [t_p, t_c], [tt["a"]])
                    dve(lambda e: e.tensor_tensor(out=A["kkn"][:, :, :], in0=k_, in1=bc(k_k[:, h0:h0 + HG], C),
                                                  op=ALU.mult), [t_xm, t_c], [tt["kkn"]])
                    dve(lambda e: e.tensor_tensor(out=A["tmp"][:, :, :], in0=A["kkn"][:, :, :],
                                                  in1=A["kkn"][:, :, :], op=ALU.mult), [tt["kkn"]], [tt["tmp"]])
                    yield
                    yield
                    p_, t_p = nph()
                    s.op("pe", lambda e: e.matmul(p_[0:64, 0:HG * 64], lhsT=ones[:, :],
                                                  rhs=A["tmp"][:, :, :].rearrange("p a b -> p (a b)"),
                                                  start=True, stop=True), [t_c, tt["tmp"]], [t_p])
                    act(lambda e: e.activation(out=A["tmp"][:, :, :], in_=v3g(p_), func=AF.Ln, bias=1e-24),
                        [t_p], [tt["tmp"]])
                    act(lambda e: e.activation(out=A["tmp"][:, :, :], in_=A["tmp"][:, :, :], func=AF.Exp, scale=-0.5),
                        [tt["tmp"]], [tt["tmp"]])
                    yield
                    dve(lambda e: e.tensor_tensor(out=A["kkn"][:, :, :], in0=A["kkn"][:, :, :],
                                                  in1=A["tmp"][:, :, :], op=ALU.mult),
                        [tt["kkn"], tt["tmp"]], [tt["kkn"]])
                    dve(lambda e: e.scalar_tensor_tensor(out=A["tmp"][:, :, :], in0=A["a"][:, :, :], scalar=-1.0,
                                                         in1=bc(k_a[:, h0:h0 + HG], C), op0=ALU.add,
                                                         op1=ALU.mult), [tt["a"], t_c], [tt["tmp"]])
                    yield
                    dve(lambda e: e.scalar_tensor_tensor(out=A["krep"][:, :, :], in0=A["tmp"][:, :, :], scalar=1.0,
                                                         in1=k_, op0=ALU.add, op1=ALU.mult),
                        [tt["tmp"], t_xm], [tt["krep"]])
                    for hh in range(HG):
                        dve(lambda e: e.tensor_tensor_scan(out=A["Lc"][:, hh, :], data0=ones[:, 0:C],
                                                           data1=A["lw"][:, hh, :], initial=0.0,
                                                           op0=ALU.mult, op1=ALU.add),
                            [tt["lw"], t_c], [tt["Lc"]])
                    act(lambda e: e.activation(out=A["Ep"][:, :, :], in_=A["Lc"][:, :, :], func=AF.Exp,
                                               scale=-DECAY_SCALE), [tt["Lc"]], [tt["Ep"]])
                    yield
                    act(lambda e: e.activation(out=A["En"][:, :, :], in_=A["Lc"][:, :, :], func=AF.Exp,
                                               scale=DECAY_SCALE), [tt["Lc"]], [tt["En"]])
                    dve(lambda e: e.tensor_tensor(out=A["rt"][:, :, :], in0=r_, in1=A["Ep"][:, :, :], op=ALU.mult),
                        [t_xm, tt["Ep"]], [tt["rt"]])
                    yield
                    dve(lambda e: e.tensor_tensor(out=A["Lc"][:, :, :], in0=A["Lc"][:, :, :], in1=A["lw"][:, :, :],
                                                  op=ALU.subtract), [tt["Lc"], tt["lw"]], [tt["Lc"]])
                    act(lambda e: e.activation(out=A["Lc"][:, :, :], in_=A["Lc"][:, :, :], func=AF.Exp,
                                               scale=-DECAY_SCALE), [tt["Lc"]], [tt["Lc"]])
                    yield
                    dve(lambda e: e.scalar_tensor_tensor(out=A["Lc"][:, :, :], in0=A["kkn"][:, :, :], scalar=-1.0,
                                                         in1=A["Lc"][:, :, :], op0=ALU.mult, op1=ALU.mult),
                        [tt["kkn"], tt["Lc"]], [tt["Lc"]])
                    at, t_at = A["Lc"], tt["Lc"]
                    dve(lambda e: e.tensor_tensor(out=A["bh"][:, :, :], in0=A["a"][:, :, :], in1=A["kkn"][:, :, :],
                                                  op=ALU.mult), [tt["a"], tt["kkn"]], [tt["bh"]])
                    yield
                    dve(lambda e: e.tensor_tensor(out=A["bh"][:, :, :], in0=A["bh"][:, :, :], in1=A["En"][:, :, :],
                                                  op=ALU.mult), [tt["bh"], tt["En"]], [tt["bh"]])
                    dve(lambda e: e.tensor_tensor(out=A["En"][:, :, :], in0=A["krep"][:, :, :],
                                                  in1=A["En"][:, :, :], op=ALU.mult),
                        [tt["krep"], tt["En"]], [tt["En"]])
                    yield
                    kh, t_kh = A["En"], tt["En"]
                    gC = A["Ep"][:, :, C - 1]
                    dve(lambda e: e.tensor_tensor(out=A["bg"][:, :, :], in0=A["bh"][:, :, :], in1=bc(gC, C),
                                                  op=ALU.mult), [tt["bh"], tt["Ep"]], [tt["bg"]])
                    dve(lambda e: e.tensor_tensor(out=A["kg"][:, :, :], in0=kh[:, :, :], in1=bc(gC, C),
                                                  op=ALU.mult), [t_kh, tt["Ep"]], [tt["kg"]])
                    yield
                    dve(lambda e: e.tensor_tensor(out=A["rkp"][:, :, :], in0=r_, in1=A["krep"][:, :, :],
                                                  op=ALU.mult), [t_xm, tt["krep"]], [tt["rkp"]])
                    dve(lambda e: e.tensor_tensor(out=A["rkp"][:, :, :], in0=A["rkp"][:, :, :],
                                                  in1=bc(r_k[:, h0:h0 + HG], C), op=ALU.mult),
                        [tt["rkp"], t_c], [tt["rkp"]])
                    yield
                    for src, t_src, dst in ((lambda hh: v_[:, hh, :], t_xm, "Vtok"),
                                            (lambda hh: A["bg"][:, hh, :], tt["bg"], "bgT"),
                                            (lambda hh: A["kg"][:, hh, :], tt["kg"], "kgT")):
                        yield
                        p_, t_p = mm8h(src, [t_src], lambda hh: ident[0:64, 0:64], [t_c])
                        act(lambda e: e.activation(out=A[dst][:, :, :], in_=v3g(p_), func=AF.Copy),
                            [t_p], [tt[dst]])
                    yield "PREP_DONE"
                    for lh, t_lh, rh, t_rh, msk, dst in (
                            (A["bh"], tt["bh"], at, t_at, msu, "N"),
                            (at, t_at, A["bh"], tt["bh"], msl, "NT"),
                            (kh, t_kh, at, t_at, msu, "AK"),
                            (kh, t_kh, A["rt"], tt["rt"], mu8, "QK"),
                            (A["bh"], tt["bh"], A["rt"], tt["rt"], mu8, "QB")):
                        yield
                        p_, t_p = mm8h(lambda hh: lh[:, hh, :], [t_lh], lambda hh: rh[:, hh, :], [t_rh])
                        dve(lambda e: e.tensor_tensor(out=A[dst][:, :, :], in0=v3g(p_), in1=msk[:, 0:HG, :],
                                                      op=ALU.mult), [t_p, t_c], [tt[dst]])
                    dve(lambda e: e.tensor_tensor(out=A["X0"][:, :, :], in0=A["N"][:, :, :], in1=i8[:, 0:HG, :],
                                                  op=ALU.add), [tt["N"], t_c], [tt["X0"]])
                    Pn, PTn, Xn = "N", "NT", "X0"
                    for lvl in range(1, 6):
                        Pd, PTd, Xd = ("P0", "PT0", "X1") if lvl % 2 else ("P1", "PT1", "X0")
                        if lvl < 5:
                            yield
                            p_, t_p = mm8h(lambda hh: A[PTn][:, hh, :], [tt[PTn]],
                                          lambda hh: A[Pn][:, hh, :], [tt[Pn]])
                            act(lambda e: e.activation(out=A[Pd][:, :, :], in_=v3g(p_), func=AF.Copy),
                                [t_p], [tt[Pd]])
                        yield
                        p_, t_p = mm8h(lambda hh: A[Pn][:, hh, :], [tt[Pn]],
                                      lambda hh: A[PTn][:, hh, :], [tt[PTn]])
                        act(lambda e: e.activation(out=A[PTd][:, :, :], in_=v3g(p_), func=AF.Copy), [t_p], [tt[PTd]])
                        yield
                        p_, t_p = mm8h(lambda hh: A[PTd][:, hh, :], [tt[PTd]],
                                      lambda hh: A[Xn][:, hh, :], [tt[Xn]])
                        dve(lambda e: e.tensor_tensor(out=A[Xd][:, :, :], in0=v3g(p_), in1=A[Xn][:, :, :],
                                                      op=ALU.add), [t_p, tt[Xn]], [tt[Xd]])
                        Pn, PTn, Xn = Pd, PTd, Xd
                    yield
                    p_, t_p = nph()
                    for hh in range(HG):
                        o_ = p_[0:64, hh * 64:(hh + 1) * 64]
                        s.op("pe", lambda e: e.matmul(o_, lhsT=at[:, hh, :], rhs=ST[:, h0 + hh, :],
                                                      start=True, stop=False), [t_at, t_ST], [t_p])
                        s.op("pe", lambda e: e.matmul(o_, lhsT=A["AK"][:, hh, :], rhs=A["Vtok"][:, hh, :],
                                                      start=False, stop=True), [tt["AK"], tt["Vtok"]], [t_p])
                    act(lambda e: e.activation(out=A["W0"][:, :, :], in_=v3g(p_), func=AF.Copy), [t_p], [tt["W0"]])
                    yield
                    yield
                    p_, t_p = mm8h(lambda hh: A[Xn][:, hh, :], [tt[Xn]], lambda hh: A["W0"][:, hh, :], [tt["W0"]])
                    act(lambda e: e.activation(out=A["U"][:, :, :], in_=v3g(p_), func=AF.Copy), [t_p], [tt["U"]])
                    yield
                    pY, t_pY = nph()
                    for hh in range(HG):
                        o_ = pY[0:64, hh * 64:(hh + 1) * 64]
                        s.op("pe", lambda e: e.matmul(o_, lhsT=A["rt"][:, hh, :], rhs=ST[:, h0 + hh, :],
                                                      start=True, stop=False), [tt["rt"], t_ST], [t_pY])
                        s.op("pe", lambda e: e.matmul(o_, lhsT=A["QK"][:, hh, :], rhs=A["Vtok"][:, hh, :],
                                                      start=False, stop=False), [tt["QK"], tt["Vtok"]], [t_pY])
                        s.op("pe", lambda e: e.matmul(o_, lhsT=A["QB"][:, hh, :], rhs=A["U"][:, hh, :],
                                                      start=False, stop=True), [tt["QB"], tt["U"]], [t_pY])
                    yield
                    pS, t_pS = nph()
                    for hh in range(HG):
                        o_ = pS[0:64, hh * 64:(hh + 1) * 64]
                        s.op("pe", lambda e: e.matmul(o_, lhsT=A["bgT"][:, hh, :], rhs=A["U"][:, hh, :],
                                                      start=True, stop=False), [tt["bgT"], tt["U"]], [t_pS])
                        s.op("pe", lambda e: e.matmul(o_, lhsT=A["kgT"][:, hh, :], rhs=A["Vtok"][:, hh, :],
                                                      start=False, stop=True), [tt["kgT"], tt["Vtok"]], [t_pS])
                    dve(lambda e: e.tensor_tensor(out=ST[:, h0:h0 + HG, :], in0=ST[:, h0:h0 + HG, :], in1=bc(gC, 64),
                                                  op=ALU.mult), [t_ST, tt["Ep"]], [t_ST])
                    yield
                    dve(lambda e: e.tensor_tensor(out=ST[:, h0:h0 + HG, :], in0=ST[:, h0:h0 + HG, :], in1=v3g(pS),
                                                  op=ALU.add), [t_ST, t_pS], [t_ST])
                    dve(lambda e: e.tensor_reduce(out=st8[:, 0:HG], in_=v3g(pY), axis=AX.X, op=ALU.add),
                        [t_pY], [t_st8])
                    yield
                    dve(lambda e: e.tensor_scalar(out=st8[:, 0:HG], in0=st8[:, 0:HG], scalar1=1.0 / 64,
                                                  scalar2=None, op0=ALU.mult), [t_st8], [t_st8])
                    dve(lambda e: e.tensor_tensor(out=A["yc"][:, :, :], in0=v3g(pY), in1=bc(st8[:, 0:HG], 64),
                                                  op=ALU.subtract), [t_pY, t_st8], [tt["yc"]])
                    yield
                    dve(lambda e: e.tensor_tensor(out=A["sq"][:, :, :], in0=A["yc"][:, :, :], in1=A["yc"][:, :, :],
                                                  op=ALU.mult), [tt["yc"]], [tt["sq"]])
                    dve(lambda e: e.tensor_reduce(out=st8[:, 8:8 + HG], in_=A["sq"][:, :, :], axis=AX.X, op=ALU.add),
                        [tt["sq"]], [t_st8])
                    yield
                    act(lambda e: e.activation(out=st8[:, 16:16 + HG], in_=st8[:, 8:8 + HG], func=AF.Sqrt, scale=1.0 / 64,
                                               bias=GN_EPS), [t_st8], [t_st8])
                    dve(lambda e: e.reciprocal(out=st8[:, 24:24 + HG], in_=st8[:, 16:16 + HG]), [t_st8], [t_st8])
                    yield
                    dve(lambda e: e.tensor_tensor(out=A["yc"][:, :, :], in0=A["yc"][:, :, :],
                                                  in1=bc(st8[:, 24:24 + HG], 64), op=ALU.mult),
                        [tt["yc"], t_st8], [tt["yc"]])
                    lg3 = lgb[:, h0 * 64:(h0 + HG) * 64].rearrange("p (a b) -> p a b", b=64)
                    lb3 = lbb[:, h0 * 64:(h0 + HG) * 64].rearrange("p (a b) -> p a b", b=64)
                    dve(lambda e: e.tensor_tensor(out=A["yc"][:, :, :], in0=A["yc"][:, :, :], in1=lg3, op=ALU.mult),
                        [tt["yc"], t_c], [tt["yc"]])
                    yield
                    dve(lambda e: e.tensor_tensor(out=A["yc"][:, :, :], in0=A["yc"][:, :, :], in1=lb3, op=ALU.add),
                        [tt["yc"], t_c], [tt["yc"]])
                    yield
                    pC, t_pC = nph()
                    for hh in range(HG):
                        s.op("pe", lambda e: e.matmul(pC[0:64, hh * 2:hh * 2 + 2], lhsT=A["rkp"][:, hh, :],
                                                      rhs=ones[:, 0:2], start=True, stop=True),
                             [tt["rkp"], t_c], [t_pC])
                    dve(lambda e: e.tensor_copy(out=st8[:, 0:HG], in_=pC[0:64, 0:2 * HG:2]), [t_pC], [t_st8])
                    yield
                    dve(lambda e: e.tensor_tensor(out=A["sq"][:, :, :], in0=A["Vtok"][:, :, :],
                                                  in1=bc(st8[:, 0:HG], 64), op=ALU.mult),
                        [tt["Vtok"], t_st8], [tt["sq"]])
                    dve(lambda e: e.tensor_tensor(out=A["yc"][:, :, :], in0=A["yc"][:, :, :], in1=A["sq"][:, :, :],
                                                  op=ALU.add), [tt["yc"], tt["sq"]], [tt["yc"]])
                    yield
                    yield
                    pG, t_pG = nph()
                    for part, kp in ((0, 64), (1, 64), (2, 32)):
                        s.op("pe", lambda e: e.matmul(pG[0:64, 0:HG * 64], lhsT=sxg[0:kp, part, :],
                                                      rhs=wg2[0:kp, part, h0 * 64:(h0 + HG) * 64],
                                                      start=(part == 0), stop=(part == 2)),
                             [t_sxg, t_c], [t_pG])
                    dve(lambda e: e.tensor_tensor(out=A["rwtok"][:, :, :], in0=v3g(pG), in1=A["yc"][:, :, :],
                                                  op=ALU.mult), [t_pG, tt["yc"]], [tt["rwtok"]])
                    yield
                    pT, t_pT = nph()
                    for jj in range(HG // 2):
                        s.op("pe", lambda e: e.matmul(pT[:, jj * 64:(jj + 1) * 64],
                                                      lhsT=A["rwtok"][:, 2 * jj:2 * jj + 2, :].rearrange(
                                                          "p a b -> p (a b)"),
                                                      rhs=ident[0:64, 0:64], start=True, stop=True),
                             [tt["rwtok"], t_c], [t_pT])
                    act(lambda e: e.activation(out=rwoT[:, :, :].rearrange("p a b -> p (a b)"), in_=pT[:, 0:(HG // 2) * 64],
                                               func=AF.Copy), [t_pT], [t_rwoT])
                    yield
                    s.dma(STQ, rwoT_d[(HG // 2) * g:(HG // 2) * (g + 1), :, t0:t0 + C].rearrange("j p t -> p j t"),
                          rwoT[:, :, :], reads=[t_rwoT])
                order = [(c_, g_) for c_ in range(Lq // C) for g_ in range(NG)]
                active = []
                running = set()
                state = {"oi": 0}
                pro_started, pro_done = set(), set()

                def try_start():
                    oi_ = state["oi"]
                    if oi_ >= len(order):
                        return False
                    c_, g_ = order[oi_]
                    bodies = [e_ for e_ in active if e_[1] != "P"]
                    if g_ == 0 and c_ not in pro_started:
                        if any(not e_[2] for e_ in bodies):
                            return False
                        active.append([prologue(c_), "P", True, c_])
                        pro_started.add(c_)
                        return True
                    if g_ == 0 and c_ not in pro_done:
                        return False
                    if g_ in running:
                        return False
                    if any(not e_[2] for e_ in bodies):
                        return False
                    if len(bodies) >= 2:
                        return False
                    active.append([body(c_, g_), g_, False])
                    running.add(g_)
                    state["oi"] = oi_ + 1
                    return True

                try_start()
                while active:
                    for ent in list(active):
                        try:
                            v_ = next(ent[0])
                            if v_ == "PREP_DONE":
                                ent[2] = True
                        except StopIteration:
                            active.remove(ent)
                            if ent[1] == "P":
                                pro_done.add(ent[3])
                            else:
                                running.discard(ent[1])
                        try_start()
                    if not active:
                        try_start()
                for g in range(2):
                    p_, t_p = nps()
                    for hh in range(8):
                        s.op("pe", lambda e: e.matmul(p_[0:64, hh * 64:(hh + 1) * 64], lhsT=ST[:, g * 8 + hh, :],
                                                      rhs=ident[0:64, 0:64], start=True, stop=True),
                             t_STs + [t_c], [t_p])
                    dve(lambda e: e.tensor_copy(out=S0[:, g * 8:g * 8 + 8, :], in_=v3(p_)), [t_p], [t_S0])
                s.dma(STQ, wkv_out[si].rearrange("h v k -> v h k"), S0[:, :, :], reads=[t_S0])
            s.barrier()


    CAPB = 4
    CAPR = 512
    NSLOT = NEXP * 512
    BIG = 1.0e6

    def breg(self):
        if not hasattr(self, "_breg"):
            self._breg = self.nc.gpsimd.to_reg(self.NSLOT - 1)
        return self._breg

    def groups(self):
        L, LS = self.L, self.LS
        gs = []
        for sq in range(2):
            for g0 in range(0, L, 512):
                gs.append((sq * L + g0, min(512, L - g0)))
        gs.append((2 * L, 2 * LS))
        return gs

    def phase_merge(self):
        nc, s = self.nc, self.s
        L, LS, PAST, TT = self.L, self.LS, self.PAST, self.TT
        wfu_d = self.din("w_fox_up", [FW, D])
        wru_d = self.din("w_rwkv_up", [RW, D])
        wo_d = self.din("w_o", [D, D])
        gffn_d = self.din("g_ffn", [1, D])
        wrg_d = self.din("w_rg", [D, 4])
        brg_d = self.din("b_rg", [1, 4])
        wre_d = self.din("w_re", [D, NEXP])
        bre_d = self.din("b_re", [1, NEXP])
        slt_d = self.din("slt", [128, 128])
        ebase_d = self.din("ebase", [128, NEXP])
        ones_d = self.cin("ones", [128, 128])
        x2_d = self.x2_d = self.dscr("x2_d", [TT, D])
        xs_d = self.xs_d = self.dscr("xs_d", [self.NSLOT, D])
        if "dbgmerge" in (self.phases or ()):
            x2_d = self.x2_d = self.dout("dbg_x2", [TT, D])
            self.dbg_gates = self.dout("dbg_gates", [128, TT // 128, 2])
            self.dbg_dest = self.dout("dbg_dest", [128, TT // 128, 2], I32)
            xs_d = self.xs_d = self.dout("dbg_xs", [self.NSLOT, D])
        NTt = TT // 128
        self.gates_all = nc.alloc_sbuf_tensor("gates_all", [128, NTt, 2], F32)
        self.dest_all = nc.alloc_sbuf_tensor("dest_all", [128, NTt, 2], I32)
        self.t_route = T("route")
        gates_all, dest_all, t_route = self.gates_all, self.dest_all, self.t_route
        with ExitStack() as es:
            def sb(name, shape, dt=F32):
                return es.enter_context(nc.sbuf_tensor("m_" + name, shape, dt))
            ident = sb("ident", [128, 128]); slt = sb("slt", [128, 128]); onesf = sb("onesf", [128, 128])
            ebase = sb("ebase", [128, NEXP]); gfbc = sb("gfbc", [128, D]); brbc = sb("brbc", [128, 36])
            wr = sb("wr", [128, 16, 36]); cnt = sb("cnt", [128, NEXP])
            foxT = sb("foxT", [128, 8, 512], F32R); rwoT = sb("rwoT", [128, 8, 512], F32R)
            wfu = [sb(f"wfu{i}", [128, 8, 128], F32R) for i in range(2)]
            wru = [sb(f"wru{i}", [128, 8, 128], F32R) for i in range(2)]
            sgf = [sb(f"sgf{i}", [128, 512]) for i in range(2)]
            sgr = [sb(f"sgr{i}", [128, 512]) for i in range(2)]
            tmpA = sb("tmpA", [128, 512]); tmpB = sb("tmpB", [128, 512])
            mT = sb("mT", [128, 16, 512], F32R)
            X2 = sb("X2", [128, 4, D])
            wo = [sb(f"wo{i}", [128, 16, 256], F32R) for i in range(2)]
            H2s = [sb(f"H2{i}", [128, D]) for i in range(2)]; h2T = sb("h2T", [128, 16, 128])
            junk = h2T[:, :, :].rearrange("p a b -> p (a b)")
            lg = sb("lg", [128, 36]); G = sb("G", [128, 4]); pen = sb("pen", [128, 4]); ge = sb("ge", [128, 4])
            ml = sb("ml", [128, NEXP]); ml2 = sb("ml2", [128, NEXP]); oh0 = sb("oh0", [128, NEXP])
            oh1 = sb("oh1", [128, NEXP]); oh = sb("oh", [128, NEXP]); pos = sb("pos", [128, NEXP])
            t32 = sb("t32", [128, NEXP]); m = sb("m", [128, 32])
            ps, t_ps = self.ps, self.t_ps
            t_c = T("mconsts")
            s.dma("sp", ident[:], self.ident_d[:, :], writes=[t_c])
            s.dma("sp", slt[:], slt_d[:, :], writes=[t_c])
            s.dma("sp", onesf[:], ones_d[:, :], writes=[t_c])
            s.dma("sp", ebase[:], ebase_d[:, :], writes=[t_c])
            s.dma("sp", gfbc[:], gffn_d[0:1, :].partition_broadcast(128), writes=[t_c])
            s.dma("sp", brbc[:, 0:4], brg_d[0:1, :].partition_broadcast(128), writes=[t_c])
            s.dma("sp", brbc[:, 4:36], bre_d[0:1, :].partition_broadcast(128), writes=[t_c])
            s.dma("sp", wr[:, :, 0:4], wrg_d[:, :].rearrange("(kc p) g -> p kc g", p=128), writes=[t_c])
            s.dma("sp", wr[:, :, 4:36], wre_d[:, :].rearrange("(kc p) g -> p kc g", p=128), writes=[t_c])
            t_cnt = T()
            s.op("dve", lambda e: e.memset(cnt[:, :], 0.0), writes=[t_cnt])
            t_foxT, t_rwoT, t_tmpA, t_tmpB, t_mT, t_X2, t_h2T, t_r = (T() for _ in range(8))
            t_H2s = [T(), T()]
            t_junk = t_h2T
            t_wfu = [T(), T()]; t_wru = [T(), T()]; t_sgf = [T(), T()]; t_sgr = [T(), T()]; t_wo = [T(), T()]
            self.mps = 0

            def nps():
                i = self.mps
                self.mps = (i + 1) % 8
                return ps[i], t_ps[i]
            wi = 0
            woi = 0
            for (tok0, ntok) in self.groups():
                ntile = ntok // 128
                s.dma("sp", foxT[:, :, 0:ntok], r32(self.foxT_d[:, :, tok0:tok0 + ntok]).rearrange("h p t -> p h t"),
                      writes=[t_foxT])
                s.dma("sp", rwoT[:, :, 0:ntok], r32(self.rwoT_d[:, :, tok0:tok0 + ntok]).rearrange("h p t -> p h t"),
                      writes=[t_rwoT])
                s.dma("sp", X2[:, 0:ntile, :], self.x_all[tok0:tok0 + ntok, :].rearrange("(a p) c -> p a c", p=128),
                      writes=[t_X2])
                for cb in range(16):
                    b = wi % 2
                    wi += 1
                    s.dma("sp", wfu[b][:, :, :], r32(wfu_d[:, cb * 128:(cb + 1) * 128]).rearrange(
                        "(kc p) c -> p kc c", p=128), writes=[t_wfu[b]])
                    s.dma("sp", wru[b][:, :, :], r32(wru_d[:, cb * 128:(cb + 1) * 128]).rearrange(
                        "(kc p) c -> p kc c", p=128), writes=[t_wru[b]])
                    s.dma("sp", sgf[b][:, 0:ntok], self.sgT_d[cb, :, tok0:tok0 + ntok], writes=[t_sgf[b]])
                    s.dma("sp", sgr[b][:, 0:ntok], self.sgT_d[16 + cb, :, tok0:tok0 + ntok], writes=[t_sgr[b]])
                    pF, t_pF = nps()
                    for kc in range(8):
                        s.op("pe", lambda e: e.matmul(pF[:, 0:ntok], lhsT=wfu[b][:, kc, :], rhs=foxT[:, kc, 0:ntok],
                                                      start=(kc == 0), stop=(kc == 7)),
                             [t_wfu[b], t_foxT], [t_pF])
                    pR, t_pR = nps()
                    for kc in range(8):
                        s.op("pe", lambda e: e.matmul(pR[:, 0:ntok], lhsT=wru[b][:, kc, :], rhs=rwoT[:, kc, 0:ntok],
                                                      start=(kc == 0), stop=(kc == 7)),
                             [t_wru[b], t_rwoT], [t_pR])
                    s.op("dve", lambda e: e.tensor_tensor(out=tmpA[:, 0:ntok], in0=pF[:, 0:ntok], in1=sgf[b][:, 0:ntok],
                                                          op=ALU.mult), [t_pF, t_sgf[b]], [t_tmpA])
                    s.op("dve", lambda e: e.tensor_tensor(out=tmpB[:, 0:ntok], in0=pR[:, 0:ntok], in1=sgr[b][:, 0:ntok],
                                                          op=ALU.mult), [t_pR, t_sgr[b]], [t_tmpB])
                    s.op("dve", lambda e: e.tensor_tensor(out=mT[:, cb, 0:ntok], in0=tmpA[:, 0:ntok],
                                                          in1=tmpB[:, 0:ntok], op=ALU.add),
                         [t_tmpA, t_tmpB], [t_mT])
                for cs in range(8):
                    b = woi % 2
                    woi += 1
                    s.dma("sp", wo[b][:, :, :], r32(wo_d[:, cs * 256:(cs + 1) * 256]).rearrange(
                        "(kc p) c -> p kc c", p=128), writes=[t_wo[b]])
                    for ti in range(ntile):
                        p_, t_p = nps()
                        for kc in range(16):
                            s.op("pe", lambda e: e.matmul(p_[:, 0:256], lhsT=mT[:, kc, ti * 128:(ti + 1) * 128],
                                                          rhs=wo[b][:, kc, :], start=(kc == 0), stop=(kc == 15)),
                                 [t_mT, t_wo[b]], [t_p])
                        s.op("dve", lambda e: e.tensor_tensor(out=X2[:, ti, cs * 256:(cs + 1) * 256], in0=p_[:, 0:256],
                                                              in1=X2[:, ti, cs * 256:(cs + 1) * 256], op=ALU.add),
                             [t_p, t_X2], [t_X2])
                for ti in range(ntile):
                    tti = (tok0 + ti * 128) // 128
                    r0 = tok0 + ti * 128
                    H2, t_H2 = H2s[tti % 2], t_H2s[tti % 2]
                    s.dma(STQ, x2_d[r0:r0 + 128, :], X2[:, ti, :], reads=[t_X2])
                    s.op("act", lambda e: e.activation(out=junk, in_=X2[:, ti, :], func=AF.Square,
                                                       accum_out=m[:, 16:17]), [t_X2], [t_junk, t_r])
                    s.op("act", lambda e: e.activation(out=m[:, 17:18], in_=m[:, 16:17], func=AF.Sqrt,
                                                       scale=1.0 / D, bias=NORM_EPS), [t_r], [t_r])
                    s.op("dve", lambda e: e.reciprocal(out=m[:, 18:19], in_=m[:, 17:18]), [t_r], [t_r])
                    s.op("dve", lambda e: e.scalar_tensor_tensor(out=H2[:, :], in0=X2[:, ti, :], scalar=m[:, 18:19],
                                                                 in1=gfbc[:, :], op0=ALU.mult, op1=ALU.mult),
                         [t_X2, t_r, t_c], [t_H2])
                    for k4 in range(4):
                        p_, t_p = nps()
                        for j in range(4):
                            kc = k4 * 4 + j
                            s.op("pe", lambda e: e.transpose(out=p_[:, j * 128:(j + 1) * 128],
                                                             in_=H2[:, kc * 128:(kc + 1) * 128], identity=ident[:, :]),
                                 [t_H2, t_c], [t_p])
                        s.op("act", lambda e: e.activation(out=h2T[:, k4 * 4:k4 * 4 + 4, :].rearrange("p a b -> p (a b)"),
                                                           in_=p_[:, :], func=AF.Copy), [t_p], [t_h2T])
                    pL, t_pL = nps()
                    for kc in range(16):
                        s.op("pe", lambda e: e.matmul(pL[:, 0:36], lhsT=h2T[:, kc, :], rhs=wr[:, kc, :],
                                                      start=(kc == 0), stop=(kc == 15)), [t_h2T, t_c], [t_pL])
                    dv = lambda fn, rd, wrt: s.op("dve", fn, rd, wrt)
                    dv(lambda e: e.tensor_tensor(out=lg[:, :], in0=pL[:, 0:36], in1=brbc[:, :], op=ALU.add),
                       [t_pL, t_c], [t_r])
                    dv(lambda e: e.tensor_reduce(out=m[:, 0:1], in_=lg[:, 0:4], axis=AX.X, op=ALU.max), [t_r], [t_r])
                    dv(lambda e: e.tensor_scalar(out=G[:, :], in0=lg[:, 0:4], scalar1=m[:, 0:1], scalar2=None,
                                                 op0=ALU.is_equal), [t_r], [t_r])
                    dv(lambda e: e.tensor_scalar(out=m[:, 1:2], in0=m[:, 0:1], scalar1=-1.0, scalar2=None,
                                                 op0=ALU.mult), [t_r], [t_r])
                    s.op("act", lambda e: e.activation(out=ge[:, :], in_=lg[:, 0:4], func=AF.Exp, bias=m[:, 1:2],
                                                       accum_out=m[:, 2:3]), [t_r], [t_r])
                    dv(lambda e: e.reciprocal(out=m[:, 3:4], in_=m[:, 2:3]), [t_r], [t_r])
                    dv(lambda e: e.tensor_scalar(out=pen[:, :], in0=G[:, :], scalar1=-1.0, scalar2=1.0e30,
                                                 op0=ALU.add, op1=ALU.mult), [t_r], [t_r])
                    dv(lambda e: e.tensor_tensor(out=ml[:, :].rearrange("p (a b) -> p a b", b=8),
                                                 in0=lg[:, 4:36].rearrange("p (a b) -> p a b", b=8),
                                                 in1=pen[:, :].unsqueeze(2).to_broadcast([128, 4, 8]), op=ALU.add),
                       [t_r], [t_r])
                    dv(lambda e: e.tensor_reduce(out=m[:, 4:5], in_=ml[:, :], axis=AX.X, op=ALU.max), [t_r], [t_r])
                    dv(lambda e: e.tensor_scalar(out=oh0[:, :], in0=ml[:, :], scalar1=m[:, 4:5], scalar2=None,
                                                 op0=ALU.is_equal), [t_r], [t_r])
                    dv(lambda e: e.scalar_tensor_tensor(out=ml2[:, :], in0=oh0[:, :], scalar=-1.0e30, in1=ml[:, :],
                                                        op0=ALU.mult, op1=ALU.add), [t_r], [t_r])
                    dv(lambda e: e.tensor_reduce(out=m[:, 5:6], in_=ml2[:, :], axis=AX.X, op=ALU.max), [t_r], [t_r])
                    dv(lambda e: e.tensor_scalar(out=oh1[:, :], in0=ml2[:, :], scalar1=m[:, 5:6], scalar2=None,
                                                 op0=ALU.is_equal), [t_r], [t_r])
                    dv(lambda e: e.tensor_tensor(out=m[:, 6:7], in0=m[:, 5:6], in1=m[:, 4:5], op=ALU.subtract),
                       [t_r], [t_r])
                    s.op("act", lambda e: e.activation(out=m[:, 7:8], in_=m[:, 6:7], func=AF.Exp), [t_r], [t_r])
                    dv(lambda e: e.tensor_scalar(out=m[:, 8:9], in0=m[:, 7:8], scalar1=1.0, scalar2=None,
                                                 op0=ALU.add), [t_r], [t_r])
                    dv(lambda e: e.reciprocal(out=m[:, 9:10], in_=m[:, 8:9]), [t_r], [t_r])
                    dv(lambda e: e.tensor_tensor(out=gates_all[:, tti, 0:1], in0=m[:, 3:4], in1=m[:, 9:10],
                                                 op=ALU.mult), [t_r], [t_route])
                    dv(lambda e: e.tensor_tensor(out=gates_all[:, tti, 1:2], in0=gates_all[:, tti, 0:1],
                                                 in1=m[:, 7:8], op=ALU.mult), [t_r, t_route], [t_route])
                    dv(lambda e: e.tensor_tensor(out=oh[:, :], in0=oh0[:, :], in1=oh1[:, :], op=ALU.add), [t_r], [t_r])
                    pP, t_pP = nps()
                    s.op("pe", lambda e: e.matmul(pP[:, 0:NEXP], lhsT=slt[:, :], rhs=oh[:, :], start=True, stop=True),
                         [t_c, t_r], [t_pP])
                    s.op("pe", lambda e: e.matmul(pP[:, 64:64 + NEXP], lhsT=onesf[:, :], rhs=oh[:, :],
                                                  start=True, stop=True), [t_c, t_r], [t_pP])
                    dv(lambda e: e.tensor_tensor(out=pos[:, :], in0=pP[:, 0:NEXP], in1=cnt[:, :], op=ALU.add),
                       [t_pP, t_cnt], [t_r])
                    dv(lambda e: e.tensor_tensor(out=cnt[:, :], in0=pP[:, 64:64 + NEXP], in1=cnt[:, :], op=ALU.add),
                       [t_pP, t_cnt, t_r], [t_cnt])
                    for k, ohk in ((0, oh0), (1, oh1)):
                        dv(lambda e: e.tensor_tensor(out=t32[:, :], in0=ohk[:, :], in1=pos[:, :], op=ALU.mult),
                           [t_r], [t_r])
                        dv(lambda e: e.tensor_reduce(out=m[:, 10 + k:11 + k], in_=t32[:, :], axis=AX.X, op=ALU.add),
                           [t_r], [t_r])
                        dv(lambda e: e.tensor_tensor(out=t32[:, :], in0=ohk[:, :], in1=ebase[:, :], op=ALU.mult),
                           [t_r, t_c], [t_r])
                        dv(lambda e: e.tensor_reduce(out=m[:, 12 + k:13 + k], in_=t32[:, :], axis=AX.X, op=ALU.add),
                           [t_r], [t_r])
                    dv(lambda e: e.tensor_scalar(out=m[:, 14:16], in0=m[:, 10:12], scalar1=float(self.CAPR),
                                                 scalar2=None, op0=ALU.is_lt), [t_r], [t_r])
                    dv(lambda e: e.tensor_tensor(out=m[:, 20:22], in0=m[:, 10:12], in1=m[:, 12:14], op=ALU.add),
                       [t_r], [t_r])
                    dv(lambda e: e.tensor_tensor(out=m[:, 20:22], in0=m[:, 20:22], in1=m[:, 14:16], op=ALU.mult),
                       [t_r], [t_r])
                    dv(lambda e: e.tensor_scalar(out=m[:, 22:24], in0=m[:, 14:16], scalar1=-1.0, scalar2=-self.BIG,
                                                 op0=ALU.add, op1=ALU.mult), [t_r], [t_r])
                    dv(lambda e: e.tensor_tensor(out=m[:, 20:22], in0=m[:, 20:22], in1=m[:, 22:24], op=ALU.add),
                       [t_r], [t_r])
                    dv(lambda e: e.tensor_copy(out=dest_all[:, tti, :], in_=m[:, 20:22]), [t_r], [t_route])
                    for k in range(2):
                        s.dma("pool", None, None, reads=[t_H2, t_route], writes=[],
                              fn=lambda e: e.indirect_dma_start(
                                  out=xs_d[:, :],
                                  out_offset=bass.IndirectOffsetOnAxis(ap=dest_all[:, tti, k:k + 1], axis=0),
                                  in_=H2[:, :], in_offset=None, bounds_check=self.breg(), oob_is_err=False))
            if "dbgmerge" in (self.phases or ()):
                s.dma("sp", self.dbg_gates[:, :, :], gates_all[:, :, :], reads=[t_route])
                s.dma("sp", self.dbg_dest[:, :, :], dest_all[:, :, :], reads=[t_route])
            s.barrier()

    def phase_experts(self):
        nc, s = self.nc, self.s
        w1_d = self.din("w1", [NEXP, D, DE])
        w3_d = self.din("w3", [NEXP, D, DE])
        w2_d = self.din("w2", [NEXP, DE, D])
        ys_d = self.ys_d = self.dscr("ys_d", [self.NSLOT, D])
        xs_d = self.xs_d
        CAPB, CAPR = self.CAPB, self.CAPR
        with ExitStack() as es:
            def sb(name, shape, dt=F32):
                return es.enter_context(nc.sbuf_tensor("e_" + name, shape, dt))
            ident = sb("ident", [128, 128])
            xr = [sb(f"xr{i}", [128, D]) for i in range(2)]
            xT = sb("xT", [128, 16, CAPR], F32R)
            w1p = [sb(f"w1p{i}", [128, 16, 128], F32R) for i in range(3)]
            w3p = [sb(f"w3p{i}", [128, 16, 128], F32R) for i in range(3)]
            w2p = [sb(f"w2p{i}", [128, 4, 512], F32R) for i in range(2)]
            sil = sb("sil", [128, CAPR])
            GT = sb("GT", [128, 4, CAPR], F32R)
            yst = [sb(f"yst{i}", [128, 512]) for i in range(2)]
            ps, t_ps = self.ps, self.t_ps
            t_c = T()
            s.dma("sp", ident[:], self.ident_d[:, :], writes=[t_c])
            t_xr = [T(), T()]; t_w1 = [T(), T(), T()]; t_w3 = [T(), T(), T()]; t_w2 = [T(), T()]; t_yst = [T(), T()]
            t_xT, t_sil, t_GT = T(), T(), T()
            self.eps = 0

            def nps():
                i = self.eps
                self.eps = (i + 1) % 8
                return ps[i], t_ps[i]
            xi = wi = w2i = yi = 0
            ev = 0
            for e_ in range(NEXP):
                base = e_ * CAPR
                for blk in range(CAPB):
                    b = xi % 2
                    xi += 1
                    s.dma("sp", xr[b][:, :], xs_d[base + blk * 128:base + (blk + 1) * 128, :], writes=[t_xr[b]])
                    for k4 in range(4):
                        p_, t_p = nps()
                        for j in range(4):
                            kc = k4 * 4 + j
                            s.op("pe", lambda e: e.transpose(out=p_[:, j * 128:(j + 1) * 128],
                                                             in_=xr[b][:, kc * 128:(kc + 1) * 128],
                                                             identity=ident[:, :]), [t_xr[b], t_c], [t_p])
                        ev += 1
                        o_ = xT[:, k4 * 4:k4 * 4 + 4, blk * 128:(blk + 1) * 128]
                        i_ = p_[:, :].rearrange("p (a b) -> p a b", b=128)
                        if ev % 2:
                            s.op("act", lambda e: e.activation(out=o_, in_=i_, func=AF.Copy), [t_p], [t_xT])
                        else:
                            s.op("dve", lambda e: e.tensor_copy(out=o_, in_=i_), [t_p], [t_xT])
                for fc in range(4):
                    b = wi % 3
                    wi += 1
                    s.dma("sp", w1p[b][:, :, :], r32(w1_d[e_, :, fc * 128:(fc + 1) * 128]).rearrange(
                        "(kc p) f -> p kc f", p=128), writes=[t_w1[b]])
                    s.dma("sp", w3p[b][:, :, :], r32(w3_d[e_, :, fc * 128:(fc + 1) * 128]).rearrange(
                        "(kc p) f -> p kc f", p=128), writes=[t_w3[b]])
                    p1, t_p1 = nps()
                    for kc in range(16):
                        s.op("pe", lambda e: e.matmul(p1[:, :], lhsT=w1p[b][:, kc, :], rhs=xT[:, kc, :],
                                                      start=(kc == 0), stop=(kc == 15)), [t_w1[b], t_xT], [t_p1])
                    p3, t_p3 = nps()
                    for kc in range(16):
                        s.op("pe", lambda e: e.matmul(p3[:, :], lhsT=w3p[b][:, kc, :], rhs=xT[:, kc, :],
                                                      start=(kc == 0), stop=(kc == 15)), [t_w3[b], t_xT], [t_p3])
                    s.op("act", lambda e: e.activation(out=sil[:, :], in_=p1[:, :], func=AF.Silu), [t_p1], [t_sil])
                    s.op("dve", lambda e: e.tensor_tensor(out=GT[:, fc, :], in0=p3[:, :], in1=sil[:, :], op=ALU.mult),
                         [t_p3, t_sil], [t_GT])
                for cs in range(4):
                    b = w2i % 2
                    w2i += 1
                    s.dma("sp", w2p[b][:, :, :], r32(w2_d[e_, :, cs * 512:(cs + 1) * 512]).rearrange(
                        "(fc p) c -> p fc c", p=128), writes=[t_w2[b]])
                    for blk in range(CAPB):
                        p_, t_p = nps()
                        for fc in range(4):
                            s.op("pe", lambda e: e.matmul(p_[:, :], lhsT=GT[:, fc, blk * 128:(blk + 1) * 128],
                                                          rhs=w2p[b][:, fc, :], start=(fc == 0), stop=(fc == 3)),
                                 [t_GT, t_w2[b]], [t_p])
                        yb = yi % 2
                        yi += 1
                        ev += 1
                        if ev % 2:
                            s.op("act", lambda e: e.activation(out=yst[yb][:, :], in_=p_[:, :], func=AF.Copy),
                                 [t_p], [t_yst[yb]])
                        else:
                            s.op("dve", lambda e: e.tensor_copy(out=yst[yb][:, :], in_=p_[:, :]), [t_p], [t_yst[yb]])
                        s.dma(STQ, ys_d[base + blk * 128:base + (blk + 1) * 128, cs * 512:(cs + 1) * 512],
                              yst[yb][:, :], reads=[t_yst[yb]])
            s.barrier()

    def phase_final(self):
        nc, s = self.nc, self.s
        L, LS, PAST, TT = self.L, self.LS, self.PAST, self.TT
        p_all = self.din("p_all", [TT, PLE])
        wple_d = self.din("w_ple", [PLE, D])
        wpg_d = self.din("w_pg", [D, D])
        bpg_d = self.din("b_pg", [1, D])
        gfin_d = self.din("g_final", [1, D])
        y_all = self.dout("y_all", [TT, D])
        gates_all, dest_all, t_route = self.gates_all, self.dest_all, self.t_route
        ys_d, x2_d = self.ys_d, self.x2_d
        with ExitStack() as es:
            def sb(name, shape, dt=F32):
                return es.enter_context(nc.sbuf_tensor("f_" + name, shape, dt))
            ident = sb("ident", [128, 128])
            bpbc = sb("bpbc", [128, D]); gfbc = sb("gfbc", [128, D])
            X3 = sb("X3", [128, 4, D])
            y0s = [sb(f"y0{i}", [128, D]) for i in range(4)]; y1s = [sb(f"y1{i}", [128, D]) for i in range(4)]
            x3T = sb("x3T", [128, 16, 512], F32R)
            pts = [sb(f"pt{i}", [128, PLE]) for i in range(4)]; pT = sb("pT", [128, 2, 512], F32R)
            wpg = [sb(f"wpg{i}", [128, 16, 256], F32R) for i in range(2)]
            wpl = [sb(f"wpl{i}", [128, 2, 256], F32R) for i in range(2)]
            gt = sb("gt", [128, 256]); junk = sb("junk", [128, D]); m = sb("m", [128, 8])
            ps, t_ps = self.ps, self.t_ps
            t_c = T()
            s.dma("sp", ident[:], self.ident_d[:, :], writes=[t_c])
            s.dma("sp", bpbc[:], bpg_d[0:1, :].partition_broadcast(128), writes=[t_c])
            s.dma("sp", gfbc[:], gfin_d[0:1, :].partition_broadcast(128), writes=[t_c])
            t_X3, t_x3T, t_pT, t_gt, t_junk, t_m = (T() for _ in range(6))
            t_y0s = [T() for _ in range(4)]; t_y1s = [T() for _ in range(4)]; t_pts = [T() for _ in range(4)]
            t_wpg = [T(), T()]; t_wpl = [T(), T()]
            self.fps = 0

            def nps():
                i = self.fps
                self.fps = (i + 1) % 8
                return ps[i], t_ps[i]
            wi = 0
            for (tok0, ntok) in self.groups():
                ntile = ntok // 128
                for ti in range(ntile):
                    r0 = tok0 + ti * 128
                    tti = r0 // 128
                    y0, y1, pt = y0s[ti % 4], y1s[ti % 4], pts[ti % 4]
                    t_y0, t_y1, t_pt = t_y0s[ti % 4], t_y1s[ti % 4], t_pts[ti % 4]
                    s.dma("sp", X3[:, ti, :], x2_d[r0:r0 + 128, :], writes=[t_X3])
                    s.dma("sp", pt[:, :], p_all[r0:r0 + 128, :], writes=[t_pt])
                    s.op("dve", lambda e: e.memset(y0[:, :], 0.0), [], [t_y0])
                    s.op("dve", lambda e: e.memset(y1[:, :], 0.0), [], [t_y1])
                    for k, (yk, t_yk) in enumerate(((y0, t_y0), (y1, t_y1))):
                        s.dma("pool", None, None, reads=[t_route], writes=[t_yk],
                              fn=lambda e: e.indirect_dma_start(
                                  out=yk[:, :], out_offset=None, in_=ys_d[:, :],
                                  in_offset=bass.IndirectOffsetOnAxis(ap=dest_all[:, tti, k:k + 1], axis=0),
                                  bounds_check=self.breg(), oob_is_err=False))
                        s.op("dve", lambda e: e.scalar_tensor_tensor(out=X3[:, ti, :], in0=yk[:, :],
                                                                     scalar=gates_all[:, tti, k:k + 1],
                                                                     in1=X3[:, ti, :], op0=ALU.mult, op1=ALU.add),
                             [t_yk, t_route, t_X3], [t_X3])
                    for k4 in range(4):
                        p_, t_p = nps()
                        for j in range(4):
                            kc = k4 * 4 + j
                            s.op("pe", lambda e: e.transpose(out=p_[:, j * 128:(j + 1) * 128],
                                                             in_=X3[:, ti, kc * 128:(kc + 1) * 128],
                                                             identity=ident[:, :]), [t_X3, t_c], [t_p])
                        s.op("act", lambda e: e.activation(out=x3T[:, k4 * 4:k4 * 4 + 4, ti * 128:(ti + 1) * 128],
                                                           in_=p_[:, :].rearrange("p (a b) -> p a b", b=128),
                                                           func=AF.Copy), [t_p], [t_x3T])
                    p_, t_p = nps()
                    for j in range(2):
                        s.op("pe", lambda e: e.transpose(out=p_[:, j * 128:(j + 1) * 128],
                                                         in_=pt[:, j * 128:(j + 1) * 128], identity=ident[:, :]),
                             [t_pt, t_c], [t_p])
                    s.op("dve", lambda e: e.tensor_copy(out=pT[:, :, ti * 128:(ti + 1) * 128],
                                                        in_=p_[:, 0:256].rearrange("p (a b) -> p a b", b=128)),
                         [t_p], [t_pT])
                for cs in range(8):
                    b = wi % 2
                    wi += 1
                    s.dma("sp", wpg[b][:, :, :], r32(wpg_d[:, cs * 256:(cs + 1) * 256]).rearrange(
                        "(kc p) c -> p kc c", p=128), writes=[t_wpg[b]])
                    s.dma("sp", wpl[b][:, :, :], r32(wple_d[:, cs * 256:(cs + 1) * 256]).rearrange(
                        "(kc p) c -> p kc c", p=128), writes=[t_wpl[b]])
                    for ti in range(ntile):
                        p1, t_p1 = nps()
                        for kc in range(16):
                            s.op("pe", lambda e: e.matmul(p1[:, 0:256], lhsT=x3T[:, kc, ti * 128:(ti + 1) * 128],
                                                          rhs=wpg[b][:, kc, :], start=(kc == 0), stop=(kc == 15)),
                                 [t_x3T, t_wpg[b]], [t_p1])
                        p2, t_p2 = nps()
                        for kc in range(2):
                            s.op("pe", lambda e: e.matmul(p2[:, 0:256], lhsT=pT[:, kc, ti * 128:(ti + 1) * 128],
                                                          rhs=wpl[b][:, kc, :], start=(kc == 0), stop=(kc == 1)),
                                 [t_pT, t_wpl[b]], [t_p2])
                        s.op("dve", lambda e: e.tensor_tensor(out=gt[:, :], in0=p1[:, 0:256],
                                                              in1=bpbc[:, cs * 256:(cs + 1) * 256], op=ALU.add),
                             [t_p1, t_c], [t_gt])
                        s.op("act", lambda e: e.activation(out=gt[:, :], in_=gt[:, :], func=AF.Sigmoid),
                             [t_gt], [t_gt])
                        s.op("dve", lambda e: e.tensor_tensor(out=gt[:, :], in0=p2[:, 0:256], in1=gt[:, :],
                                                              op=ALU.mult), [t_p2, t_gt], [t_gt])
                        s.op("dve", lambda e: e.tensor_tensor(out=X3[:, ti, cs * 256:(cs + 1) * 256], in0=gt[:, :],
                                                              in1=X3[:, ti, cs * 256:(cs + 1) * 256], op=ALU.add),
                             [t_gt, t_X3], [t_X3])
                for ti in range(ntile):
                    r0 = tok0 + ti * 128
                    s.op("act", lambda e: e.activation(out=junk[:, :], in_=X3[:, ti, :], func=AF.Square,
                                                       accum_out=m[:, 0:1]), [t_X3], [t_junk, t_m])
                    s.op("act", lambda e: e.activation(out=m[:, 1:2], in_=m[:, 0:1], func=AF.Sqrt,
                                                       scale=1.0 / D, bias=NORM_EPS), [t_m], [t_m])
                    s.op("dve", lambda e: e.reciprocal(out=m[:, 2:3], in_=m[:, 1:2]), [t_m], [t_m])
                    s.op("dve", lambda e: e.scalar_tensor_tensor(out=junk[:, :], in0=X3[:, ti, :], scalar=m[:, 2:3],
                                                                 in1=gfbc[:, :], op0=ALU.mult, op1=ALU.mult),
                         [t_X3, t_m, t_c, t_junk], [t_junk])
                    s.dma(STQ, y_all[r0:r0 + 128, :], junk[:, :], reads=[t_junk])
            s.barrier()


_CACHE = {}


def _consts():
    return {"ident": np.eye(128, dtype=np.float32),
            "triu": np.triu(np.ones((128, 128), dtype=np.float32)),
            "ones": np.ones((128, 128), dtype=np.float32),
            "m_su": np.tile(np.triu(np.ones((64, 64), np.float32), 1), (1, 8)),
            "m_sl": np.tile(np.tril(np.ones((64, 64), np.float32), -1), (1, 8)),
            "m_u": np.tile(np.triu(np.ones((64, 64), np.float32), 0), (1, 8)),
            "i8": np.tile(np.eye(64, dtype=np.float32), (1, 8)),
            "slt": np.triu(np.ones((128, 128), dtype=np.float32), 1),
            "ebase": np.tile((np.arange(NEXP, dtype=np.float32) * 512.0)[None, :], (128, 1))}


L_FULL, LS_FULL, PAST_FULL = 2048, 64, 2048


def run(inputs, L, LS, PAST, ncores, phases=None):
    key = (L, LS, PAST, None if phases is None else tuple(sorted(phases)))
    if key not in _CACHE:
        _CACHE[key] = Prog(L, LS, PAST, phases=phases)
    p = _CACHE[key]
    f32 = lambda a: np.ascontiguousarray(np.asarray(a, dtype=np.float32))
    xp, xs = f32(inputs["x_prompt"]), f32(inputs["x_sample"])
    pp, psm = f32(inputs["p_prompt"][0]), f32(inputs["p_sample"][0])
    shared = dict(_consts())
    for k in ("w_in", "w_w2", "w_a2", "w_g2", "w_fox_up", "w_rwkv_up", "w_o", "w_rg", "w_re", "w1", "w3", "w2",
              "w_ple", "w_pg"):
        shared[k] = f32(inputs[k][0])
    for k in ("g_mix", "b_f", "mu_shift", "w0", "a0", "k_k", "k_a", "lnx_g", "lnx_b", "g_ffn", "b_rg", "b_re", "b_pg"):
        shared[k] = f32(inputs[k]).reshape(1, -1)
    shared["r_k"] = f32(inputs["r_k"]).reshape(1, -1)
    shared["g_final"] = f32(inputs["g_final"]).reshape(1, -1)
    in_maps = []
    for c in range(ncores):
        m = dict(shared)
        m["x_all"] = np.concatenate([xp[2 * c], xp[2 * c + 1], xs[2 * c], xs[2 * c + 1]], axis=0)
        m["p_all"] = np.concatenate([pp[2 * c], pp[2 * c + 1], psm[2 * c], psm[2 * c + 1]], axis=0)
        m["cache_k"] = f32(inputs["cache_k"][0, 2 * c:2 * c + 2])
        m["cache_v"] = f32(inputs["cache_v"][0, 2 * c:2 * c + 2])
        m["cache_logf"] = f32(inputs["cache_logf"][0, 2 * c:2 * c + 2])
        m["state_wkv"] = f32(inputs["state_wkv"][0, 2 * c:2 * c + 2])
        m["state_shift"] = f32(inputs["state_shift"][0, 2 * c:2 * c + 2])
        in_maps.append({k: v for k, v in m.items() if k in p.ins})
    res = run_bass_kernel_spmd(p.nc, in_maps, core_ids=list(range(ncores)))
    R = res.results
    B = BS = 2 * ncores
    z = lambda *sh: np.zeros(sh, np.float32)
    yp, ys = z(B, L, D), z(BS, LS, D)
    nkp, nvp, nlp = z(1, B, L, NH, HD), z(1, B, L, NH, HD), z(1, B, L, NH)
    nks, nvs, nls = z(1, BS, LS, NH, HD), z(1, BS, LS, NH, HD), z(1, BS, LS, NH)
    shp, shs = z(1, B, RSW), z(1, BS, RSW)
    wkvp, wkvs = z(1, B, RH, RD, RD), z(1, BS, RH, RD, RD)
    for c in range(ncores):
        r = R[c]
        for i in range(2):
            b = 2 * c + i
            o = 2 * L + i * LS
            nkp[0, b] = r["k_all"][i * L:(i + 1) * L].reshape(L, NH, HD)
            nvp[0, b] = r["v_all"][i * L:(i + 1) * L].reshape(L, NH, HD)
            nlp[0, b] = r["logf_all"][i * L:(i + 1) * L]
            nks[0, b] = r["k_all"][o:o + LS].reshape(LS, NH, HD)
            nvs[0, b] = r["v_all"][o:o + LS].reshape(LS, NH, HD)
            nls[0, b] = r["logf_all"][o:o + LS]
            shp[0, b] = r["shift_out"][i]
            shs[0, b] = r["shift_out"][2 + i]
            if "wkv_out" in r:
                wkvp[0, b] = r["wkv_out"][i]
                wkvs[0, b] = r["wkv_out"][2 + i]
            if "y_all" in r:
                yp[b] = r["y_all"][i * L:(i + 1) * L]
                ys[b] = r["y_all"][o:o + LS]
    _CACHE["last"] = R
    return (yp, ys, nkp, nvp, nlp, wkvp, shp, nks, nvs, nls, wkvs, shs)


def kernel(**inputs):
    return run(inputs, L_FULL, LS_FULL, PAST_FULL, 8)
```
